# Optimizing a Trainium2 kernel written in Bass

```python
import jax, jax.numpy as jnp
from jax import lax
import numpy as np

D_MODEL = 1024
BATCH = 8
SEQ = 4096
DEPTH = 1

PLE_DIM = 256
D_LRU = 512
LRU_BLOCKS = 8
LRU_BW = D_LRU // LRU_BLOCKS
CONV_W = 4
LRU_C = 8.0
D_RET = 512
RET_HEADS = 4
RET_HD = D_RET // RET_HEADS
RET_CHUNK = 128
ROPE_BASE = 10000.0
D_MIX = D_LRU + D_RET
IN_COLS = 2 * D_LRU + 4 * D_RET
SPLITS = (D_LRU, 2 * D_LRU, 2 * D_LRU + D_RET, 2 * D_LRU + 2 * D_RET, 2 * D_LRU + 3 * D_RET)
N_GROUPS = 8
EXPERTS_PER_GROUP = 8
N_EXPERTS = N_GROUPS * EXPERTS_PER_GROUP
TOP_K = 2
D_EXPERT = 512
MOE_BLOCK = 128
EPS = 1e-6

kernel_name = "hymba_rglru_retention_hmoe_ple"

F32 = jnp.float32


def rms_norm(x, g):
    xf = x.astype(F32)
    y = xf * lax.rsqrt(jnp.mean(xf * xf, axis=-1, keepdims=True) + EPS)
    return (y * g.astype(F32)).astype(x.dtype)


def causal_depthwise_conv(x, w, b):
    S = x.shape[1]
    xp = jnp.pad(x, ((0, 0), (CONV_W - 1, 0), (0, 0)))
    y = b + xp[:, 0:S, :] * w[0]
    for k in range(1, CONV_W):
        y = y + xp[:, k:k + S, :] * w[k]
    return y


def rg_lru(x, w_a, b_a, w_x, b_x, lam):
    B, S, _ = x.shape
    xf = x.astype(F32)
    xb = xf.reshape(B, S, LRU_BLOCKS, LRU_BW)
    r = jax.nn.sigmoid(jnp.einsum('bshi,hij->bshj', xb, w_a.astype(F32)) + b_a.astype(F32)).reshape(B, S, D_LRU)
    i = jax.nn.sigmoid(jnp.einsum('bshi,hij->bshj', xb, w_x.astype(F32)) + b_x.astype(F32)).reshape(B, S, D_LRU)
    log_a = -LRU_C * r * jax.nn.softplus(-lam.astype(F32))
    a = jnp.exp(log_a)
    u = jnp.sqrt(-jnp.expm1(2.0 * log_a)) * (i * xf)

    def combine(left, right):
        a1, b1 = left
        a2, b2 = right
        return a1 * a2, a2 * b1 + b2

    _, h = lax.associative_scan(combine, (a, u), axis=1)
    return h.astype(x.dtype)


def rotary(x, pos):
    half = RET_HD // 2
    inv = ROPE_BASE ** (-jnp.arange(half, dtype=F32) / half)
    ang = pos.astype(F32)[:, None] * inv[None, :]
    cos = jnp.cos(ang).astype(x.dtype)
    sin = jnp.sin(ang).astype(x.dtype)
    x1, x2 = x[..., :half], x[..., half:]
    return jnp.concatenate([x1 * cos - x2 * sin, x1 * sin + x2 * cos], axis=-1)


def chunkwise_retention(q, k, v):
    B, H, S, Dh = q.shape
    C = RET_CHUNK
    N = S // C
    dt = q.dtype
    log_g = jnp.log(1.0 - 2.0 ** (-5.0 - jnp.arange(H, dtype=F32)))
    idx = jnp.arange(C, dtype=F32)
    diff = idx[:, None] - idx[None, :]
    decay = jnp.where(diff >= 0, jnp.exp(jnp.maximum(diff, 0.0)[None] * log_g[:, None, None]), 0.0)
    qc = q.reshape(B, H, N, C, Dh)
    kc = k.reshape(B, H, N, C, Dh)
    vc = v.reshape(B, H, N, C, Dh)
    scores = jnp.einsum('bhncd,bhnmd->bhncm', qc, kc) * decay[None, :, None].astype(dt)
    inner = jnp.einsum('bhncm,bhnme->bhnce', scores, vc)
    k_decay = jnp.exp((C - 1.0 - idx)[None, :] * log_g[:, None]).astype(dt)
    chunk_kv = jnp.einsum('bhncd,bhnce->bhnde', kc * k_decay[None, :, None, :, None], vc)
    chunk_decay = jnp.exp(C * log_g).astype(dt)[None, :, None, None]

    def step(state, kv):
        return state * chunk_decay + kv, state

    init = jnp.zeros((B, H, Dh, Dh), chunk_kv.dtype)
    _, prev = lax.scan(step, init, jnp.moveaxis(chunk_kv, 2, 0))
    prev = jnp.moveaxis(prev, 0, 2)
    q_decay = jnp.exp((idx + 1.0)[None, :] * log_g[:, None]).astype(dt)
    cross = jnp.einsum('bhncd,bhnde->bhnce', qc * q_decay[None, :, None, :, None], prev)
    return (inner + cross).reshape(B, H, S, Dh)


def head_group_norm(o):
    of = o.astype(F32)
    mu = jnp.mean(of, axis=-1, keepdims=True)
    var = jnp.mean(jnp.square(of - mu), axis=-1, keepdims=True)
    return ((of - mu) * lax.rsqrt(var + EPS)).astype(o.dtype)


def hierarchical_route(h, w_grp, b_grp, w_exp, b_exp):
    T = h.shape[0]
    hf = h.astype(F32)
    g_logits = hf @ w_grp.astype(F32) + b_grp.astype(F32)
    g_prob = jax.nn.softmax(g_logits, axis=-1)
    g_sel = jnp.argmax(g_logits, axis=-1)
    g_w = jnp.max(g_prob, axis=-1)
    e_logits = (hf @ w_exp.astype(F32) + b_exp.astype(F32)).reshape(T, N_GROUPS, EXPERTS_PER_GROUP)
    sel = jnp.broadcast_to(g_sel[:, None, None], (T, 1, EXPERTS_PER_GROUP))
    e_in = jnp.take_along_axis(e_logits, sel, axis=1)[:, 0]
    top_v, top_i = lax.top_k(e_in, TOP_K)
    top_w = jax.nn.softmax(top_v, axis=-1) * g_w[:, None]
    expert_id = g_sel[:, None].astype(jnp.int32) * EXPERTS_PER_GROUP + top_i.astype(jnp.int32)
    return expert_id, top_w


def sparse_moe(h, expert_id, weight, w1, w3, w2):
    T, D = h.shape
    A = T * TOP_K
    e_flat = expert_id.reshape(A)
    tok_flat = jnp.repeat(jnp.arange(T, dtype=jnp.int32), TOP_K)
    w_flat = weight.reshape(A)
    order = jnp.argsort(e_flat)
    e_s, tok_s, w_s = e_flat[order], tok_flat[order], w_flat[order]
    counts = jnp.bincount(e_flat, length=N_EXPERTS)
    padded = (counts + MOE_BLOCK - 1) // MOE_BLOCK * MOE_BLOCK
    pad_end = jnp.cumsum(padded)
    pad_start = pad_end - padded
    start = jnp.cumsum(counts) - counts
    dest = pad_start[e_s] + (jnp.arange(A, dtype=jnp.int32) - start[e_s])
    n_blocks = -(-A // MOE_BLOCK) + N_EXPERTS
    n_slots = n_blocks * MOE_BLOCK
    slot_tok = jnp.zeros((n_slots,), jnp.int32).at[dest].set(tok_s)
    slot_w = jnp.zeros((n_slots,), w_s.dtype).at[dest].set(w_s)
    block_start = jnp.arange(n_blocks, dtype=pad_end.dtype) * MOE_BLOCK
    block_exp = jnp.minimum(jnp.searchsorted(pad_end, block_start, side='right'), N_EXPERTS - 1)
    xs = h[slot_tok].reshape(n_blocks, MOE_BLOCK, D)

    def expert_block(args):
        xb, e = args
        return (jax.nn.silu(xb @ w1[e]) * (xb @ w3[e])) @ w2[e]

    ys = lax.map(expert_block, (xs, block_exp)).reshape(n_slots, D)
    return jnp.zeros_like(h).at[slot_tok].add(ys * slot_w.astype(ys.dtype)[:, None])


def setup_inputs(seed: int = 0) -> dict:
    key = jax.random.key(seed)
    ks = jax.random.split(key, 32)
    L, D = DEPTH, D_MODEL
    nrm = lambda k, shape, s: jax.random.normal(k, shape, F32) * s
    u = jax.random.uniform(ks[10], (L, D_LRU), F32, 0.9, 0.999)
    sig = u ** (1.0 / LRU_C)
    lam = jnp.log(sig) - jnp.log1p(-sig)
    return {
        "x": nrm(ks[0], (BATCH, SEQ, D), 1.0),
        "p": nrm(ks[1], (L, BATCH, SEQ, PLE_DIM), 1.0),
        "g_mix": 1.0 + nrm(ks[2], (L, D), 0.01),
        "w_in": nrm(ks[3], (L, D, IN_COLS), D ** -0.5),
        "conv_w": nrm(ks[4], (L, CONV_W, D_LRU), CONV_W ** -0.5),
        "conv_b": nrm(ks[5], (L, D_LRU), 0.01),
        "lru_wa": nrm(ks[6], (L, LRU_BLOCKS, LRU_BW, LRU_BW), LRU_BW ** -0.5),
        "lru_ba": nrm(ks[7], (L, LRU_BLOCKS, LRU_BW), 0.01),
        "lru_wx": nrm(ks[8], (L, LRU_BLOCKS, LRU_BW, LRU_BW), LRU_BW ** -0.5),
        "lru_bx": nrm(ks[9], (L, LRU_BLOCKS, LRU_BW), 0.01),
        "lru_lambda": lam,
        "w_out": nrm(ks[11], (L, D_MIX, D), D_MIX ** -0.5),
        "g_ffn": 1.0 + nrm(ks[12], (L, D), 0.01),
        "w_router_group": nrm(ks[13], (L, D, N_GROUPS), D ** -0.5),
        "b_router_group": nrm(ks[14], (L, N_GROUPS), 0.01),
        "w_router_expert": nrm(ks[15], (L, D, N_EXPERTS), D ** -0.5),
        "b_router_expert": nrm(ks[16], (L, N_EXPERTS), 0.01),
        "w1": nrm(ks[17], (L, N_EXPERTS, D, D_EXPERT), D ** -0.5),
        "w3": nrm(ks[18], (L, N_EXPERTS, D, D_EXPERT), D ** -0.5),
        "w2": nrm(ks[19], (L, N_EXPERTS, D_EXPERT, D), D_EXPERT ** -0.5),
        "g_ple": 1.0 + nrm(ks[20], (L, D), 0.01),
        "w_ple_gate": nrm(ks[21], (L, D, D), D ** -0.5),
        "b_ple_gate": nrm(ks[22], (L, D), 0.01),
        "w_ple_proj": nrm(ks[23], (L, PLE_DIM, D), PLE_DIM ** -0.5),
        "g_final": 1.0 + nrm(ks[24], (D,), 0.01),
    }


def reference(x, p, g_mix, w_in, conv_w, conv_b, lru_wa, lru_ba, lru_wx, lru_bx, lru_lambda, w_out,
              g_ffn, w_router_group, b_router_group, w_router_expert, b_router_expert, w1, w3, w2,
              g_ple, w_ple_gate, b_ple_gate, w_ple_proj, g_final):
    B, S, D = x.shape
    pos = jnp.arange(S, dtype=jnp.int32)
    for l in range(DEPTH):
        h = rms_norm(x, g_mix[l])
        proj = h @ w_in[l]
        xl, gl, q, k, v, gr = jnp.split(proj, SPLITS, axis=-1)
        xl = causal_depthwise_conv(xl, conv_w[l], conv_b[l])
        y_lru = rg_lru(xl, lru_wa[l], lru_ba[l], lru_wx[l], lru_bx[l], lru_lambda[l]) * jax.nn.gelu(gl)
        to_heads = lambda t: t.reshape(B, S, RET_HEADS, RET_HD).transpose(0, 2, 1, 3)
        qh = rotary(to_heads(q), pos)
        kh = rotary(to_heads(k), pos) * (RET_HD ** -0.5)
        o = chunkwise_retention(qh, kh, to_heads(v))
        o = head_group_norm(o).transpose(0, 2, 1, 3).reshape(B, S, D_RET)
        y_ret = jax.nn.silu(gr) * o
        x = x + jnp.concatenate([y_lru, y_ret], axis=-1) @ w_out[l]
        hf = rms_norm(x, g_ffn[l]).reshape(B * S, D)
        eid, ew = hierarchical_route(hf, w_router_group[l], b_router_group[l], w_router_expert[l], b_router_expert[l])
        x = x + sparse_moe(hf, eid, ew, w1[l], w3[l], w2[l]).reshape(B, S, D)
        gate = jax.nn.sigmoid(rms_norm(x, g_ple[l]) @ w_ple_gate[l] + b_ple_gate[l])
        x = x + gate * (p[l] @ w_ple_proj[l])
    return rms_norm(x, g_final)
```

```python
import contextlib
import numpy as np
import concourse.bass as bass
import concourse.mybir as mybir
from concourse.bass_utils import run_bass_kernel_spmd

F32 = mybir.dt.float32
F32R = mybir.dt.float32r
I32 = mybir.dt.int32
BF16 = mybir.dt.bfloat16
AF = mybir.ActivationFunctionType
ALU = mybir.AluOpType
AX = mybir.AxisListType

D = 1024
KD = 8
NE = 64
DE = 512
CAP = 256
EPS = 1e-6
ENGINES = ("tensor", "vector", "scalar", "gpsimd", "sync")


class Op:
    __slots__ = ("eng", "fn", "is_dma", "needs_inc", "inc_index", "waits", "order_deps",
                 "dma_sem", "dma_val", "dma_prev", "idx", "cost", "xfer", "succ", "ndeps",
                 "finish", "ready")

    def __init__(self, eng, fn, is_dma):
        self.eng = eng
        self.fn = fn
        self.is_dma = is_dma
        self.needs_inc = False
        self.inc_index = None
        self.waits = []
        self.order_deps = []
        self.dma_sem = None
        self.dma_val = None
        self.dma_prev = 0
        self.succ = []
        self.ndeps = 0
        self.finish = 0.0
        self.ready = 0.0
        self.cost = 0.1
        self.xfer = 0.0


class _Mock:
    def __init__(self):
        self.calls = []

    def __getattr__(self, name):
        def f(*a, **k):
            self.calls.append((name, a, k))
            return self
        return f


def _fsize(ap):
    try:
        return int(ap.free_size())
    except Exception:
        return 256


def _estimate(op):
    m = _Mock()
    try:
        op.fn(m)
        name, a, k = m.calls[0]
    except Exception:
        return
    if op.is_dma:
        try:
            o = k.get("out", a[0] if a else None)
            i = k.get("in_", None)
            nb = min(o.nbytes(), i.nbytes()) if name == "indirect_dma_start" else max(o.nbytes(), i.nbytes())
        except Exception:
            nb = 65536
        op.xfer = nb / 300e3
        op.cost = 1.2 if op.eng == "gpsimd" else 0.08
        return
    if op.eng == "tensor":
        if name == "matmul":
            n = _fsize(a[2])
            mult = 4.0 if a[1].dtype == F32 else 1.0
            op.cost = max(n * mult, 64.0) / 2000.0
        else:
            op.cost = 0.13
    elif op.eng == "vector":
        o = k.get("out", a[0] if a else None)
        n = _fsize(o)
        if name == "tensor_reduce" or name == "max":
            n = _fsize(a[1])
        if name == "tensor_tensor_scan":
            n *= 2
        op.cost = (max(n, 64) + 60) / 960.0
    elif op.eng == "scalar":
        o = k.get("out", a[0] if a else None)
        op.cost = (max(_fsize(o), 64) + 250) / 1400.0
    else:
        op.cost = 0.2


class Sched:
    def __init__(self, nc, n_dma_sems=48):
        self.nc = nc
        self.last_writer = {}
        self.readers = {}
        self.n_dma_sems = n_dma_sems
        self.all_ops = []
        self.barrier_op = None
        self.last_of = {e: None for e in ENGINES}

    def add(self, eng, fn, reads=(), writes=(), dma=False):
        op = Op(eng, fn, dma)
        op.idx = len(self.all_ops)
        deps = []
        if self.barrier_op is not None:
            deps.append(self.barrier_op)
        for k in reads:
            w = self.last_writer.get(k)
            if w is not None:
                deps.append(w)
        for k in writes:
            w = self.last_writer.get(k)
            if w is not None:
                deps.append(w)
            deps.extend(self.readers.get(k, ()))
        seen = set()
        for d in deps:
            if id(d) in seen or d is op:
                continue
            seen.add(id(d))
            if (not d.is_dma) and d.eng == eng and eng == "tensor":
                op.order_deps.append(d)
            else:
                op.waits.append(d)
        for k in writes:
            self.last_writer[k] = op
            self.readers[k] = []
        for k in reads:
            if k not in writes:
                self.readers.setdefault(k, []).append(op)
        self.all_ops.append(op)
        self.last_of[eng] = op
        return op

    def barrier(self, fn):
        op = self.add("vector", fn, writes=["__barrier__"])
        have = set(id(w) for w in op.waits)
        for d in self.all_ops[:-1]:
            if id(d) not in have:
                op.waits.append(d)
        self.barrier_op = op
        self.last_writer = {}
        self.readers = {}
        return op

    def schedule(self):
        import heapq
        ops = self.all_ops
        for op in ops:
            _estimate(op)
            op.ndeps = 0
            op.succ = []
        for op in ops:
            for d in op.waits:
                d.succ.append(op)
                op.ndeps += 1
            for d in op.order_deps:
                d.succ.append(op)
                op.ndeps += 1
        future = {e: [] for e in ENGINES}
        avail = {e: [] for e in ENGINES}
        free_at = {e: 0.0 for e in ENGINES}
        dma_free = [0.0]
        order = {e: [] for e in ENGINES}
        for op in ops:
            if op.ndeps == 0:
                heapq.heappush(future[op.eng], (0.0, op.idx, op))
        remaining = len(ops)
        while remaining:
            best = None
            for e in ENGINES:
                fu, av = future[e], avail[e]
                while fu and fu[0][0] <= free_at[e]:
                    r, i, o = heapq.heappop(fu)
                    heapq.heappush(av, (i, o))
                if av:
                    cand = (free_at[e], av[0][0], e, True)
                elif fu:
                    cand = (fu[0][0], fu[0][1], e, False)
                else:
                    continue
                if best is None or cand < best:
                    best = cand
            assert best is not None, "scheduler stuck (dependency cycle?)"
            t, _, e, from_av = best
            if from_av:
                _, op = heapq.heappop(avail[e])
            else:
                _, _, op = heapq.heappop(future[e])
            start = max(t, free_at[e])
            free_at[e] = start + op.cost
            if op.is_dma:
                s0 = max(start + op.cost, dma_free[0])
                dma_free[0] = s0 + op.xfer
                op.finish = s0 + op.xfer + 2.0
            else:
                op.finish = start + op.cost
            order[e].append(op)
            remaining -= 1
            for sc in op.succ:
                lat = 0.0 if (sc.eng == op.eng and not op.is_dma) else 0.35
                r = op.finish + lat
                if r > sc.ready:
                    sc.ready = r
                sc.ndeps -= 1
                if sc.ndeps == 0:
                    heapq.heappush(future[sc.eng], (sc.ready, sc.idx, sc))
        self.est_time = max(op.finish for op in ops)
        return order

    def emit(self, final_wait_ops=()):
        nc = self.nc
        order = self.schedule()
        pos = {}
        for e in ENGINES:
            for i, op in enumerate(order[e]):
                pos[id(op)] = i
        for op in self.all_ops:
            if len(op.waits) > 256:
                keep = {}
                dm = []
                for d in op.waits:
                    if d.is_dma:
                        dm.append(d)
                    elif d.eng not in keep or pos[id(d)] > pos[id(keep[d.eng])]:
                        keep[d.eng] = d
                op.waits = dm + list(keep.values())
        for op in self.all_ops:
            for d in op.waits:
                if not d.is_dma:
                    d.needs_inc = True
        half = self.n_dma_sems // 2
        for e in ENGINES:
            c = 0
            rr = 0
            counts = [0] * self.n_dma_sems
            for op in order[e]:
                if op.is_dma:
                    base = half if e == "gpsimd" else 0
                    s = base + rr
                    rr = (rr + 1) % half
                    op.dma_sem = s
                    op.dma_prev = counts[s]
                    counts[s] += 16
                    op.dma_val = counts[s]
                elif op.needs_inc:
                    c += 1
                    op.inc_index = c
        with contextlib.ExitStack() as st:
            esem = {e: st.enter_context(nc.semaphore("es_" + e)) for e in ENGINES}
            dsem = [st.enter_context(nc.semaphore("ds_%d" % i)) for i in range(self.n_dma_sems)]
            block = st.enter_context(nc.Block())
            sched = self

            def run_engine(e, engobj):
                waited_e = {x: 0 for x in ENGINES}
                waited_d = [0] * sched.n_dma_sems
                for op in order[e]:
                    need_e = {}
                    need_d = {}
                    for d in op.waits:
                        if d.is_dma:
                            if d.dma_val > need_d.get(d.dma_sem, 0):
                                need_d[d.dma_sem] = d.dma_val
                        else:
                            if d.inc_index > need_e.get(d.eng, 0):
                                need_e[d.eng] = d.inc_index
                    if op.is_dma and op.dma_prev > need_d.get(op.dma_sem, 0):
                        need_d[op.dma_sem] = op.dma_prev
                    for pe, v in need_e.items():
                        if v > waited_e[pe]:
                            engobj.wait_ge(esem[pe], v)
                            waited_e[pe] = v
                    for s, v in need_d.items():
                        if v > waited_d[s]:
                            engobj.wait_ge(dsem[s], v)
                            waited_d[s] = v
                    ins = op.fn(engobj)
                    if op.is_dma:
                        ins.then_inc(dsem[op.dma_sem], 16)
                    elif op.needs_inc:
                        ins.then_inc(esem[e], 1)
                if e == "sync":
                    for d in final_wait_ops:
                        if d.dma_val > waited_d[d.dma_sem]:
                            engobj.wait_ge(dsem[d.dma_sem], d.dma_val)
                            waited_d[d.dma_sem] = d.dma_val

            @block.tensor
            def _(t):
                run_engine("tensor", t)

            @block.vector
            def _(v):
                run_engine("vector", v)

            @block.scalar
            def _(s):
                run_engine("scalar", s)

            @block.gpsimd
            def _(g):
                run_engine("gpsimd", g)

            @block.sync
            def _(sy):
                run_engine("sync", sy)


def make_consts(S):
    H, C, Dh = 4, 128, 128
    log_g = np.log(1.0 - 2.0 ** (-5.0 - np.arange(H, dtype=np.float64)))
    idx = np.arange(C, dtype=np.float64)
    diff = idx[:, None] - idx[None, :]
    decay = np.where(diff >= 0, np.exp(np.maximum(diff, 0.0)[None] * log_g[:, None, None]), 0.0)
    decayT = np.transpose(decay, (2, 0, 1))
    q_decay = np.exp((idx + 1.0)[None, :] * log_g[:, None])
    k_decay = np.exp((C - 1.0 - idx)[None, :] * log_g[:, None])
    chunk_decay = np.exp(C * log_g)
    QD = np.broadcast_to(q_decay[None, :, :], (128, H, C))
    KDc = np.transpose(k_decay, (1, 0))
    CDc = np.broadcast_to(chunk_decay[None, :], (128, H))
    half = Dh // 2
    inv = 10000.0 ** (-np.arange(half, dtype=np.float32).astype(np.float64) / half)
    pos = np.arange(S, dtype=np.float64)
    ang = (pos[:, None].astype(np.float32) * inv[None, :].astype(np.float32)).astype(np.float64)
    cos = np.cos(ang)
    sin = np.sin(ang)
    cosF = np.concatenate([cos, cos], axis=1)
    sinF = np.concatenate([-sin, sin], axis=1)
    sc = Dh ** -0.5
    rope = np.stack([cosF, sinF, cosF * sc, sinF * sc], axis=1)
    U = np.triu(np.ones((128, 128)), k=1)
    eC = np.broadcast_to((np.arange(NE) * CAP)[None, :], (128, NE))
    f = lambda a: np.ascontiguousarray(a, dtype=np.float32)
    return dict(c_ident=f(np.eye(128)), c_decayT=f(decayT), c_QD=f(QD), c_KD=f(KDc), c_CD=f(CDc),
                c_rope=f(rope), c_U=f(U), c_ones=f(np.ones((128, 128))), c_eC=f(eC))


def bc128(v):
    v = np.asarray(v, dtype=np.float32).reshape(1, -1)
    return np.ascontiguousarray(np.broadcast_to(v, (128, v.shape[1])))


def chan_major(v):
    return np.ascontiguousarray(np.asarray(v, np.float32).reshape(4, 128).T)


def block_diag(w):
    out = np.zeros((128, 4, 128), np.float32)
    for c in range(4):
        for hh in range(2):
            out[hh * 64:(hh + 1) * 64, c, hh * 64:(hh + 1) * 64] = w[2 * c + hh]
    return out


def build_nc(S, debug=None, stop_after=None):
    NT = S // 128
    NST = S // 512
    nc = bass.Bass("TRN2", target_bir_lowering=False)

    def din(name, shape, dt=F32):
        return nc.dram_tensor(name, list(shape), dt, kind="ExternalInput").ap()

    x_d = din("x", [S, D])
    p_d = din("p", [S, 256])
    w_in_d = din("w_in", [D, 3072])
    w_out_d = din("w_out", [D, D])
    w1_d = din("w1", [NE, D, DE])
    w3_d = din("w3", [NE, D, DE])
    w2_d = din("w2", [NE, DE, D])
    wg_d = din("w_ple_gate", [D, D])
    wp_d = din("w_ple_proj", [256, D])
    wrt_d = din("w_rt", [D, 72])
    brt_d = din("b_rt", [128, 72])
    gmix_d = din("g_mix", [128, D])
    gffn_d = din("g_ffn", [128, D])
    gple_d = din("g_ple", [128, D])
    gfin_d = din("g_final", [128, D])
    bple_d = din("b_ple", [128, D])
    cw_d = din("conv_w", [128, 4, 4])
    cb_d = din("conv_b", [128, 4])
    wa_d = din("lru_wa", [128, 4, 128])
    wx_d = din("lru_wx", [128, 4, 128])
    ba_d = din("lru_ba", [128, 4])
    bx_d = din("lru_bx", [128, 4])
    lam_d = din("lru_lam", [128, 4])
    ident_d = din("c_ident", [128, 128])
    decT_d = din("c_decayT", [128, 4, 128])
    QD_d = din("c_QD", [128, 4, 128])
    KD_d = din("c_KD", [128, 4])
    CD_d = din("c_CD", [128, 4])
    rope_d = din("c_rope", [S, 4, 128])
    U_d = din("c_U", [128, 128])
    ones_d = din("c_ones", [128, 128])
    eC_d = din("c_eC", [128, NE])
    out_d = nc.dram_tensor("out", [S, D], F32, kind="ExternalOutput").ap()
    X1_d = nc.dram_tensor("X1s", [S, D], F32, kind="Internal").ap()
    XS_d = nc.dram_tensor("XSs", [NE * CAP, D], BF16, kind="Internal").ap()
    Y_d = nc.dram_tensor("Ys", [NE * CAP, D], F32, kind="Internal").ap()
    dbg_d = None
    if debug is not None:
        dbg_d = nc.dram_tensor("dbg", [S, D], F32, kind="ExternalOutput").ap()

    with contextlib.ExitStack() as st:
        RO_BASE, RO_SIZE = 0, 16384
        ARENA = 52800 - RO_SIZE
        arena = st.enter_context(nc.sbuf_tensor("arena", [128, ARENA], F32))
        arena_ro = st.enter_context(nc.sbuf_tensor("arena_ro", [128, RO_SIZE], F32))
        PL_BASE = 400
        off = [0]
        roff = [RO_BASE]

        def A(n, *shape, ro=False, bf=False):
            if ro:
                o = roff[0]
                roff[0] += n
                assert roff[0] <= RO_BASE + RO_SIZE, ("ro overflow", roff[0])
            else:
                o = off[0]
                off[0] += n
                assert off[0] <= ARENA, ("arena overflow", o, off[0])
            ap = (arena_ro if ro else arena)[:, o:o + n]
            if bf:
                ap = ap.bitcast(BF16)
            if len(shape) == 2:
                ap = ap.rearrange("p (a b) -> p a b", a=shape[0])
            elif len(shape) == 3:
                ap = ap.rearrange("p (a b c) -> p a b c", a=shape[0], b=shape[1])
            return ap

        def psum(name, n):
            return st.enter_context(nc.psum_tensor(name, [128, n], F32))

        psT_t = psum("psT", 1024)
        psO_t = psum("psO", 1024)
        psP_t = psum("psP", 1024)
        psA_t = psum("psA", 512)
        psB_t = psum("psB", 512)
        psT = psT_t[:, :].rearrange("p (a b) -> p a b", a=8)
        psO = psO_t[:, :]
        psP = psP_t[:, :]
        psA = psA_t[:, :]
        psB = psB_t[:, :]

        S_ = Sched(nc)
        rec = [None]

        def add(eng, fn, reads=(), writes=(), dma=False):
            if rec[0] is not None:
                rec[0].append((eng, fn, tuple(reads), tuple(writes), dma))
                return None
            return S_.add(eng, fn, reads=reads, writes=writes, dma=dma)

        def record(f, *args):
            rec[0] = []
            f(*args)
            out = rec[0]
            rec[0] = None
            return out

        def emit_merged(*chains):
            chains = [c for c in chains if c]
            idx = [0] * len(chains)
            while True:
                live = [i for i in range(len(chains)) if idx[i] < len(chains[i])]
                if not live:
                    break
                i = min(live, key=lambda i: idx[i] / len(chains[i]))
                eng, fn, rd, wr, dma = chains[i][idx[i]]
                idx[i] += 1
                S_.add(eng, fn, reads=rd, writes=wr, dma=dma)

        ident = A(128)
        dest1 = A(NT)
        dest2 = A(NT)
        wsel1 = A(NT)
        wsel2 = A(NT)
        dest1i = dest1.bitcast(I32)
        dest2i = dest2.bitcast(I32)
        bscr = A(1)
        identb = A(64, bf=True)
        persist_end = off[0]
        assert persist_end <= PL_BASE
        off[0] = PL_BASE

        add("sync", lambda e: e.dma_start(out=ident, in_=ident_d), writes=["ident"], dma=True)
        add("vector", lambda e: e.tensor_copy(identb, ident), reads=["ident"], writes=["identb"])

        U_t = A(128)
        ones_t = A(128)
        decT = A(512, 4, 128)
        QDt = A(512, 4, 128)
        KDt = A(4)
        CDt = A(4)
        eCt = A(NE)
        gmix = A(D)
        gffn = A(D)
        cw = A(16, 4, 4)
        cb = A(4)
        WA = A(512, 4, 128)
        WX = A(512, 4, 128)
        ba = A(4)
        bx = A(4)
        lam = A(4)
        cneg = A(4)
        c2 = A(4)
        wrt = A(KD * 72, KD, 72)
        brt = A(72)
        hstate = A(4)
        Rcum = A(NE)
        state = A(512, 4, 128)
        tmp4 = [A(4) for _ in range(6)]

        for (dst, src, key) in [(U_t, U_d, "U"), (ones_t, ones_d, "ones"), (decT, decT_d, "decT"),
                                (QDt, QD_d, "QD"), (KDt, KD_d, "KD"), (CDt, CD_d, "CD"), (eCt, eC_d, "eC"),
                                (gmix, gmix_d, "gmix"), (gffn, gffn_d, "gffn"), (cw, cw_d, "cw"), (cb, cb_d, "cb"),
                                (WA, wa_d, "WA"), (WX, wx_d, "WX"), (ba, ba_d, "ba"), (bx, bx_d, "bx"),
                                (lam, lam_d, "lam"), (brt, brt_d, "brt")]:
            add("sync", lambda e, dst=dst, src=src: e.dma_start(out=dst, in_=src), writes=[key], dma=True)
        add("sync", lambda e: e.dma_start(out=wrt, in_=wrt_d.rearrange("(c p) n -> p c n", p=128)), writes=["wrt"], dma=True)
        add("vector", lambda e: e.memset(hstate, 0.0), writes=["hstate"])
        add("vector", lambda e: e.memset(Rcum, 0.0), writes=["Rcum"])
        add("vector", lambda e: e.memset(state, 0.0), writes=["state"])

        ya, za, sa, s2, acc, t0 = tmp4
        add("vector", lambda e: e.tensor_scalar(ya, lam, -1.0, None, ALU.mult), reads=["lam"], writes=["ya"])
        add("vector", lambda e: e.tensor_tensor(za, ya, lam, ALU.min), reads=["ya", "lam"], writes=["za"])
        add("scalar", lambda e: e.activation(out=za, in_=za, func=AF.Exp), reads=["za"], writes=["za"])
        add("vector", lambda e: e.tensor_scalar(sa, za, 2.0, None, ALU.add), reads=["za"], writes=["sa"])
        add("vector", lambda e: e.reciprocal(sa, sa), reads=["sa"], writes=["sa"])
        add("vector", lambda e: e.tensor_tensor(sa, sa, za, ALU.mult), reads=["sa", "za"], writes=["sa"])
        add("vector", lambda e: e.tensor_tensor(s2, sa, sa, ALU.mult), reads=["sa"], writes=["s2"])
        add("vector", lambda e: e.memset(acc, 1.0 / 15.0), writes=["acc"])
        for k in (13, 11, 9, 7, 5, 3, 1):
            add("vector", lambda e: e.tensor_tensor(acc, acc, s2, ALU.mult), reads=["acc", "s2"], writes=["acc"])
            add("vector", lambda e, k=k: e.tensor_scalar(acc, acc, 1.0 / k, None, ALU.add), reads=["acc"], writes=["acc"])
        add("vector", lambda e: e.tensor_tensor(acc, acc, sa, ALU.mult), reads=["acc", "sa"], writes=["acc"])
        add("vector", lambda e: e.tensor_scalar(t0, ya, 0.0, None, ALU.max), reads=["ya"], writes=["t0"])
        add("vector", lambda e: e.scalar_tensor_tensor(t0, acc, 2.0, t0, ALU.mult, ALU.add), reads=["acc", "t0"], writes=["t0"])
        add("vector", lambda e: e.tensor_scalar(cneg, t0, -8.0, None, ALU.mult), reads=["t0"], writes=["cneg"])
        add("vector", lambda e: e.tensor_scalar(c2, t0, -16.0, None, ALU.mult), reads=["t0"], writes=["c2"])

        setup_end = off[0]
        wbuf = [A(KD * 512, KD, 512, ro=True) for _ in range(2)]
        xt = [A(D) for _ in range(4)]
        hn = A(D)
        hT = A(KD * 512, KD, 512, ro=True)
        XL = A(4 * 515, 4, 515)
        GL = A(4 * 512, 4, 512)
        ylruT = A(4 * 512, 4, 512, ro=True)
        lt = [A(512) for _ in range(6)]
        QKVG = [A(4 * 512, 4, 512) for _ in range(4)]
        yretT = A(4 * 512, 4, 512, ro=True)
        rt_ = [A(512) for _ in range(7)]
        ropet = A(512, 4, 128)
        hfT = A(KD * 128, KD, 128)
        hfb = A(512, bf=True)
        rs = [A(80) for _ in range(8)]
        sm = [A(1) for _ in range(16)]
        rse = [A(4) for _ in range(4)]
        phaseA_end = off[0]

        add("vector", lambda e: e.memset(XL[:, :, 0:3], 0.0), writes=["XL"])
        zt = A(1024, bf=True)

        def rms_scale(xin, xkey, gbc, gkey, outap, outkey, tag, si=0):
            ss, rstd = sm[si], sm[si + 1]
            ssk, rk = "ss%d" % si, "rstd%d" % si
            add("scalar", lambda e: e.activation(out=outap, in_=xin, func=AF.Square, accum_out=ss),
                reads=[xkey], writes=[outkey, ssk])
            add("vector", lambda e: e.tensor_scalar(ss, ss, 1.0 / D, EPS, ALU.mult, ALU.add), reads=[ssk], writes=[ssk])
            add("scalar", lambda e: e.activation(out=ss, in_=ss, func=AF.Sqrt), reads=[ssk], writes=[ssk])
            add("vector", lambda e: e.reciprocal(rstd, ss), reads=[ssk], writes=[rk])
            add("vector", lambda e: e.scalar_tensor_tensor(outap, xin, rstd, gbc, ALU.mult, ALU.mult),
                reads=[xkey, rk, gkey], writes=[outkey])

        def transpose8(src, srckey, dst3, dstkey, n=8, rounded=True, eng="scalar"):
            pk = ["psT", "psT2"] if n > 4 else ["psT"]
            for c in range(n):
                add("tensor", lambda e, c=c: e.transpose(psT[:, c, :], src[:, c * 128:(c + 1) * 128], ident),
                    reads=[srckey, "ident"], writes=pk)
            o = dst3.bitcast(F32R) if rounded else dst3
            if eng == "scalar":
                add("scalar", lambda e: e.activation(out=o, in_=psT[:, 0:n, :], func=AF.Copy), reads=pk, writes=[dstkey])
            else:
                add("vector", lambda e: e.tensor_copy(o, psT[:, 0:n, :]), reads=pk, writes=[dstkey])

        wcount = [0]

        def load_group(src_ap, nk=KD):
            k = wcount[0] % 2
            wcount[0] += 1
            dst = wbuf[k].bitcast(F32R)
            if nk != KD:
                dst = wbuf[k][:, 0:nk, :].bitcast(F32R)
            add("gpsimd", lambda e: e.dma_start(out=dst, in_=src_ap.rearrange("(c p) n -> p c n", p=128)),
                writes=["wbuf%d" % k], dma=True)
            return wbuf[k].bitcast(F32R), "wbuf%d" % k

        pp = [0]

        def next_ps():
            pp[0] ^= 1
            return (psA, "psA") if pp[0] else (psB, "psB")

        for stile in range(NST):
            for j in range(4):
                t = stile * 4 + j
                add("sync", lambda e, j=j, t=t: e.dma_start(out=xt[j], in_=x_d[t * 128:(t + 1) * 128, :]),
                    writes=["xt%d" % j], dma=True)
                rms_scale(xt[j], "xt%d" % j, gmix, "gmix", hn, "hn", "a")
                transpose8(hn, "hn", hT[:, :, j * 128:(j + 1) * 128], "hT")
            XS_v = XS_d.rearrange("(p r) d -> p (r d)", p=128)
            nz = (NE * CAP // 128) * D // 2048

            def emit_zero(part):
                if part == 0:
                    add("vector", lambda e: e.memset(zt, 0.0), writes=["zt"])
                for kz in range(part * nz // 4, (part + 1) * nz // 4):
                    add("sync", lambda e, kz=kz: e.dma_start(out=XS_v[:, kz * 2048:(kz + 1) * 2048], in_=zt),
                        reads=["zt", "ylruT"], writes=["XSz%d" % kz], dma=True)
                if part == 3:
                    add("vector", lambda e: e.memset(bscr, 0.0), reads=["XSz%d" % kz for kz in range(nz)], writes=["XSd"])
            hTr = hT.bitcast(F32R)
            for g, (dst, dkey, o0) in enumerate([(XL, "XL", 3), (GL, "GL", 0)]):
                wb, wkey = load_group(w_in_d[:, g * 512:(g + 1) * 512])
                for c in range(4):
                    ps, pkey = next_ps()
                    for kc in range(KD):
                        add("tensor", lambda e, ps=ps, wb=wb, kc=kc, c=c: e.matmul(
                            ps, wb[:, kc, c * 128:(c + 1) * 128], hTr[:, kc, :], start=(kc == 0), stop=(kc == KD - 1)),
                            reads=[wkey, "hT"], writes=[pkey])
                    add("scalar", lambda e, ps=ps, dst=dst, c=c, o0=o0: e.activation(out=dst[:, c, o0:o0 + 512], in_=ps, func=AF.Copy),
                        reads=[pkey], writes=[dkey])
            def stage_c(c):
                xc, r_, i_, a_, u_, g_ = lt
                add("vector", lambda e, c=c: e.tensor_scalar(xc, XL[:, c, 3:515], cw[:, c, 3:4], cb[:, c:c + 1], ALU.mult, ALU.add),
                    reads=["XL", "cw", "cb"], writes=["xc"])
                for k in (2, 1, 0):
                    add("vector", lambda e, c=c, k=k: e.scalar_tensor_tensor(xc, XL[:, c, k:k + 512], cw[:, c, k:k + 1], xc, ALU.mult, ALU.add),
                        reads=["XL", "cw", "xc"], writes=["xc"])
                add("tensor", lambda e, c=c: e.matmul(psP[:, 0:512], WA[:, c, :], xc, start=True, stop=True), reads=["WA", "xc"], writes=["psP0"])
                add("tensor", lambda e, c=c: e.matmul(psP[:, 512:1024], WX[:, c, :], xc, start=True, stop=True), reads=["WX", "xc"], writes=["psP1"])
                add("scalar", lambda e, c=c: e.activation(out=r_, in_=psP[:, 0:512], func=AF.Sigmoid, bias=ba[:, c:c + 1]), reads=["psP0", "ba"], writes=["r_"])
                add("scalar", lambda e, c=c: e.activation(out=i_, in_=psP[:, 512:1024], func=AF.Sigmoid, bias=bx[:, c:c + 1]), reads=["psP1", "bx"], writes=["i_"])
                add("scalar", lambda e, c=c: e.activation(out=a_, in_=r_, func=AF.Exp, scale=cneg[:, c:c + 1]), reads=["r_", "cneg"], writes=["a_"])
                add("scalar", lambda e, c=c: e.activation(out=u_, in_=r_, func=AF.Exp, scale=c2[:, c:c + 1]), reads=["r_", "c2"], writes=["u_"])
                add("vector", lambda e: e.tensor_scalar(u_, u_, -1.0, 1.0, ALU.mult, ALU.add), reads=["u_"], writes=["u_"])
                add("scalar", lambda e: e.activation(out=u_, in_=u_, func=AF.Sqrt), reads=["u_"], writes=["u_"])
                add("vector", lambda e: e.tensor_tensor(i_, i_, xc, ALU.mult), reads=["i_", "xc"], writes=["i_"])
                add("vector", lambda e: e.tensor_tensor(u_, u_, i_, ALU.mult), reads=["u_", "i_"], writes=["u_"])
                add("vector", lambda e, c=c: e.tensor_tensor_scan(r_, a_, u_, hstate[:, c:c + 1], ALU.mult, ALU.add),
                    reads=["a_", "u_", "hstate"], writes=["r_"])
                add("vector", lambda e, c=c: e.tensor_copy(hstate[:, c:c + 1], r_[:, 511:512]), reads=["r_"], writes=["hstate"])
                add("vector", lambda e, c=c: e.tensor_tensor(g_, GL[:, c, :], GL[:, c, :], ALU.mult), reads=["GL"], writes=["g_"])
                add("vector", lambda e: e.tensor_scalar(g_, g_, 0.044715, 1.0, ALU.mult, ALU.add), reads=["g_"], writes=["g_"])
                add("vector", lambda e, c=c: e.tensor_tensor(g_, g_, GL[:, c, :], ALU.mult), reads=["g_", "GL"], writes=["g_"])
                add("scalar", lambda e: e.activation(out=g_, in_=g_, func=AF.Sigmoid, scale=1.5957691216057308), reads=["g_"], writes=["g_"])
                add("vector", lambda e, c=c: e.tensor_tensor(g_, g_, GL[:, c, :], ALU.mult), reads=["g_", "GL"], writes=["g_"])
                add("vector", lambda e, c=c: e.tensor_tensor(ylruT[:, c, :].bitcast(F32R), r_, g_, ALU.mult), reads=["r_", "g_"], writes=["ylruT"])

            def stage_d(gi):
                wb, wkey = load_group(w_in_d[:, (2 + gi) * 512:(3 + gi) * 512])
                for j in range(4):
                    ps, pkey = next_ps()
                    for kc in range(KD):
                        add("tensor", lambda e, ps=ps, wb=wb, kc=kc, j=j: e.matmul(
                            ps, hTr[:, kc, j * 128:(j + 1) * 128], wb[:, kc, :], start=(kc == 0), stop=(kc == KD - 1)),
                            reads=[wkey, "hT"], writes=[pkey])
                    add("scalar", lambda e, ps=ps, gi=gi, j=j: e.activation(out=QKVG[gi][:, j, :], in_=ps, func=AF.Copy),
                        reads=[pkey], writes=["qkvg%d_%d" % (gi, j)])

            def stage_e(j):
                t = stile * 4 + j
                Qj = QKVG[0][:, j, :].rearrange("p (h d) -> p h d", h=4)
                Kj = QKVG[1][:, j, :].rearrange("p (h d) -> p h d", h=4)
                Vj = QKVG[2][:, j, :].rearrange("p (h d) -> p h d", h=4)
                Gj = QKVG[3][:, j, :]
                qr, kr, ta, qT, qdT, kT, kd = [r.rearrange("p (h d) -> p h d", h=4) for r in rt_]
                osb, osq, sT = qr, kr, ta
                add("sync", lambda e, t=t: e.dma_start(out=ropet, in_=rope_d[t * 128:(t + 1) * 128, :, :]), writes=["rope"], dma=True)

                def rotary(src, skey, dst, dkey, ci):
                    cosb = ropet[:, ci, :].unsqueeze(1).to_broadcast([128, 4, 128])
                    add("vector", lambda e: e.tensor_tensor(dst, src, cosb, ALU.mult), reads=[skey, "rope"], writes=[dkey])
                    s_lo = ropet[:, ci + 1, 0:64].unsqueeze(1).to_broadcast([128, 4, 64])
                    s_hi = ropet[:, ci + 1, 64:128].unsqueeze(1).to_broadcast([128, 4, 64])
                    add("vector", lambda e: e.tensor_tensor(ta[:, :, 0:64], src[:, :, 64:128], s_lo, ALU.mult), reads=[skey, "rope"], writes=["R2"])
                    add("vector", lambda e: e.tensor_tensor(ta[:, :, 64:128], src[:, :, 0:64], s_hi, ALU.mult), reads=[skey, "rope"], writes=["R2"])
                    add("vector", lambda e: e.tensor_tensor(dst, dst, ta, ALU.add), reads=[dkey, "R2"], writes=[dkey])

                rotary(Qj, "qkvg0_%d" % j, qr, "R0", 0)
                rotary(Kj, "qkvg1_%d" % j, kr, "R1", 2)
                for h in range(4):
                    add("tensor", lambda e, h=h: e.transpose(psT[:, h, :], qr[:, h, :], ident), reads=["R0", "ident"], writes=["psT"])
                add("scalar", lambda e: e.activation(out=qT, in_=psT[:, 0:4, :], func=AF.Copy), reads=["psT"], writes=["R3"])
                add("vector", lambda e: e.tensor_tensor(qdT, qT, QDt, ALU.mult), reads=["R3", "QD"], writes=["R4"])
                for h in range(4):
                    add("tensor", lambda e, h=h: e.transpose(psT[:, h, :], kr[:, h, :], ident), reads=["R1", "ident"], writes=["psT"])
                add("scalar", lambda e: e.activation(out=kT, in_=psT[:, 0:4, :], func=AF.Copy), reads=["psT"], writes=["R5"])
                add("vector", lambda e: e.tensor_tensor(kd, kr, KDt.unsqueeze(2).to_broadcast([128, 4, 128]), ALU.mult), reads=["R1", "KD"], writes=["R6"])
                psA3 = psA.rearrange("p (h d) -> p h d", h=4)
                psB3 = psB.rearrange("p (h d) -> p h d", h=4)
                psO3 = psO[:, 0:512].rearrange("p (h d) -> p h d", h=4)
                for h in range(4):
                    add("tensor", lambda e, h=h: e.matmul(psA3[:, h, :], kT[:, h, :], qT[:, h, :], start=True, stop=True),
                        reads=["R5", "R3"], writes=["psA"])
                add("vector", lambda e: e.tensor_tensor(sT, psA3, decT, ALU.mult), reads=["psA", "decT"], writes=["R2"])
                for h in range(4):
                    add("tensor", lambda e, h=h, Vj=Vj: e.matmul(psB3[:, h, :], sT[:, h, :], Vj[:, h, :], start=True, stop=False),
                        reads=["R2", "qkvg2_%d" % j], writes=["psB"])
                    add("tensor", lambda e, h=h: e.matmul(psB3[:, h, :], qdT[:, h, :], state[:, h, :], start=False, stop=True),
                        reads=["R4", "state"], writes=["psB"])
                for h in range(4):
                    add("tensor", lambda e, h=h, Vj=Vj: e.matmul(psO3[:, h, :], kd[:, h, :], Vj[:, h, :], start=True, stop=True),
                        reads=["R6", "qkvg2_%d" % j], writes=["psO"])
                add("vector", lambda e: e.tensor_tensor(state, state, CDt.unsqueeze(2).to_broadcast([128, 4, 128]), ALU.mult),
                    reads=["state", "CD"], writes=["state"])
                add("vector", lambda e: e.tensor_tensor(state, state, psO3, ALU.add), reads=["state", "psO"], writes=["state"])
                s1, s2_, mu, var = rse
                add("scalar", lambda e: e.activation(out=osb, in_=psB3, func=AF.Copy), reads=["psB"], writes=["R0"])
                add("scalar", lambda e: e.activation(out=osq, in_=psB3, func=AF.Square), reads=["psB"], writes=["R1"])
                add("vector", lambda e: e.tensor_reduce(s1, osb, AX.X, ALU.add), reads=["R0"], writes=["s1"])
                add("vector", lambda e: e.tensor_reduce(s2_, osq, AX.X, ALU.add), reads=["R1"], writes=["s2_"])
                add("vector", lambda e: e.tensor_scalar(mu, s1, 1.0 / 128, None, ALU.mult), reads=["s1"], writes=["mu"])
                add("vector", lambda e: e.tensor_tensor(var, mu, mu, ALU.mult), reads=["mu"], writes=["var"])
                add("vector", lambda e: e.scalar_tensor_tensor(var, s2_, 1.0 / 128, var, ALU.mult, ALU.subtract), reads=["s2_", "var"], writes=["var"])
                add("vector", lambda e: e.tensor_scalar(var, var, EPS, None, ALU.add), reads=["var"], writes=["var"])
                add("scalar", lambda e: e.activation(out=var, in_=var, func=AF.Sqrt), reads=["var"], writes=["var"])
                add("vector", lambda e: e.reciprocal(var, var), reads=["var"], writes=["var"])
                add("vector", lambda e: e.tensor_tensor(osb, osb, mu.unsqueeze(2).to_broadcast([128, 4, 128]), ALU.subtract), reads=["R0", "mu"], writes=["R0"])
                add("vector", lambda e: e.tensor_tensor(osb, osb, var.unsqueeze(2).to_broadcast([128, 4, 128]), ALU.mult), reads=["R0", "var"], writes=["R0"])
                osq2 = rt_[1]
                osb2 = rt_[0]
                add("scalar", lambda e, Gj=Gj: e.activation(out=osq2, in_=Gj, func=AF.Silu), reads=["qkvg3_%d" % j], writes=["R1"])
                add("vector", lambda e: e.tensor_tensor(osb2, osb2, osq2, ALU.mult), reads=["R0", "R1"], writes=["R0"])
                for h in range(4):
                    add("tensor", lambda e, h=h: e.transpose(psT[:, h, :], osb[:, h, :], ident), reads=["R0", "ident"], writes=["psT"])
                add("scalar", lambda e, j=j: e.activation(out=yretT[:, j, :].rearrange("p (h d) -> p h d", h=4).bitcast(F32R),
                                                        in_=psT[:, 0:4, :], func=AF.Copy), reads=["psT"], writes=["yretT%d" % j])

            ylr = ylruT.bitcast(F32R)
            yrr = yretT.bitcast(F32R)

            def stage_f(j, wbs):
                for half in range(2):
                    wb, wkey = wbs[half]
                    for kc in range(KD):
                        if kc < 4:
                            lhs = ylr[:, kc, j * 128:(j + 1) * 128]
                            rk = "ylruT"
                        else:
                            lhs = yrr[:, j, (kc - 4) * 128:(kc - 3) * 128]
                            rk = "yretT%d" % j
                        add("tensor", lambda e, lhs=lhs, wb=wb, kc=kc, half=half: e.matmul(
                            psP[:, half * 512:(half + 1) * 512], lhs, wb[:, kc, :], start=(kc == 0), stop=(kc == KD - 1)),
                            reads=[wkey, rk], writes=["psP%d" % half])
                add("vector", lambda e: e.tensor_tensor(xt[j], xt[j], psP, ALU.add), reads=["psP0", "psP1", "xt%d" % j], writes=["xt%d" % j])

            def stage_g(j):
                t = stile * 4 + j
                x1 = xt[j]
                xk = "xt%d" % j
                add("sync", lambda e, t=t, x1=x1: e.dma_start(out=X1_d[t * 128:(t + 1) * 128, :], in_=x1), reads=[xk], writes=["X1d"], dma=True)
                hf = hn
                rms_scale(x1, xk, gffn, "gffn", hf, "hn", "g")
                if debug == "hf":
                    add("sync", lambda e, t=t: e.dma_start(out=dbg_d[t * 128:(t + 1) * 128, :], in_=hf), reads=["hn"], dma=True)
                if debug == "x1":
                    add("sync", lambda e, t=t, x1=x1: e.dma_start(out=dbg_d[t * 128:(t + 1) * 128, :], in_=x1), reads=[xk], dma=True)
                add("scalar", lambda e: e.activation(out=hfb, in_=hf, func=AF.Copy), reads=["hn"], writes=["hfb"])
                for rnd in range(2):
                    for c in range(4):
                        add("tensor", lambda e, c=c, rnd=rnd: e.transpose(psT[:, 4 + c, :], hf[:, (rnd * 4 + c) * 128:(rnd * 4 + c + 1) * 128], ident),
                            reads=["hn", "ident"], writes=["psT2"])
                    add("scalar", lambda e, rnd=rnd: e.activation(out=hfT[:, rnd * 4:(rnd + 1) * 4, :], in_=psT[:, 4:8, :], func=AF.Copy),
                        reads=["psT2"], writes=["hfT"])
                psR = psO[:, 512:584]
                psC = psO[:, 640:704]
                for kc in range(KD):
                    add("tensor", lambda e, kc=kc: e.matmul(psR, hfT[:, kc, :], wrt[:, kc, :], start=(kc == 0), stop=(kc == KD - 1)),
                        reads=["hfT", "wrt"], writes=["psO1"])
                lg = rs[0][:, 0:72]
                goh, ein, mx8, oh1, oh2 = rs[1][:, 0:8], rs[2][:, 0:8], rs[3][:, 0:8], rs[4][:, 0:8], rs[5][:, 0:8]
                tmp64, A1, A2 = rs[6][:, 0:64], rs[7][:, 0:64], rs[1][:, 8:72]
                gmax, nmax, gsum, gw, dd = sm[2], sm[3], sm[4], sm[5], sm[6]
                gl_, el_ = lg[:, 0:8], lg[:, 8:72]
                add("vector", lambda e: e.tensor_tensor(lg, psR, brt, ALU.add), reads=["psO1", "brt"], writes=["lg"])
                add("vector", lambda e: e.tensor_reduce(gmax, gl_, AX.X, ALU.max), reads=["lg"], writes=["gmax"])
                add("vector", lambda e: e.tensor_scalar(goh, gl_, gmax, None, ALU.is_equal), reads=["lg", "gmax"], writes=["goh"])
                add("vector", lambda e: e.tensor_scalar(nmax, gmax, -1.0, None, ALU.mult), reads=["gmax"], writes=["nmax"])
                add("scalar", lambda e: e.activation(out=ein, in_=gl_, func=AF.Exp, bias=nmax, accum_out=gsum), reads=["lg", "nmax"], writes=["ein", "gsum"])
                add("vector", lambda e: e.reciprocal(gw, gsum), reads=["gsum"], writes=["gw"])
                add("vector", lambda e: e.tensor_tensor(tmp64.rearrange("p (g j) -> p g j", g=8), el_.rearrange("p (g j) -> p g j", g=8),
                                                        goh.unsqueeze(2).to_broadcast([128, 8, 8]), ALU.mult), reads=["lg", "goh"], writes=["tmp64"])
                add("vector", lambda e: e.tensor_reduce(ein, tmp64.rearrange("p (g j) -> p j g", g=8), AX.X, ALU.add), reads=["tmp64"], writes=["ein"])
                add("vector", lambda e: e.max(mx8, ein), reads=["ein"], writes=["mx8"])
                add("vector", lambda e: e.tensor_scalar(oh1, ein, mx8[:, 0:1], None, ALU.is_equal), reads=["ein", "mx8"], writes=["oh1"])
                add("vector", lambda e: e.tensor_scalar(oh2, ein, mx8[:, 1:2], None, ALU.is_equal), reads=["ein", "mx8"], writes=["oh2"])
                add("vector", lambda e: e.tensor_tensor(dd, mx8[:, 0:1], mx8[:, 1:2], ALU.subtract), reads=["mx8"], writes=["dd"])
                add("scalar", lambda e: e.activation(out=dd, in_=dd, func=AF.Sigmoid), reads=["dd"], writes=["dd"])
                add("vector", lambda e, t=t: e.tensor_tensor(wsel1[:, t:t + 1], dd, gw, ALU.mult), reads=["dd", "gw"], writes=["wsel1"])
                add("vector", lambda e, t=t: e.tensor_tensor(wsel2[:, t:t + 1], gw, wsel1[:, t:t + 1], ALU.subtract), reads=["gw", "wsel1"], writes=["wsel2"])
                gb = goh.unsqueeze(2).to_broadcast([128, 8, 8])
                add("vector", lambda e: e.tensor_tensor(A1.rearrange("p (g j) -> p g j", g=8), gb, oh1.unsqueeze(1).to_broadcast([128, 8, 8]), ALU.mult),
                    reads=["goh", "oh1"], writes=["A1"])
                add("vector", lambda e: e.tensor_tensor(A2.rearrange("p (g j) -> p g j", g=8), gb, oh2.unsqueeze(1).to_broadcast([128, 8, 8]), ALU.mult),
                    reads=["goh", "oh2"], writes=["A2"])
                add("vector", lambda e: e.tensor_tensor(tmp64, A1, A2, ALU.add), reads=["A1", "A2"], writes=["tmp64"])
                add("tensor", lambda e: e.matmul(psC, U_t, tmp64, start=True, stop=False), reads=["U", "tmp64"], writes=["psO1"])
                add("tensor", lambda e: e.matmul(psC, ones_t, Rcum, start=False, stop=True), reads=["ones", "Rcum"], writes=["psO1"])
                add("vector", lambda e: e.tensor_tensor(Rcum, Rcum, tmp64, ALU.add), reads=["Rcum", "tmp64"], writes=["Rcum"])
                pe_ = rs[6][:, 0:64]
                add("vector", lambda e: e.scalar_tensor_tensor(pe_, psC, float(CAP - 1), eCt, ALU.min, ALU.add), reads=["psO1", "eC", "tmp64"], writes=["tmp64"])
                add("vector", lambda e: e.tensor_tensor(A1, A1, pe_, ALU.mult), reads=["A1", "tmp64"], writes=["A1"])
                add("vector", lambda e: e.tensor_tensor(A2, A2, pe_, ALU.mult), reads=["A2", "tmp64"], writes=["A2"])
                d1f, d2f = sm[7], sm[8]
                add("vector", lambda e: e.tensor_reduce(d1f, A1, AX.X, ALU.add), reads=["A1"], writes=["d1f"])
                add("vector", lambda e: e.tensor_reduce(d2f, A2, AX.X, ALU.add), reads=["A2"], writes=["d2f"])
                add("vector", lambda e, t=t: e.tensor_copy(dest1i[:, t:t + 1], d1f), reads=["d1f"], writes=["dest1"])
                add("vector", lambda e, t=t: e.tensor_copy(dest2i[:, t:t + 1], d2f), reads=["d2f"], writes=["dest2"])
                if debug in ("x1", "hf"):
                    return
                add("gpsimd", lambda e, t=t: e.indirect_dma_start(out=XS_d, out_offset=bass.IndirectOffsetOnAxis(ap=dest1i[:, t:t + 1], axis=0),
                                                                  in_=hfb, in_offset=None), reads=["hfb", "dest1"], writes=["XSd"], dma=True)
                add("gpsimd", lambda e, t=t: e.indirect_dma_start(out=XS_d, out_offset=bass.IndirectOffsetOnAxis(ap=dest2i[:, t:t + 1], axis=0),
                                                                  in_=hfb, in_offset=None), reads=["hfb", "dest2"], writes=["XSd"], dma=True)


            stage_d(0)
            for gi in range(3):
                emit_merged(record(stage_c, gi), record(stage_d, gi + 1))
                if stile == 0:
                    emit_zero(gi)
            wbs = [load_group(w_out_d[:, half * 512:(half + 1) * 512]) for half in range(2)]

            def stage_fg(j):
                stage_f(j, wbs)
                stage_g(j)

            emit_merged(record(stage_c, 3), record(stage_e, 0))
            if stile == 0:
                emit_zero(3)
            add("vector", lambda e: e.tensor_copy(XL[:, :, 0:3], XL[:, :, 512:515]), reads=["XL"], writes=["XL"])
            for j in range(4):
                emit_merged(record(stage_fg, j), record(stage_e, j + 1) if j + 1 < 4 else [])

        if debug in ("x1", "hf"):
            S_.emit(final_wait_ops=[op for op in S_.all_ops if op.is_dma])
            return nc
        S_.barrier(lambda e: e.memset(bscr, 0.0))
        off[0] = PL_BASE
        W1b = [A(KD * 256, KD, 512, bf=True) for _ in range(2)]
        W3b = [A(KD * 256, KD, 512, bf=True) for _ in range(2)]
        W2b = [A(4 * 512, 4, 1024, bf=True) for _ in range(2)]
        xs = [A(D, 2, D, bf=True) for _ in range(2)]
        xsT = A(KD * 128, KD, 256, bf=True)
        gT = A(4 * 128, 4, 256, bf=True)
        silt = A(256)
        yb = [A(D) for _ in range(2)]
        psTb = [psT_t[:, 0:512].bitcast(BF16).rearrange("p (a b) -> p a b", a=8),
                psT_t[:, 512:1024].bitcast(BF16).rearrange("p (a b) -> p a b", a=8)]
        psTk = ["psT", "psT2"]
        hbanks = [(psA, "psA"), (psB, "psB"), (psO[:, 0:512], "psO0"), (psO[:, 512:1024], "psO1")]
        ybanks = [(psP[:, 0:512], "psP0"), (psP[:, 512:1024], "psP1")]
        silt2 = [silt, A(256)]

        def b_loads(ex):
            k = ex % 2
            add("gpsimd", lambda e: e.dma_start(out=W1b[k], in_=w1_d[ex].rearrange("(c p) n -> p c n", p=128)), writes=["W1b%d" % k], dma=True)
            add("gpsimd", lambda e: e.dma_start(out=W3b[k], in_=w3_d[ex].rearrange("(c p) n -> p c n", p=128)), writes=["W3b%d" % k], dma=True)
            add("gpsimd", lambda e: e.dma_start(out=W2b[k], in_=w2_d[ex].rearrange("(c p) n -> p c n", p=128)), writes=["W2b%d" % k], dma=True)
            add("sync", lambda e: e.dma_start(out=xs[k], in_=XS_d[ex * CAP:(ex + 1) * CAP, :].rearrange("(b p) d -> p b d", p=128)),
                reads=["XSd"], writes=["xs%d" % k], dma=True)

        ycnt = [0]

        def b_compute(ex):
            k = ex % 2
            for blk in range(2):
                for c in range(KD):
                    add("tensor", lambda e, c=c, blk=blk: e.transpose(psTb[blk][:, c, :], xs[k][:, blk, c * 128:(c + 1) * 128], identb),
                        reads=["xs%d" % k, "identb"], writes=[psTk[blk]])
                if blk == 0:
                    add("scalar", lambda e: e.activation(out=xsT[:, :, 0:128], in_=psTb[0], func=AF.Copy), reads=[psTk[0]], writes=["xsT"])
                else:
                    add("vector", lambda e: e.tensor_copy(xsT[:, :, 128:256], psTb[1]), reads=[psTk[1]], writes=["xsT"])
            for f in range(4):
                (h1, h1k), (h3, h3k) = hbanks[2 * (f % 2)], hbanks[2 * (f % 2) + 1]
                sl = silt2[f % 2]
                slk = "silt%d" % (f % 2)
                for kc in range(KD):
                    add("tensor", lambda e, f=f, kc=kc, h1=h1: e.matmul(h1[:, 0:256], W1b[k][:, kc, f * 128:(f + 1) * 128], xsT[:, kc, :],
                                                                     start=(kc == 0), stop=(kc == KD - 1)), reads=["W1b%d" % k, "xsT"], writes=[h1k])
                for kc in range(KD):
                    add("tensor", lambda e, f=f, kc=kc, h3=h3: e.matmul(h3[:, 0:256], W3b[k][:, kc, f * 128:(f + 1) * 128], xsT[:, kc, :],
                                                                     start=(kc == 0), stop=(kc == KD - 1)), reads=["W3b%d" % k, "xsT"], writes=[h3k])
                add("scalar", lambda e, h1=h1, sl=sl: e.activation(out=sl, in_=h1[:, 0:256], func=AF.Silu), reads=[h1k], writes=[slk])
                add("vector", lambda e, f=f, h3=h3, sl=sl: e.tensor_tensor(gT[:, f, :], sl, h3[:, 0:256], ALU.mult), reads=[slk, h3k], writes=["gT%d" % f])
            for blk in range(2):
                yk = ycnt[0] % 2
                ycnt[0] += 1
                for half in range(2):
                    yp, ypk = ybanks[half]
                    for f in range(4):
                        add("tensor", lambda e, yp=yp, half=half, f=f, blk=blk: e.matmul(
                            yp, gT[:, f, blk * 128:(blk + 1) * 128], W2b[k][:, f, half * 512:(half + 1) * 512],
                            start=(f == 0), stop=(f == 3)), reads=["gT%d" % f, "W2b%d" % k], writes=[ypk])
                    if half == 0:
                        add("scalar", lambda e, yk=yk, yp=yp: e.activation(out=yb[yk][:, 0:512], in_=yp, func=AF.Copy), reads=[ypk], writes=["yb%d" % yk])
                    else:
                        add("vector", lambda e, yk=yk, yp=yp: e.tensor_copy(yb[yk][:, 512:1024], yp), reads=[ypk], writes=["yb%d" % yk])
                r0 = ex * CAP + blk * 128
                add("sync", lambda e, yk=yk, r0=r0: e.dma_start(out=Y_d[r0:r0 + 128, :], in_=yb[yk]), reads=["yb%d" % yk], writes=["Yd"], dma=True)

        roff[0] = RO_BASE
        WG = A(KD * D, KD, D, ro=True)
        WP = A(2 * D, 2, D, ro=True)
        gple = A(D)
        gfin = A(D)
        bple = A(D)
        x1c = [A(D) for _ in range(2)]
        yac = [A(D) for _ in range(2)]
        ybc = [A(D) for _ in range(2)]
        pt = [A(256) for _ in range(2)]
        hp = A(D)
        hpT2 = [A(KD * 128, KD, 128, ro=True) for _ in range(2)]
        pT2 = [A(256, 2, 128, ro=True) for _ in range(2)]
        gate = A(D)
        outt = [A(D) for _ in range(2)]
        sm = [A(1) for _ in range(4)]
        add("gpsimd", lambda e: e.dma_start(out=WG.bitcast(F32R), in_=wg_d.rearrange("(c p) n -> p c n", p=128)), writes=["WG"], dma=True)
        add("gpsimd", lambda e: e.dma_start(out=WP.bitcast(F32R), in_=wp_d.rearrange("(c p) n -> p c n", p=128)), writes=["WP"], dma=True)
        for (dst, src, key) in [(gple, gple_d, "gple"), (gfin, gfin_d, "gfin"), (bple, bple_d, "bple")]:
            add("sync", lambda e, dst=dst, src=src: e.dma_start(out=dst, in_=src), writes=[key], dma=True)
        WGr = WG.bitcast(F32R)
        WPr = WP.bitcast(F32R)
        b_loads(0)
        for ex in range(NE):
            if ex + 1 < NE:
                b_loads(ex + 1)
            b_compute(ex)

        def c_loads(t):
            k = t % 2
            add("sync", lambda e: e.dma_start(out=x1c[k], in_=X1_d[t * 128:(t + 1) * 128, :]), reads=["X1d"], writes=["x1c%d" % k], dma=True)
            add("sync", lambda e: e.dma_start(out=pt[k], in_=p_d[t * 128:(t + 1) * 128, :]), writes=["pt%d" % k], dma=True)
            add("gpsimd", lambda e: e.indirect_dma_start(out=yac[k], out_offset=None, in_=Y_d,
                                                         in_offset=bass.IndirectOffsetOnAxis(ap=dest1i[:, t:t + 1], axis=0)),
                reads=["Yd", "dest1"], writes=["yac%d" % k], dma=True)
            add("gpsimd", lambda e: e.indirect_dma_start(out=ybc[k], out_offset=None, in_=Y_d,
                                                         in_offset=bass.IndirectOffsetOnAxis(ap=dest2i[:, t:t + 1], axis=0)),
                reads=["Yd", "dest2"], writes=["ybc%d" % k], dma=True)

        def c_stage1(t):
            k = t % 2
            x2 = x1c[k]
            xk = "x1c%d" % k
            hpT, pT_ = hpT2[k], pT2[k]
            add("vector", lambda e: e.scalar_tensor_tensor(x2, yac[k], wsel1[:, t:t + 1], x2, ALU.mult, ALU.add),
                reads=["yac%d" % k, "wsel1", xk], writes=[xk])
            add("vector", lambda e: e.scalar_tensor_tensor(x2, ybc[k], wsel2[:, t:t + 1], x2, ALU.mult, ALU.add),
                reads=["ybc%d" % k, "wsel2", xk], writes=[xk])
            rms_scale(x2, xk, gple, "gple", hp, "hp", "c", si=0)
            transpose8(hp, "hp", hpT, "hpT%d" % k)
            for c in range(2):
                add("tensor", lambda e, c=c: e.transpose(psA[:, c * 128:(c + 1) * 128], pt[k][:, c * 128:(c + 1) * 128], ident),
                    reads=["pt%d" % k, "ident"], writes=["psA"])
            add("vector", lambda e: e.tensor_copy(pT_.bitcast(F32R), psA[:, 0:256].rearrange("p (a b) -> p a b", a=2)), reads=["psA"], writes=["pT_%d" % k])

        def c_stage2(t):
            k = t % 2
            x2 = x1c[k]
            xk = "x1c%d" % k
            hpTr = hpT2[k].bitcast(F32R)
            pTr = pT2[k].bitcast(F32R)
            for half in range(2):
                for kc in range(KD):
                    add("tensor", lambda e, half=half, kc=kc: e.matmul(psO[:, half * 512:(half + 1) * 512], hpTr[:, kc, :], WGr[:, kc, half * 512:(half + 1) * 512],
                                                                      start=(kc == 0), stop=(kc == KD - 1)), reads=["hpT%d" % k, "WG"], writes=["psO0", "psO1"])
            for half in range(2):
                for kc in range(2):
                    add("tensor", lambda e, half=half, kc=kc: e.matmul(psP[:, half * 512:(half + 1) * 512], pTr[:, kc, :], WPr[:, kc, half * 512:(half + 1) * 512],
                                                                      start=(kc == 0), stop=(kc == 1)), reads=["pT_%d" % k, "WP"], writes=["psP0", "psP1"])
            add("vector", lambda e: e.tensor_tensor(gate, psO, bple, ALU.add), reads=["psO0", "psO1", "bple"], writes=["gate"])
            add("scalar", lambda e: e.activation(out=gate, in_=gate, func=AF.Sigmoid), reads=["gate"], writes=["gate"])
            add("vector", lambda e: e.tensor_tensor(gate, gate, psP, ALU.mult), reads=["gate", "psP0", "psP1"], writes=["gate"])
            add("vector", lambda e: e.tensor_tensor(x2, x2, gate, ALU.add), reads=["gate", xk], writes=[xk])
            rms_scale(x2, xk, gfin, "gfin", outt[k], "outt%d" % k, "f", si=2)
            add("sync", lambda e: e.dma_start(out=out_d[t * 128:(t + 1) * 128, :], in_=outt[k]), reads=["outt%d" % k], dma=True)

        c_loads(0)
        if NT > 1:
            c_loads(1)
        c_stage1(0)
        for t in range(NT):
            ch2 = record(c_stage2, t)
            ch1 = record(c_stage1, t + 1) if t + 1 < NT else []
            emit_merged(ch2, ch1)
            if t + 2 < NT:
                c_loads(t + 2)

        S_.emit(final_wait_ops=[op for op in S_.all_ops if op.is_dma])
    return nc


def core_inputs(b, S, consts, x, p, g_mix, w_in, conv_w, conv_b, lru_wa, lru_ba, lru_wx, lru_bx, lru_lambda, w_out,
                g_ffn, w_router_group, b_router_group, w_router_expert, b_router_expert, w1, w3, w2,
                g_ple, w_ple_gate, b_ple_gate, w_ple_proj, g_final):
    f = lambda a: np.ascontiguousarray(a, dtype=np.float32)
    m = dict(
        x=f(x[b]), p=f(p[0, b]), w_in=f(w_in[0]), w_out=f(w_out[0]), w1=f(w1[0]), w3=f(w3[0]), w2=f(w2[0]),
        w_ple_gate=f(w_ple_gate[0]), w_ple_proj=f(w_ple_proj[0]),
        w_rt=f(np.concatenate([w_router_group[0], w_router_expert[0]], axis=1)),
        b_rt=bc128(np.concatenate([b_router_group[0], b_router_expert[0]], axis=0)),
        g_mix=bc128(g_mix[0]), g_ffn=bc128(g_ffn[0]), g_ple=bc128(g_ple[0]), g_final=bc128(g_final), b_ple=bc128(b_ple_gate[0]),
        conv_w=f(np.transpose(np.asarray(conv_w[0]).reshape(4, 4, 128), (2, 1, 0))),
        conv_b=chan_major(conv_b[0]),
        lru_wa=block_diag(np.asarray(lru_wa[0])), lru_wx=block_diag(np.asarray(lru_wx[0])),
        lru_ba=chan_major(np.asarray(lru_ba[0]).reshape(-1)), lru_bx=chan_major(np.asarray(lru_bx[0]).reshape(-1)),
        lru_lam=chan_major(lru_lambda[0]),
    )
    m.update(consts)
    return m


_NC_CACHE = {}


def kernel(**inputs):
    inputs = {k: np.asarray(v) for k, v in inputs.items()}
    x = inputs["x"]
    B, S, _ = x.shape
    consts = make_consts(S)
    if S not in _NC_CACHE:
        _NC_CACHE[S] = build_nc(S)
    nc = _NC_CACHE[S]
    in_maps = [core_inputs(b, S, consts, **inputs) for b in range(B)]
    res = run_bass_kernel_spmd(nc, in_maps, core_ids=list(range(B)))
    return np.stack([np.asarray(r["out"]) for r in res.results], axis=0).astype(np.float32)
```

```python
import contextlib
import numpy as np
import concourse.bass as bass
import concourse.mybir as mybir
from concourse.bass_utils import run_bass_kernel_spmd

F32 = mybir.dt.float32
F32R = mybir.dt.float32r
I32 = mybir.dt.int32
BF16 = mybir.dt.bfloat16
AF = mybir.ActivationFunctionType
ALU = mybir.AluOpType
AX = mybir.AxisListType

D = 1024
KD = 8
NE = 64
DE = 512
CAP = 256
EPS = 1e-6
ENGINES = ("tensor", "vector", "scalar", "gpsimd", "sync")


class Op:
    __slots__ = ("eng", "fn", "is_dma", "needs_inc", "inc_index", "waits", "order_deps",
                 "dma_sem", "dma_val", "dma_prev", "idx", "cost", "xfer", "succ", "ndeps",
                 "finish", "ready")

    def __init__(self, eng, fn, is_dma):
        self.eng = eng
        self.fn = fn
        self.is_dma = is_dma
        self.needs_inc = False
        self.inc_index = None
        self.waits = []
        self.order_deps = []
        self.dma_sem = None
        self.dma_val = None
        self.dma_prev = 0
        self.succ = []
        self.ndeps = 0
        self.finish = 0.0
        self.ready = 0.0
        self.cost = 0.1
        self.xfer = 0.0


class _Mock:
    def __init__(self):
        self.calls = []

    def __getattr__(self, name):
        def f(*a, **k):
            self.calls.append((name, a, k))
            return self
        return f


def _fsize(ap):
    try:
        return int(ap.free_size())
    except Exception:
        return 256


def _estimate(op):
    m = _Mock()
    try:
        op.fn(m)
        name, a, k = m.calls[0]
    except Exception:
        return
    if op.is_dma:
        try:
            o = k.get("out", a[0] if a else None)
            i = k.get("in_", None)
            nb = min(o.nbytes(), i.nbytes()) if name == "indirect_dma_start" else max(o.nbytes(), i.nbytes())
        except Exception:
            nb = 65536
        op.xfer = nb / 300e3
        op.cost = 1.2 if op.eng == "gpsimd" else 0.08
        return
    if op.eng == "tensor":
        if name == "matmul":
            n = _fsize(a[2])
            mult = 4.0 if a[1].dtype == F32 else 1.0
            op.cost = max(n * mult, 64.0) / 2000.0
        else:
            op.cost = 0.13
    elif op.eng == "vector":
        o = k.get("out", a[0] if a else None)
        n = _fsize(o)
        if name == "tensor_reduce" or name == "max":
            n = _fsize(a[1])
        if name == "tensor_tensor_scan":
            n *= 2
        op.cost = (max(n, 64) + 60) / 960.0
    elif op.eng == "scalar":
        o = k.get("out", a[0] if a else None)
        op.cost = (max(_fsize(o), 64) + 250) / 1400.0
    else:
        op.cost = 0.2


class Sched:
    def __init__(self, nc, n_dma_sems=48):
        self.nc = nc
        self.last_writer = {}
        self.readers = {}
        self.n_dma_sems = n_dma_sems
        self.all_ops = []
        self.barrier_op = None
        self.last_of = {e: None for e in ENGINES}

    def add(self, eng, fn, reads=(), writes=(), dma=False):
        op = Op(eng, fn, dma)
        op.idx = len(self.all_ops)
        deps = []
        if self.barrier_op is not None:
            deps.append(self.barrier_op)
        for k in reads:
            w = self.last_writer.get(k)
            if w is not None:
                deps.append(w)
        for k in writes:
            w = self.last_writer.get(k)
            if w is not None:
                deps.append(w)
            deps.extend(self.readers.get(k, ()))
        seen = set()
        for d in deps:
            if id(d) in seen or d is op:
                continue
            seen.add(id(d))
            if (not d.is_dma) and d.eng == eng and eng == "tensor":
                op.order_deps.append(d)
            else:
                op.waits.append(d)
        for k in writes:
            self.last_writer[k] = op
            self.readers[k] = []
        for k in reads:
            if k not in writes:
                self.readers.setdefault(k, []).append(op)
        self.all_ops.append(op)
        self.last_of[eng] = op
        return op

    def barrier(self, fn):
        op = self.add("vector", fn, writes=["__barrier__"])
        have = set(id(w) for w in op.waits)
        for d in self.all_ops[:-1]:
            if id(d) not in have:
                op.waits.append(d)
        self.barrier_op = op
        self.last_writer = {}
        self.readers = {}
        return op

    def schedule(self):
        import heapq
        ops = self.all_ops
        for op in ops:
            _estimate(op)
            op.ndeps = 0
            op.succ = []
        for op in ops:
            for d in op.waits:
                d.succ.append(op)
                op.ndeps += 1
            for d in op.order_deps:
                d.succ.append(op)
                op.ndeps += 1
        future = {e: [] for e in ENGINES}
        avail = {e: [] for e in ENGINES}
        free_at = {e: 0.0 for e in ENGINES}
        dma_free = [0.0]
        order = {e: [] for e in ENGINES}
        for op in ops:
            if op.ndeps == 0:
                heapq.heappush(future[op.eng], (0.0, op.idx, op))
        remaining = len(ops)
        while remaining:
            best = None
            for e in ENGINES:
                fu, av = future[e], avail[e]
                while fu and fu[0][0] <= free_at[e]:
                    r, i, o = heapq.heappop(fu)
                    heapq.heappush(av, (i, o))
                if av:
                    cand = (free_at[e], av[0][0], e, True)
                elif fu:
                    cand = (fu[0][0], fu[0][1], e, False)
                else:
                    continue
                if best is None or cand < best:
                    best = cand
            assert best is not None, "scheduler stuck (dependency cycle?)"
            t, _, e, from_av = best
            if from_av:
                _, op = heapq.heappop(avail[e])
            else:
                _, _, op = heapq.heappop(future[e])
            start = max(t, free_at[e])
            free_at[e] = start + op.cost
            if op.is_dma:
                s0 = max(start + op.cost, dma_free[0])
                dma_free[0] = s0 + op.xfer
                op.finish = s0 + op.xfer + 2.0
            else:
                op.finish = start + op.cost
            order[e].append(op)
            remaining -= 1
            for sc in op.succ:
                lat = 0.0 if (sc.eng == op.eng and not op.is_dma) else 0.35
                r = op.finish + lat
                if r > sc.ready:
                    sc.ready = r
                sc.ndeps -= 1
                if sc.ndeps == 0:
                    heapq.heappush(future[sc.eng], (sc.ready, sc.idx, sc))
        self.est_time = max(op.finish for op in ops)
        return order

    def emit(self, final_wait_ops=()):
        nc = self.nc
        order = self.schedule()
        pos = {}
        for e in ENGINES:
            for i, op in enumerate(order[e]):
                pos[id(op)] = i
        for op in self.all_ops:
            if len(op.waits) > 256:
                keep = {}
                dm = []
                for d in op.waits:
                    if d.is_dma:
                        dm.append(d)
                    elif d.eng not in keep or pos[id(d)] > pos[id(keep[d.eng])]:
                        keep[d.eng] = d
                op.waits = dm + list(keep.values())
        for op in self.all_ops:
            for d in op.waits:
                if not d.is_dma:
                    d.needs_inc = True
        half = self.n_dma_sems // 2
        for e in ENGINES:
            c = 0
            rr = 0
            counts = [0] * self.n_dma_sems
            for op in order[e]:
                if op.is_dma:
                    base = half if e == "gpsimd" else 0
                    s = base + rr
                    rr = (rr + 1) % half
                    op.dma_sem = s
                    op.dma_prev = counts[s]
                    counts[s] += 16
                    op.dma_val = counts[s]
                elif op.needs_inc:
                    c += 1
                    op.inc_index = c
        with contextlib.ExitStack() as st:
            esem = {e: st.enter_context(nc.semaphore("es_" + e)) for e in ENGINES}
            dsem = [st.enter_context(nc.semaphore("ds_%d" % i)) for i in range(self.n_dma_sems)]
            block = st.enter_context(nc.Block())
            sched = self

            def run_engine(e, engobj):
                waited_e = {x: 0 for x in ENGINES}
                waited_d = [0] * sched.n_dma_sems
                for op in order[e]:
                    need_e = {}
                    need_d = {}
                    for d in op.waits:
                        if d.is_dma:
                            if d.dma_val > need_d.get(d.dma_sem, 0):
                                need_d[d.dma_sem] = d.dma_val
                        else:
                            if d.inc_index > need_e.get(d.eng, 0):
                                need_e[d.eng] = d.inc_index
                    if op.is_dma and op.dma_prev > need_d.get(op.dma_sem, 0):
                        need_d[op.dma_sem] = op.dma_prev
                    for pe, v in need_e.items():
                        if v > waited_e[pe]:
                            engobj.wait_ge(esem[pe], v)
                            waited_e[pe] = v
                    for s, v in need_d.items():
                        if v > waited_d[s]:
                            engobj.wait_ge(dsem[s], v)
                            waited_d[s] = v
                    ins = op.fn(engobj)
                    if op.is_dma:
                        ins.then_inc(dsem[op.dma_sem], 16)
                    elif op.needs_inc:
                        ins.then_inc(esem[e], 1)
                if e == "sync":
                    for d in final_wait_ops:
                        if d.dma_val > waited_d[d.dma_sem]:
                            engobj.wait_ge(dsem[d.dma_sem], d.dma_val)
                            waited_d[d.dma_sem] = d.dma_val

            @block.tensor
            def _(t):
                run_engine("tensor", t)

            @block.vector
            def _(v):
                run_engine("vector", v)

            @block.scalar
            def _(s):
                run_engine("scalar", s)

            @block.gpsimd
            def _(g):
                run_engine("gpsimd", g)

            @block.sync
            def _(sy):
                run_engine("sync", sy)


def make_consts(S):
    H, C, Dh = 4, 128, 128
    log_g = np.log(1.0 - 2.0 ** (-5.0 - np.arange(H, dtype=np.float64)))
    idx = np.arange(C, dtype=np.float64)
    diff = idx[:, None] - idx[None, :]
    decay = np.where(diff >= 0, np.exp(np.maximum(diff, 0.0)[None] * log_g[:, None, None]), 0.0)
    decayT = np.transpose(decay, (2, 0, 1))
    q_decay = np.exp((idx + 1.0)[None, :] * log_g[:, None])
    k_decay = np.exp((C - 1.0 - idx)[None, :] * log_g[:, None])
    chunk_decay = np.exp(C * log_g)
    QD = np.broadcast_to(q_decay[None, :, :], (128, H, C))
    KDc = np.transpose(k_decay, (1, 0))
    CDc = np.broadcast_to(chunk_decay[None, :], (128, H))
    half = Dh // 2
    inv = 10000.0 ** (-np.arange(half, dtype=np.float32).astype(np.float64) / half)
    pos = np.arange(S, dtype=np.float64)
    ang = (pos[:, None].astype(np.float32) * inv[None, :].astype(np.float32)).astype(np.float64)
    cos = np.cos(ang)
    sin = np.sin(ang)
    cosF = np.concatenate([cos, cos], axis=1)
    sinF = np.concatenate([-sin, sin], axis=1)
    sc = Dh ** -0.5
    rope = np.stack([cosF, sinF, cosF * sc, sinF * sc], axis=1)
    U = np.triu(np.ones((128, 128)), k=1)
    eC = np.broadcast_to((np.arange(NE) * CAP)[None, :], (128, NE))
    f = lambda a: np.ascontiguousarray(a, dtype=np.float32)
    return dict(c_ident=f(np.eye(128)), c_decayT=f(decayT), c_QD=f(QD), c_KD=f(KDc), c_CD=f(CDc),
                c_rope=f(rope), c_U=f(U), c_ones=f(np.ones((128, 128))), c_eC=f(eC))


def bc128(v):
    v = np.asarray(v, dtype=np.float32).reshape(1, -1)
    return np.ascontiguousarray(np.broadcast_to(v, (128, v.shape[1])))


def chan_major(v):
    return np.ascontiguousarray(np.asarray(v, np.float32).reshape(4, 128).T)


def block_diag(w):
    out = np.zeros((128, 4, 128), np.float32)
    for c in range(4):
        for hh in range(2):
            out[hh * 64:(hh + 1) * 64, c, hh * 64:(hh + 1) * 64] = w[2 * c + hh]
    return out


def build_nc(S, debug=None, stop_after=None):
    NT = S // 128
    NST = S // 512
    nc = bass.Bass("TRN2", target_bir_lowering=False)

    def din(name, shape, dt=F32):
        return nc.dram_tensor(name, list(shape), dt, kind="ExternalInput").ap()

    x_d = din("x", [S, D])
    p_d = din("p", [S, 256])
    w_in_d = din("w_in", [6, 128, KD * 512])
    w_out_d = din("w_out", [2, 128, KD * 512])
    w1_d = din("w1", [NE, 128, KD * DE])
    w3_d = din("w3", [NE, 128, KD * DE])
    w2_d = din("w2", [NE, 128, 4 * D])
    wg_d = din("w_ple_gate", [D, D])
    wp_d = din("w_ple_proj", [256, D])
    wrt_d = din("w_rt", [D, 72])
    brt_d = din("b_rt", [128, 72])
    gmix_d = din("g_mix", [128, D])
    gffn_d = din("g_ffn", [128, D])
    gple_d = din("g_ple", [128, D])
    gfin_d = din("g_final", [128, D])
    bple_d = din("b_ple", [128, D])
    cw_d = din("conv_w", [128, 4, 4])
    cb_d = din("conv_b", [128, 4])
    wa_d = din("lru_wa", [128, 4, 128])
    wx_d = din("lru_wx", [128, 4, 128])
    ba_d = din("lru_ba", [128, 4])
    bx_d = din("lru_bx", [128, 4])
    lam_d = din("lru_lam", [128, 4])
    ident_d = din("c_ident", [128, 128])
    decT_d = din("c_decayT", [128, 4, 128])
    QD_d = din("c_QD", [128, 4, 128])
    KD_d = din("c_KD", [128, 4])
    CD_d = din("c_CD", [128, 4])
    rope_d = din("c_rope", [S, 4, 128])
    U_d = din("c_U", [128, 128])
    ones_d = din("c_ones", [128, 128])
    eC_d = din("c_eC", [128, NE])
    out_d = nc.dram_tensor("out", [S, D], F32, kind="ExternalOutput").ap()
    X1_d = nc.dram_tensor("X1s", [S, D], F32, kind="Internal").ap()
    XS_d = nc.dram_tensor("XSs", [NE * CAP, D], BF16, kind="Internal").ap()
    Y_d = nc.dram_tensor("Ys", [NE * CAP, D], F32, kind="Internal").ap()
    dbg_d = None
    if debug is not None:
        dbg_d = nc.dram_tensor("dbg", [S, D], F32, kind="ExternalOutput").ap()

    with contextlib.ExitStack() as st:
        RO_BASE, RO_SIZE = 0, 16384
        ARENA = 52800 - RO_SIZE
        arena = st.enter_context(nc.sbuf_tensor("arena", [128, ARENA], F32))
        arena_ro = st.enter_context(nc.sbuf_tensor("arena_ro", [128, RO_SIZE], F32))
        PL_BASE = 400
        off = [0]
        roff = [RO_BASE]

        def A(n, *shape, ro=False, bf=False):
            if ro:
                o = roff[0]
                roff[0] += n
                assert roff[0] <= RO_BASE + RO_SIZE, ("ro overflow", roff[0])
            else:
                o = off[0]
                off[0] += n
                assert off[0] <= ARENA, ("arena overflow", o, off[0])
            ap = (arena_ro if ro else arena)[:, o:o + n]
            if bf:
                ap = ap.bitcast(BF16)
            if len(shape) == 2:
                ap = ap.rearrange("p (a b) -> p a b", a=shape[0])
            elif len(shape) == 3:
                ap = ap.rearrange("p (a b c) -> p a b c", a=shape[0], b=shape[1])
            return ap

        def psum(name, n):
            return st.enter_context(nc.psum_tensor(name, [128, n], F32))

        psT_t = psum("psT", 1024)
        psO_t = psum("psO", 1024)
        psP_t = psum("psP", 1024)
        psA_t = psum("psA", 512)
        psB_t = psum("psB", 512)
        psT = psT_t[:, :].rearrange("p (a b) -> p a b", a=8)
        psO = psO_t[:, :]
        psP = psP_t[:, :]
        psA = psA_t[:, :]
        psB = psB_t[:, :]

        S_ = Sched(nc)
        rec = [None]

        def add(eng, fn, reads=(), writes=(), dma=False):
            if rec[0] is not None:
                rec[0].append((eng, fn, tuple(reads), tuple(writes), dma))
                return None
            return S_.add(eng, fn, reads=reads, writes=writes, dma=dma)

        def record(f, *args):
            rec[0] = []
            f(*args)
            out = rec[0]
            rec[0] = None
            return out

        def emit_merged(*chains):
            chains = [c for c in chains if c]
            idx = [0] * len(chains)
            while True:
                live = [i for i in range(len(chains)) if idx[i] < len(chains[i])]
                if not live:
                    break
                i = min(live, key=lambda i: idx[i] / len(chains[i]))
                eng, fn, rd, wr, dma = chains[i][idx[i]]
                idx[i] += 1
                S_.add(eng, fn, reads=rd, writes=wr, dma=dma)

        ident = A(128)
        dest1 = A(NT)
        dest2 = A(NT)
        wsel1 = A(NT)
        wsel2 = A(NT)
        dest1i = dest1.bitcast(I32)
        dest2i = dest2.bitcast(I32)
        bscr = A(1)
        identb = A(64, bf=True)
        persist_end = off[0]
        assert persist_end <= PL_BASE
        off[0] = PL_BASE

        add("sync", lambda e: e.dma_start(out=ident, in_=ident_d), writes=["ident"], dma=True)
        add("vector", lambda e: e.tensor_copy(identb, ident), reads=["ident"], writes=["identb"])

        U_t = A(128)
        ones_t = A(128)
        decT = A(512, 4, 128)
        QDt = A(512, 4, 128)
        KDt = A(4)
        CDt = A(4)
        eCt = A(NE)
        gmix = A(D)
        gffn = A(D)
        cw = A(16, 4, 4)
        cb = A(4)
        WA = A(512, 4, 128)
        WX = A(512, 4, 128)
        ba = A(4)
        bx = A(4)
        lam = A(4)
        cneg = A(4)
        c2 = A(4)
        wrt = A(KD * 72, KD, 72)
        brt = A(72)
        hstate = A(4)
        Rcum = A(NE)
        state = A(512, 4, 128)
        tmp4 = [A(4) for _ in range(6)]

        for (dst, src, key) in [(U_t, U_d, "U"), (ones_t, ones_d, "ones"), (decT, decT_d, "decT"),
                                (QDt, QD_d, "QD"), (KDt, KD_d, "KD"), (CDt, CD_d, "CD"), (eCt, eC_d, "eC"),
                                (gmix, gmix_d, "gmix"), (gffn, gffn_d, "gffn"), (cw, cw_d, "cw"), (cb, cb_d, "cb"),
                                (WA, wa_d, "WA"), (WX, wx_d, "WX"), (ba, ba_d, "ba"), (bx, bx_d, "bx"),
                                (lam, lam_d, "lam"), (brt, brt_d, "brt")]:
            add("sync", lambda e, dst=dst, src=src: e.dma_start(out=dst, in_=src), writes=[key], dma=True)
        add("sync", lambda e: e.dma_start(out=wrt, in_=wrt_d.rearrange("(c p) n -> p c n", p=128)), writes=["wrt"], dma=True)
        add("vector", lambda e: e.memset(hstate, 0.0), writes=["hstate"])
        add("vector", lambda e: e.memset(Rcum, 0.0), writes=["Rcum"])
        add("vector", lambda e: e.memset(state, 0.0), writes=["state"])

        ya, za, sa, s2, acc, t0 = tmp4
        add("vector", lambda e: e.tensor_scalar(ya, lam, -1.0, None, ALU.mult), reads=["lam"], writes=["ya"])
        add("vector", lambda e: e.tensor_tensor(za, ya, lam, ALU.min), reads=["ya", "lam"], writes=["za"])
        add("scalar", lambda e: e.activation(out=za, in_=za, func=AF.Exp), reads=["za"], writes=["za"])
        add("vector", lambda e: e.tensor_scalar(sa, za, 2.0, None, ALU.add), reads=["za"], writes=["sa"])
        add("vector", lambda e: e.reciprocal(sa, sa), reads=["sa"], writes=["sa"])
        add("vector", lambda e: e.tensor_tensor(sa, sa, za, ALU.mult), reads=["sa", "za"], writes=["sa"])
        add("vector", lambda e: e.tensor_tensor(s2, sa, sa, ALU.mult), reads=["sa"], writes=["s2"])
        add("vector", lambda e: e.memset(acc, 1.0 / 15.0), writes=["acc"])
        for k in (13, 11, 9, 7, 5, 3, 1):
            add("vector", lambda e: e.tensor_tensor(acc, acc, s2, ALU.mult), reads=["acc", "s2"], writes=["acc"])
            add("vector", lambda e, k=k: e.tensor_scalar(acc, acc, 1.0 / k, None, ALU.add), reads=["acc"], writes=["acc"])
        add("vector", lambda e: e.tensor_tensor(acc, acc, sa, ALU.mult), reads=["acc", "sa"], writes=["acc"])
        add("vector", lambda e: e.tensor_scalar(t0, ya, 0.0, None, ALU.max), reads=["ya"], writes=["t0"])
        add("vector", lambda e: e.scalar_tensor_tensor(t0, acc, 2.0, t0, ALU.mult, ALU.add), reads=["acc", "t0"], writes=["t0"])
        add("vector", lambda e: e.tensor_scalar(cneg, t0, -8.0, None, ALU.mult), reads=["t0"], writes=["cneg"])
        add("vector", lambda e: e.tensor_scalar(c2, t0, -16.0, None, ALU.mult), reads=["t0"], writes=["c2"])

        setup_end = off[0]
        wbuf = [A(KD * 512, KD, 512, ro=True) for _ in range(2)]
        xt = [A(D) for _ in range(4)]
        hn = A(D)
        hT = A(KD * 512, KD, 512, ro=True)
        XL = A(4 * 515, 4, 515)
        GL = A(4 * 512, 4, 512)
        ylruT = A(4 * 512, 4, 512, ro=True)
        lt = [A(512) for _ in range(6)]
        QKVG = [A(4 * 512, 4, 512) for _ in range(4)]
        yretT = A(4 * 512, 4, 512, ro=True)
        rt_ = [A(512) for _ in range(7)]
        ropet = A(512, 4, 128)
        hfT = A(KD * 128, KD, 128)
        hfb = A(512, bf=True)
        rs = [A(80) for _ in range(8)]
        sm = [A(1) for _ in range(16)]
        rse = [A(4) for _ in range(4)]
        phaseA_end = off[0]

        add("vector", lambda e: e.memset(XL[:, :, 0:3], 0.0), writes=["XL"])
        zt = A(1024, bf=True)

        def rms_scale(xin, xkey, gbc, gkey, outap, outkey, tag, si=0):
            ss, rstd = sm[si], sm[si + 1]
            ssk, rk = "ss%d" % si, "rstd%d" % si
            add("scalar", lambda e: e.activation(out=outap, in_=xin, func=AF.Square, accum_out=ss),
                reads=[xkey], writes=[outkey, ssk])
            add("vector", lambda e: e.tensor_scalar(ss, ss, 1.0 / D, EPS, ALU.mult, ALU.add), reads=[ssk], writes=[ssk])
            add("scalar", lambda e: e.activation(out=ss, in_=ss, func=AF.Sqrt), reads=[ssk], writes=[ssk])
            add("vector", lambda e: e.reciprocal(rstd, ss), reads=[ssk], writes=[rk])
            add("vector", lambda e: e.scalar_tensor_tensor(outap, xin, rstd, gbc, ALU.mult, ALU.mult),
                reads=[xkey, rk, gkey], writes=[outkey])

        def transpose8(src, srckey, dst3, dstkey, n=8, rounded=True, eng="scalar"):
            pk = ["psT", "psT2"] if n > 4 else ["psT"]
            for c in range(n):
                add("tensor", lambda e, c=c: e.transpose(psT[:, c, :], src[:, c * 128:(c + 1) * 128], ident),
                    reads=[srckey, "ident"], writes=pk)
            o = dst3.bitcast(F32R) if rounded else dst3
            if eng == "scalar":
                add("scalar", lambda e: e.activation(out=o, in_=psT[:, 0:n, :], func=AF.Copy), reads=pk, writes=[dstkey])
            else:
                add("vector", lambda e: e.tensor_copy(o, psT[:, 0:n, :]), reads=pk, writes=[dstkey])

        wcount = [0]

        def load_group(src_ap, nk=KD):
            k = wcount[0] % 2
            wcount[0] += 1
            dst = wbuf[k].bitcast(F32R).rearrange("p c n -> p (c n)")
            add("gpsimd", lambda e: e.dma_start(out=dst, in_=src_ap, max_dma_last_dim=8192),
                writes=["wbuf%d" % k], dma=True)
            return wbuf[k].bitcast(F32R), "wbuf%d" % k

        pp = [0]

        def next_ps():
            pp[0] ^= 1
            return (psA, "psA") if pp[0] else (psB, "psB")

        for stile in range(NST):
            for j in range(4):
                t = stile * 4 + j
                add("sync", lambda e, j=j, t=t: e.dma_start(out=xt[j], in_=x_d[t * 128:(t + 1) * 128, :]),
                    writes=["xt%d" % j], dma=True)
                rms_scale(xt[j], "xt%d" % j, gmix, "gmix", hn, "hn", "a")
                transpose8(hn, "hn", hT[:, :, j * 128:(j + 1) * 128], "hT")
            XS_v = XS_d.rearrange("(p r) d -> p (r d)", p=128)
            nz = (NE * CAP // 128) * D // 2048

            def emit_zero(part):
                if part == 0:
                    add("vector", lambda e: e.memset(zt, 0.0), writes=["zt"])
                for kz in range(part * nz // 4, (part + 1) * nz // 4):
                    add("sync", lambda e, kz=kz: e.dma_start(out=XS_v[:, kz * 2048:(kz + 1) * 2048], in_=zt),
                        reads=["zt", "ylruT"], writes=["XSz%d" % kz], dma=True)
                if part == 3:
                    add("vector", lambda e: e.memset(bscr, 0.0), reads=["XSz%d" % kz for kz in range(nz)], writes=["XSd"])
            hTr = hT.bitcast(F32R)
            for g, (dst, dkey, o0) in enumerate([(XL, "XL", 3), (GL, "GL", 0)]):
                wb, wkey = load_group(w_in_d[g])
                for c in range(4):
                    ps, pkey = next_ps()
                    for kc in range(KD):
                        add("tensor", lambda e, ps=ps, wb=wb, kc=kc, c=c: e.matmul(
                            ps, wb[:, kc, c * 128:(c + 1) * 128], hTr[:, kc, :], start=(kc == 0), stop=(kc == KD - 1)),
                            reads=[wkey, "hT"], writes=[pkey])
                    add("scalar", lambda e, ps=ps, dst=dst, c=c, o0=o0: e.activation(out=dst[:, c, o0:o0 + 512], in_=ps, func=AF.Copy),
                        reads=[pkey], writes=[dkey])
            def stage_c(c):
                xc, r_, i_, a_, u_, g_ = lt
                add("vector", lambda e, c=c: e.tensor_scalar(xc, XL[:, c, 3:515], cw[:, c, 3:4], cb[:, c:c + 1], ALU.mult, ALU.add),
                    reads=["XL", "cw", "cb"], writes=["xc"])
                for k in (2, 1, 0):
                    add("vector", lambda e, c=c, k=k: e.scalar_tensor_tensor(xc, XL[:, c, k:k + 512], cw[:, c, k:k + 1], xc, ALU.mult, ALU.add),
                        reads=["XL", "cw", "xc"], writes=["xc"])
                add("tensor", lambda e, c=c: e.matmul(psP[:, 0:512], WA[:, c, :], xc, start=True, stop=True), reads=["WA", "xc"], writes=["psP0"])
                add("tensor", lambda e, c=c: e.matmul(psP[:, 512:1024], WX[:, c, :], xc, start=True, stop=True), reads=["WX", "xc"], writes=["psP1"])
                add("scalar", lambda e, c=c: e.activation(out=r_, in_=psP[:, 0:512], func=AF.Sigmoid, bias=ba[:, c:c + 1]), reads=["psP0", "ba"], writes=["r_"])
                add("scalar", lambda e, c=c: e.activation(out=i_, in_=psP[:, 512:1024], func=AF.Sigmoid, bias=bx[:, c:c + 1]), reads=["psP1", "bx"], writes=["i_"])
                add("scalar", lambda e, c=c: e.activation(out=a_, in_=r_, func=AF.Exp, scale=cneg[:, c:c + 1]), reads=["r_", "cneg"], writes=["a_"])
                add("scalar", lambda e, c=c: e.activation(out=u_, in_=r_, func=AF.Exp, scale=c2[:, c:c + 1]), reads=["r_", "c2"], writes=["u_"])
                add("vector", lambda e: e.tensor_scalar(u_, u_, -1.0, 1.0, ALU.mult, ALU.add), reads=["u_"], writes=["u_"])
                add("scalar", lambda e: e.activation(out=u_, in_=u_, func=AF.Sqrt), reads=["u_"], writes=["u_"])
                add("vector", lambda e: e.tensor_tensor(i_, i_, xc, ALU.mult), reads=["i_", "xc"], writes=["i_"])
                add("vector", lambda e: e.tensor_tensor(u_, u_, i_, ALU.mult), reads=["u_", "i_"], writes=["u_"])
                add("vector", lambda e, c=c: e.tensor_tensor_scan(r_, a_, u_, hstate[:, c:c + 1], ALU.mult, ALU.add),
                    reads=["a_", "u_", "hstate"], writes=["r_"])
                add("vector", lambda e, c=c: e.tensor_copy(hstate[:, c:c + 1], r_[:, 511:512]), reads=["r_"], writes=["hstate"])
                add("vector", lambda e, c=c: e.tensor_tensor(g_, GL[:, c, :], GL[:, c, :], ALU.mult), reads=["GL"], writes=["g_"])
                add("vector", lambda e: e.tensor_scalar(g_, g_, 0.044715, 1.0, ALU.mult, ALU.add), reads=["g_"], writes=["g_"])
                add("vector", lambda e, c=c: e.tensor_tensor(g_, g_, GL[:, c, :], ALU.mult), reads=["g_", "GL"], writes=["g_"])
                add("scalar", lambda e: e.activation(out=g_, in_=g_, func=AF.Sigmoid, scale=1.5957691216057308), reads=["g_"], writes=["g_"])
                add("vector", lambda e, c=c: e.tensor_tensor(g_, g_, GL[:, c, :], ALU.mult), reads=["g_", "GL"], writes=["g_"])
                add("vector", lambda e, c=c: e.tensor_tensor(ylruT[:, c, :].bitcast(F32R), r_, g_, ALU.mult), reads=["r_", "g_"], writes=["ylruT"])

            def stage_d(gi):
                wb, wkey = load_group(w_in_d[2 + gi])
                for j in range(4):
                    ps, pkey = next_ps()
                    for kc in range(KD):
                        add("tensor", lambda e, ps=ps, wb=wb, kc=kc, j=j: e.matmul(
                            ps, hTr[:, kc, j * 128:(j + 1) * 128], wb[:, kc, :], start=(kc == 0), stop=(kc == KD - 1)),
                            reads=[wkey, "hT"], writes=[pkey])
                    add("scalar", lambda e, ps=ps, gi=gi, j=j: e.activation(out=QKVG[gi][:, j, :], in_=ps, func=AF.Copy),
                        reads=[pkey], writes=["qkvg%d_%d" % (gi, j)])

            def stage_e(j):
                t = stile * 4 + j
                Qj = QKVG[0][:, j, :].rearrange("p (h d) -> p h d", h=4)
                Kj = QKVG[1][:, j, :].rearrange("p (h d) -> p h d", h=4)
                Vj = QKVG[2][:, j, :].rearrange("p (h d) -> p h d", h=4)
                Gj = QKVG[3][:, j, :]
                qr, kr, ta, qT, qdT, kT, kd = [r.rearrange("p (h d) -> p h d", h=4) for r in rt_]
                osb, osq, sT = qr, kr, ta
                add("sync", lambda e, t=t: e.dma_start(out=ropet, in_=rope_d[t * 128:(t + 1) * 128, :, :]), writes=["rope"], dma=True)

                def rotary(src, skey, dst, dkey, ci):
                    cosb = ropet[:, ci, :].unsqueeze(1).to_broadcast([128, 4, 128])
                    add("vector", lambda e: e.tensor_tensor(dst, src, cosb, ALU.mult), reads=[skey, "rope"], writes=[dkey])
                    s_lo = ropet[:, ci + 1, 0:64].unsqueeze(1).to_broadcast([128, 4, 64])
                    s_hi = ropet[:, ci + 1, 64:128].unsqueeze(1).to_broadcast([128, 4, 64])
                    add("vector", lambda e: e.tensor_tensor(ta[:, :, 0:64], src[:, :, 64:128], s_lo, ALU.mult), reads=[skey, "rope"], writes=["R2"])
                    add("vector", lambda e: e.tensor_tensor(ta[:, :, 64:128], src[:, :, 0:64], s_hi, ALU.mult), reads=[skey, "rope"], writes=["R2"])
                    add("vector", lambda e: e.tensor_tensor(dst, dst, ta, ALU.add), reads=[dkey, "R2"], writes=[dkey])

                rotary(Qj, "qkvg0_%d" % j, qr, "R0", 0)
                rotary(Kj, "qkvg1_%d" % j, kr, "R1", 2)
                for h in range(4):
                    add("tensor", lambda e, h=h: e.transpose(psT[:, h, :], qr[:, h, :], ident), reads=["R0", "ident"], writes=["psT"])
                add("scalar", lambda e: e.activation(out=qT, in_=psT[:, 0:4, :], func=AF.Copy), reads=["psT"], writes=["R3"])
                add("vector", lambda e: e.tensor_tensor(qdT, qT, QDt, ALU.mult), reads=["R3", "QD"], writes=["R4"])
                for h in range(4):
                    add("tensor", lambda e, h=h: e.transpose(psT[:, h, :], kr[:, h, :], ident), reads=["R1", "ident"], writes=["psT"])
                add("scalar", lambda e: e.activation(out=kT, in_=psT[:, 0:4, :], func=AF.Copy), reads=["psT"], writes=["R5"])
                add("vector", lambda e: e.tensor_tensor(kd, kr, KDt.unsqueeze(2).to_broadcast([128, 4, 128]), ALU.mult), reads=["R1", "KD"], writes=["R6"])
                psA3 = psA.rearrange("p (h d) -> p h d", h=4)
                psB3 = psB.rearrange("p (h d) -> p h d", h=4)
                psO3 = psO[:, 0:512].rearrange("p (h d) -> p h d", h=4)
                for h in range(4):
                    add("tensor", lambda e, h=h: e.matmul(psA3[:, h, :], kT[:, h, :], qT[:, h, :], start=True, stop=True),
                        reads=["R5", "R3"], writes=["psA"])
                add("vector", lambda e: e.tensor_tensor(sT, psA3, decT, ALU.mult), reads=["psA", "decT"], writes=["R2"])
                for h in range(4):
                    add("tensor", lambda e, h=h, Vj=Vj: e.matmul(psB3[:, h, :], sT[:, h, :], Vj[:, h, :], start=True, stop=False),
                        reads=["R2", "qkvg2_%d" % j], writes=["psB"])
                    add("tensor", lambda e, h=h: e.matmul(psB3[:, h, :], qdT[:, h, :], state[:, h, :], start=False, stop=True),
                        reads=["R4", "state"], writes=["psB"])
                for h in range(4):
                    add("tensor", lambda e, h=h, Vj=Vj: e.matmul(psO3[:, h, :], kd[:, h, :], Vj[:, h, :], start=True, stop=True),
                        reads=["R6", "qkvg2_%d" % j], writes=["psO"])
                add("vector", lambda e: e.tensor_tensor(state, state, CDt.unsqueeze(2).to_broadcast([128, 4, 128]), ALU.mult),
                    reads=["state", "CD"], writes=["state"])
                add("vector", lambda e: e.tensor_tensor(state, state, psO3, ALU.add), reads=["state", "psO"], writes=["state"])
                s1, s2_, mu, var = rse
                add("scalar", lambda e: e.activation(out=osb, in_=psB3, func=AF.Copy), reads=["psB"], writes=["R0"])
                add("scalar", lambda e: e.activation(out=osq, in_=psB3, func=AF.Square), reads=["psB"], writes=["R1"])
                add("vector", lambda e: e.tensor_reduce(s1, osb, AX.X, ALU.add), reads=["R0"], writes=["s1"])
                add("vector", lambda e: e.tensor_reduce(s2_, osq, AX.X, ALU.add), reads=["R1"], writes=["s2_"])
                add("vector", lambda e: e.tensor_scalar(mu, s1, 1.0 / 128, None, ALU.mult), reads=["s1"], writes=["mu"])
                add("vector", lambda e: e.tensor_tensor(var, mu, mu, ALU.mult), reads=["mu"], writes=["var"])
                add("vector", lambda e: e.scalar_tensor_tensor(var, s2_, 1.0 / 128, var, ALU.mult, ALU.subtract), reads=["s2_", "var"], writes=["var"])
                add("vector", lambda e: e.tensor_scalar(var, var, EPS, None, ALU.add), reads=["var"], writes=["var"])
                add("scalar", lambda e: e.activation(out=var, in_=var, func=AF.Sqrt), reads=["var"], writes=["var"])
                add("vector", lambda e: e.reciprocal(var, var), reads=["var"], writes=["var"])
                add("vector", lambda e: e.tensor_tensor(osb, osb, mu.unsqueeze(2).to_broadcast([128, 4, 128]), ALU.subtract), reads=["R0", "mu"], writes=["R0"])
                add("vector", lambda e: e.tensor_tensor(osb, osb, var.unsqueeze(2).to_broadcast([128, 4, 128]), ALU.mult), reads=["R0", "var"], writes=["R0"])
                osq2 = rt_[1]
                osb2 = rt_[0]
                add("scalar", lambda e, Gj=Gj: e.activation(out=osq2, in_=Gj, func=AF.Silu), reads=["qkvg3_%d" % j], writes=["R1"])
                add("vector", lambda e: e.tensor_tensor(osb2, osb2, osq2, ALU.mult), reads=["R0", "R1"], writes=["R0"])
                for h in range(4):
                    add("tensor", lambda e, h=h: e.transpose(psT[:, h, :], osb[:, h, :], ident), reads=["R0", "ident"], writes=["psT"])
                add("scalar", lambda e, j=j: e.activation(out=yretT[:, j, :].rearrange("p (h d) -> p h d", h=4).bitcast(F32R),
                                                        in_=psT[:, 0:4, :], func=AF.Copy), reads=["psT"], writes=["yretT%d" % j])

            ylr = ylruT.bitcast(F32R)
            yrr = yretT.bitcast(F32R)

            def stage_f(j, wbs):
                for half in range(2):
                    wb, wkey = wbs[half]
                    for kc in range(KD):
                        if kc < 4:
                            lhs = ylr[:, kc, j * 128:(j + 1) * 128]
                            rk = "ylruT"
                        else:
                            lhs = yrr[:, j, (kc - 4) * 128:(kc - 3) * 128]
                            rk = "yretT%d" % j
                        add("tensor", lambda e, lhs=lhs, wb=wb, kc=kc, half=half: e.matmul(
                            psP[:, half * 512:(half + 1) * 512], lhs, wb[:, kc, :], start=(kc == 0), stop=(kc == KD - 1)),
                            reads=[wkey, rk], writes=["psP%d" % half])
                add("vector", lambda e: e.tensor_tensor(xt[j], xt[j], psP, ALU.add), reads=["psP0", "psP1", "xt%d" % j], writes=["xt%d" % j])

            def stage_g(j):
                t = stile * 4 + j
                x1 = xt[j]
                xk = "xt%d" % j
                add("sync", lambda e, t=t, x1=x1: e.dma_start(out=X1_d[t * 128:(t + 1) * 128, :], in_=x1), reads=[xk], writes=["X1d"], dma=True)
                hf = hn
                rms_scale(x1, xk, gffn, "gffn", hf, "hn", "g")
                if debug == "hf":
                    add("sync", lambda e, t=t: e.dma_start(out=dbg_d[t * 128:(t + 1) * 128, :], in_=hf), reads=["hn"], dma=True)
                if debug == "x1":
                    add("sync", lambda e, t=t, x1=x1: e.dma_start(out=dbg_d[t * 128:(t + 1) * 128, :], in_=x1), reads=[xk], dma=True)
                add("scalar", lambda e: e.activation(out=hfb, in_=hf, func=AF.Copy), reads=["hn"], writes=["hfb"])
                for rnd in range(2):
                    for c in range(4):
                        add("tensor", lambda e, c=c, rnd=rnd: e.transpose(psT[:, 4 + c, :], hf[:, (rnd * 4 + c) * 128:(rnd * 4 + c + 1) * 128], ident),
                            reads=["hn", "ident"], writes=["psT2"])
                    add("scalar", lambda e, rnd=rnd: e.activation(out=hfT[:, rnd * 4:(rnd + 1) * 4, :], in_=psT[:, 4:8, :], func=AF.Copy),
                        reads=["psT2"], writes=["hfT"])
                psR = psO[:, 512:584]
                psC = psO[:, 640:704]
                for kc in range(KD):
                    add("tensor", lambda e, kc=kc: e.matmul(psR, hfT[:, kc, :], wrt[:, kc, :], start=(kc == 0), stop=(kc == KD - 1)),
                        reads=["hfT", "wrt"], writes=["psO1"])
                lg = rs[0][:, 0:72]
                goh, ein, mx8, oh1, oh2 = rs[1][:, 0:8], rs[2][:, 0:8], rs[3][:, 0:8], rs[4][:, 0:8], rs[5][:, 0:8]
                tmp64, A1, A2 = rs[6][:, 0:64], rs[7][:, 0:64], rs[1][:, 8:72]
                gmax, nmax, gsum, gw, dd = sm[2], sm[3], sm[4], sm[5], sm[6]
                gl_, el_ = lg[:, 0:8], lg[:, 8:72]
                add("vector", lambda e: e.tensor_tensor(lg, psR, brt, ALU.add), reads=["psO1", "brt"], writes=["lg"])
                add("vector", lambda e: e.tensor_reduce(gmax, gl_, AX.X, ALU.max), reads=["lg"], writes=["gmax"])
                add("vector", lambda e: e.tensor_scalar(goh, gl_, gmax, None, ALU.is_equal), reads=["lg", "gmax"], writes=["goh"])
                add("vector", lambda e: e.tensor_scalar(nmax, gmax, -1.0, None, ALU.mult), reads=["gmax"], writes=["nmax"])
                add("scalar", lambda e: e.activation(out=ein, in_=gl_, func=AF.Exp, bias=nmax, accum_out=gsum), reads=["lg", "nmax"], writes=["ein", "gsum"])
                add("vector", lambda e: e.reciprocal(gw, gsum), reads=["gsum"], writes=["gw"])
                add("vector", lambda e: e.tensor_tensor(tmp64.rearrange("p (g j) -> p g j", g=8), el_.rearrange("p (g j) -> p g j", g=8),
                                                        goh.unsqueeze(2).to_broadcast([128, 8, 8]), ALU.mult), reads=["lg", "goh"], writes=["tmp64"])
                add("vector", lambda e: e.tensor_reduce(ein, tmp64.rearrange("p (g j) -> p j g", g=8), AX.X, ALU.add), reads=["tmp64"], writes=["ein"])
                add("vector", lambda e: e.max(mx8, ein), reads=["ein"], writes=["mx8"])
                add("vector", lambda e: e.tensor_scalar(oh1, ein, mx8[:, 0:1], None, ALU.is_equal), reads=["ein", "mx8"], writes=["oh1"])
                add("vector", lambda e: e.tensor_scalar(oh2, ein, mx8[:, 1:2], None, ALU.is_equal), reads=["ein", "mx8"], writes=["oh2"])
                add("vector", lambda e: e.tensor_tensor(dd, mx8[:, 0:1], mx8[:, 1:2], ALU.subtract), reads=["mx8"], writes=["dd"])
                add("scalar", lambda e: e.activation(out=dd, in_=dd, func=AF.Sigmoid), reads=["dd"], writes=["dd"])
                add("vector", lambda e, t=t: e.tensor_tensor(wsel1[:, t:t + 1], dd, gw, ALU.mult), reads=["dd", "gw"], writes=["wsel1"])
                add("vector", lambda e, t=t: e.tensor_tensor(wsel2[:, t:t + 1], gw, wsel1[:, t:t + 1], ALU.subtract), reads=["gw", "wsel1"], writes=["wsel2"])
                gb = goh.unsqueeze(2).to_broadcast([128, 8, 8])
                add("vector", lambda e: e.tensor_tensor(A1.rearrange("p (g j) -> p g j", g=8), gb, oh1.unsqueeze(1).to_broadcast([128, 8, 8]), ALU.mult),
                    reads=["goh", "oh1"], writes=["A1"])
                add("vector", lambda e: e.tensor_tensor(A2.rearrange("p (g j) -> p g j", g=8), gb, oh2.unsqueeze(1).to_broadcast([128, 8, 8]), ALU.mult),
                    reads=["goh", "oh2"], writes=["A2"])
                add("vector", lambda e: e.tensor_tensor(tmp64, A1, A2, ALU.add), reads=["A1", "A2"], writes=["tmp64"])
                add("tensor", lambda e: e.matmul(psC, U_t, tmp64, start=True, stop=False), reads=["U", "tmp64"], writes=["psO1"])
                add("tensor", lambda e: e.matmul(psC, ones_t, Rcum, start=False, stop=True), reads=["ones", "Rcum"], writes=["psO1"])
                add("vector", lambda e: e.tensor_tensor(Rcum, Rcum, tmp64, ALU.add), reads=["Rcum", "tmp64"], writes=["Rcum"])
                pe_ = rs[6][:, 0:64]
                add("vector", lambda e: e.scalar_tensor_tensor(pe_, psC, float(CAP - 1), eCt, ALU.min, ALU.add), reads=["psO1", "eC", "tmp64"], writes=["tmp64"])
                add("vector", lambda e: e.tensor_tensor(A1, A1, pe_, ALU.mult), reads=["A1", "tmp64"], writes=["A1"])
                add("vector", lambda e: e.tensor_tensor(A2, A2, pe_, ALU.mult), reads=["A2", "tmp64"], writes=["A2"])
                d1f, d2f = sm[7], sm[8]
                add("vector", lambda e: e.tensor_reduce(d1f, A1, AX.X, ALU.add), reads=["A1"], writes=["d1f"])
                add("vector", lambda e: e.tensor_reduce(d2f, A2, AX.X, ALU.add), reads=["A2"], writes=["d2f"])
                add("vector", lambda e, t=t: e.tensor_copy(dest1i[:, t:t + 1], d1f), reads=["d1f"], writes=["dest1"])
                add("vector", lambda e, t=t: e.tensor_copy(dest2i[:, t:t + 1], d2f), reads=["d2f"], writes=["dest2"])
                if debug in ("x1", "hf"):
                    return
                add("gpsimd", lambda e, t=t: e.indirect_dma_start(out=XS_d, out_offset=bass.IndirectOffsetOnAxis(ap=dest1i[:, t:t + 1], axis=0),
                                                                  in_=hfb, in_offset=None), reads=["hfb", "dest1"], writes=["XSd"], dma=True)
                add("gpsimd", lambda e, t=t: e.indirect_dma_start(out=XS_d, out_offset=bass.IndirectOffsetOnAxis(ap=dest2i[:, t:t + 1], axis=0),
                                                                  in_=hfb, in_offset=None), reads=["hfb", "dest2"], writes=["XSd"], dma=True)


            stage_d(0)
            for gi in range(3):
                emit_merged(record(stage_c, gi), record(stage_d, gi + 1))
                if stile == 0:
                    emit_zero(gi)
            wbs = [load_group(w_out_d[half]) for half in range(2)]

            def stage_fg(j):
                stage_f(j, wbs)
                stage_g(j)

            emit_merged(record(stage_c, 3), record(stage_e, 0))
            if stile == 0:
                emit_zero(3)
            add("vector", lambda e: e.tensor_copy(XL[:, :, 0:3], XL[:, :, 512:515]), reads=["XL"], writes=["XL"])
            for j in range(4):
                emit_merged(record(stage_fg, j), record(stage_e, j + 1) if j + 1 < 4 else [])

        if debug in ("x1", "hf"):
            S_.emit(final_wait_ops=[op for op in S_.all_ops if op.is_dma])
            return nc
        S_.barrier(lambda e: e.memset(bscr, 0.0))
        off[0] = PL_BASE
        W1b = [A(KD * 256, KD, 512, bf=True) for _ in range(2)]
        W3b = [A(KD * 256, KD, 512, bf=True) for _ in range(2)]
        W2b = [A(4 * 512, 4, 1024, bf=True) for _ in range(2)]
        xs = [A(D, 2, D, bf=True) for _ in range(2)]
        xsT = A(KD * 128, KD, 256, bf=True)
        gT = A(4 * 128, 4, 256, bf=True)
        silt = A(256)
        yb = [A(D) for _ in range(2)]
        psTb = [psT_t[:, 0:512].bitcast(BF16).rearrange("p (a b) -> p a b", a=8),
                psT_t[:, 512:1024].bitcast(BF16).rearrange("p (a b) -> p a b", a=8)]
        psTk = ["psT", "psT2"]
        hbanks = [(psA, "psA"), (psB, "psB"), (psO[:, 0:512], "psO0"), (psO[:, 512:1024], "psO1")]
        ybanks = [(psP[:, 0:512], "psP0"), (psP[:, 512:1024], "psP1")]
        silt2 = [silt, A(256)]

        def b_loads(ex):
            k = ex % 2
            fl = lambda ap: ap.rearrange("p c n -> p (c n)")
            add("gpsimd", lambda e: e.dma_start(out=fl(W1b[k]), in_=w1_d[ex], max_dma_last_dim=8192), writes=["W1b%d" % k], dma=True)
            add("gpsimd", lambda e: e.dma_start(out=fl(W3b[k]), in_=w3_d[ex], max_dma_last_dim=8192), writes=["W3b%d" % k], dma=True)
            add("gpsimd", lambda e: e.dma_start(out=fl(W2b[k]), in_=w2_d[ex], max_dma_last_dim=8192), writes=["W2b%d" % k], dma=True)
            add("sync", lambda e: e.dma_start(out=xs[k], in_=XS_d[ex * CAP:(ex + 1) * CAP, :].rearrange("(b p) d -> p b d", p=128)),
                reads=["XSd"], writes=["xs%d" % k], dma=True)

        ycnt = [0]

        def b_compute(ex):
            k = ex % 2
            for blk in range(2):
                for c in range(KD):
                    add("tensor", lambda e, c=c, blk=blk: e.transpose(psTb[blk][:, c, :], xs[k][:, blk, c * 128:(c + 1) * 128], identb),
                        reads=["xs%d" % k, "identb"], writes=[psTk[blk]])
                if blk == 0:
                    add("scalar", lambda e: e.activation(out=xsT[:, :, 0:128], in_=psTb[0], func=AF.Copy), reads=[psTk[0]], writes=["xsT"])
                else:
                    add("vector", lambda e: e.tensor_copy(xsT[:, :, 128:256], psTb[1]), reads=[psTk[1]], writes=["xsT"])
            for f in range(4):
                (h1, h1k), (h3, h3k) = hbanks[2 * (f % 2)], hbanks[2 * (f % 2) + 1]
                sl = silt2[f % 2]
                slk = "silt%d" % (f % 2)
                for kc in range(KD):
                    add("tensor", lambda e, f=f, kc=kc, h1=h1: e.matmul(h1[:, 0:256], W1b[k][:, kc, f * 128:(f + 1) * 128], xsT[:, kc, :],
                                                                     start=(kc == 0), stop=(kc == KD - 1)), reads=["W1b%d" % k, "xsT"], writes=[h1k])
                for kc in range(KD):
                    add("tensor", lambda e, f=f, kc=kc, h3=h3: e.matmul(h3[:, 0:256], W3b[k][:, kc, f * 128:(f + 1) * 128], xsT[:, kc, :],
                                                                     start=(kc == 0), stop=(kc == KD - 1)), reads=["W3b%d" % k, "xsT"], writes=[h3k])
                add("scalar", lambda e, h1=h1, sl=sl: e.activation(out=sl, in_=h1[:, 0:256], func=AF.Silu), reads=[h1k], writes=[slk])
                add("vector", lambda e, f=f, h3=h3, sl=sl: e.tensor_tensor(gT[:, f, :], sl, h3[:, 0:256], ALU.mult), reads=[slk, h3k], writes=["gT%d" % f])
            for blk in range(2):
                yk = ycnt[0] % 2
                ycnt[0] += 1
                for half in range(2):
                    yp, ypk = ybanks[half]
                    for f in range(4):
                        add("tensor", lambda e, yp=yp, half=half, f=f, blk=blk: e.matmul(
                            yp, gT[:, f, blk * 128:(blk + 1) * 128], W2b[k][:, f, half * 512:(half + 1) * 512],
                            start=(f == 0), stop=(f == 3)), reads=["gT%d" % f, "W2b%d" % k], writes=[ypk])
                    if half == 0:
                        add("scalar", lambda e, yk=yk, yp=yp: e.activation(out=yb[yk][:, 0:512], in_=yp, func=AF.Copy), reads=[ypk], writes=["yb%d" % yk])
                    else:
                        add("vector", lambda e, yk=yk, yp=yp: e.tensor_copy(yb[yk][:, 512:1024], yp), reads=[ypk], writes=["yb%d" % yk])
                r0 = ex * CAP + blk * 128
                add("sync", lambda e, yk=yk, r0=r0: e.dma_start(out=Y_d[r0:r0 + 128, :], in_=yb[yk]), reads=["yb%d" % yk], writes=["Yd"], dma=True)

        roff[0] = RO_BASE
        WG = A(KD * D, KD, D, ro=True)
        WP = A(2 * D, 2, D, ro=True)
        gple = A(D)
        gfin = A(D)
        bple = A(D)
        x1c = [A(D) for _ in range(2)]
        yac = [A(D) for _ in range(2)]
        ybc = [A(D) for _ in range(2)]
        pt = [A(256) for _ in range(2)]
        hp = A(D)
        hpT2 = [A(KD * 128, KD, 128, ro=True) for _ in range(2)]
        pT2 = [A(256, 2, 128, ro=True) for _ in range(2)]
        gate = A(D)
        outt = [A(D) for _ in range(2)]
        sm = [A(1) for _ in range(4)]
        add("gpsimd", lambda e: e.dma_start(out=WG.bitcast(F32R), in_=wg_d.rearrange("(c p) n -> p c n", p=128)), writes=["WG"], dma=True)
        add("gpsimd", lambda e: e.dma_start(out=WP.bitcast(F32R), in_=wp_d.rearrange("(c p) n -> p c n", p=128)), writes=["WP"], dma=True)
        for (dst, src, key) in [(gple, gple_d, "gple"), (gfin, gfin_d, "gfin"), (bple, bple_d, "bple")]:
            add("sync", lambda e, dst=dst, src=src: e.dma_start(out=dst, in_=src), writes=[key], dma=True)
        WGr = WG.bitcast(F32R)
        WPr = WP.bitcast(F32R)
        b_loads(0)
        for ex in range(NE):
            if ex + 1 < NE:
                b_loads(ex + 1)
            b_compute(ex)

        def c_loads(t):
            k = t % 2
            add("sync", lambda e: e.dma_start(out=x1c[k], in_=X1_d[t * 128:(t + 1) * 128, :]), reads=["X1d"], writes=["x1c%d" % k], dma=True)
            add("sync", lambda e: e.dma_start(out=pt[k], in_=p_d[t * 128:(t + 1) * 128, :]), writes=["pt%d" % k], dma=True)
            add("gpsimd", lambda e: e.indirect_dma_start(out=yac[k], out_offset=None, in_=Y_d,
                                                         in_offset=bass.IndirectOffsetOnAxis(ap=dest1i[:, t:t + 1], axis=0)),
                reads=["Yd", "dest1"], writes=["yac%d" % k], dma=True)
            add("gpsimd", lambda e: e.indirect_dma_start(out=ybc[k], out_offset=None, in_=Y_d,
                                                         in_offset=bass.IndirectOffsetOnAxis(ap=dest2i[:, t:t + 1], axis=0)),
                reads=["Yd", "dest2"], writes=["ybc%d" % k], dma=True)

        def c_stage1(t):
            k = t % 2
            x2 = x1c[k]
            xk = "x1c%d" % k
            hpT, pT_ = hpT2[k], pT2[k]
            add("vector", lambda e: e.scalar_tensor_tensor(x2, yac[k], wsel1[:, t:t + 1], x2, ALU.mult, ALU.add),
                reads=["yac%d" % k, "wsel1", xk], writes=[xk])
            add("vector", lambda e: e.scalar_tensor_tensor(x2, ybc[k], wsel2[:, t:t + 1], x2, ALU.mult, ALU.add),
                reads=["ybc%d" % k, "wsel2", xk], writes=[xk])
            rms_scale(x2, xk, gple, "gple", hp, "hp", "c", si=0)
            transpose8(hp, "hp", hpT, "hpT%d" % k)
            for c in range(2):
                add("tensor", lambda e, c=c: e.transpose(psA[:, c * 128:(c + 1) * 128], pt[k][:, c * 128:(c + 1) * 128], ident),
                    reads=["pt%d" % k, "ident"], writes=["psA"])
            add("vector", lambda e: e.tensor_copy(pT_.bitcast(F32R), psA[:, 0:256].rearrange("p (a b) -> p a b", a=2)), reads=["psA"], writes=["pT_%d" % k])

        def c_stage2(t):
            k = t % 2
            x2 = x1c[k]
            xk = "x1c%d" % k
            hpTr = hpT2[k].bitcast(F32R)
            pTr = pT2[k].bitcast(F32R)
            for half in range(2):
                for kc in range(KD):
                    add("tensor", lambda e, half=half, kc=kc: e.matmul(psO[:, half * 512:(half + 1) * 512], hpTr[:, kc, :], WGr[:, kc, half * 512:(half + 1) * 512],
                                                                      start=(kc == 0), stop=(kc == KD - 1)), reads=["hpT%d" % k, "WG"], writes=["psO0", "psO1"])
            for half in range(2):
                for kc in range(2):
                    add("tensor", lambda e, half=half, kc=kc: e.matmul(psP[:, half * 512:(half + 1) * 512], pTr[:, kc, :], WPr[:, kc, half * 512:(half + 1) * 512],
                                                                      start=(kc == 0), stop=(kc == 1)), reads=["pT_%d" % k, "WP"], writes=["psP0", "psP1"])
            add("vector", lambda e: e.tensor_tensor(gate, psO, bple, ALU.add), reads=["psO0", "psO1", "bple"], writes=["gate"])
            add("scalar", lambda e: e.activation(out=gate, in_=gate, func=AF.Sigmoid), reads=["gate"], writes=["gate"])
            add("vector", lambda e: e.tensor_tensor(gate, gate, psP, ALU.mult), reads=["gate", "psP0", "psP1"], writes=["gate"])
            add("vector", lambda e: e.tensor_tensor(x2, x2, gate, ALU.add), reads=["gate", xk], writes=[xk])
            rms_scale(x2, xk, gfin, "gfin", outt[k], "outt%d" % k, "f", si=2)
            add("sync", lambda e: e.dma_start(out=out_d[t * 128:(t + 1) * 128, :], in_=outt[k]), reads=["outt%d" % k], dma=True)

        c_loads(0)
        if NT > 1:
            c_loads(1)
        c_stage1(0)
        for t in range(NT):
            ch2 = record(c_stage2, t)
            ch1 = record(c_stage1, t + 1) if t + 1 < NT else []
            emit_merged(ch2, ch1)
            if t + 2 < NT:
                c_loads(t + 2)

        S_.emit(final_wait_ops=[op for op in S_.all_ops if op.is_dma])
    return nc


def core_inputs(b, S, consts, shared, x, p, g_mix, w_in, conv_w, conv_b, lru_wa, lru_ba, lru_wx, lru_bx, lru_lambda, w_out,
                g_ffn, w_router_group, b_router_group, w_router_expert, b_router_expert, w1, w3, w2,
                g_ple, w_ple_gate, b_ple_gate, w_ple_proj, g_final):
    f = lambda a: np.ascontiguousarray(a, dtype=np.float32)
    m = dict(
        x=f(x[b]), p=f(p[0, b]), w_in=shared["w_in"], w_out=shared["w_out"], w1=shared["w1"], w3=shared["w3"], w2=shared["w2"],
        w_ple_gate=f(w_ple_gate[0]), w_ple_proj=f(w_ple_proj[0]),
        w_rt=f(np.concatenate([w_router_group[0], w_router_expert[0]], axis=1)),
        b_rt=bc128(np.concatenate([b_router_group[0], b_router_expert[0]], axis=0)),
        g_mix=bc128(g_mix[0]), g_ffn=bc128(g_ffn[0]), g_ple=bc128(g_ple[0]), g_final=bc128(g_final), b_ple=bc128(b_ple_gate[0]),
        conv_w=f(np.transpose(np.asarray(conv_w[0]).reshape(4, 4, 128), (2, 1, 0))),
        conv_b=chan_major(conv_b[0]),
        lru_wa=block_diag(np.asarray(lru_wa[0])), lru_wx=block_diag(np.asarray(lru_wx[0])),
        lru_ba=chan_major(np.asarray(lru_ba[0]).reshape(-1)), lru_bx=chan_major(np.asarray(lru_bx[0]).reshape(-1)),
        lru_lam=chan_major(lru_lambda[0]),
    )
    m.update(consts)
    return m


def shared_layout(w_in, w_out, w1, w3, w2):
    def pmajor(w):
        lead = w.shape[:-2]
        C = w.shape[-2] // 128
        n = w.shape[-1]
        v = np.asarray(w, np.float32).reshape(*lead, C, 128, n)
        v = np.moveaxis(v, -3, -2)
        return np.ascontiguousarray(v).reshape(*lead, 128, C * n)
    w_in_g = np.stack([np.asarray(w_in[0])[:, g * 512:(g + 1) * 512] for g in range(6)], axis=0)
    w_out_g = np.stack([np.asarray(w_out[0])[:, h * 512:(h + 1) * 512] for h in range(2)], axis=0)
    return dict(w_in=pmajor(w_in_g), w_out=pmajor(w_out_g), w1=pmajor(w1[0]), w3=pmajor(w3[0]), w2=pmajor(w2[0]))


_NC_CACHE = {}


def kernel(**inputs):
    inputs = {k: np.asarray(v) for k, v in inputs.items()}
    x = inputs["x"]
    B, S, _ = x.shape
    consts = make_consts(S)
    if S not in _NC_CACHE:
        _NC_CACHE[S] = build_nc(S)
    nc = _NC_CACHE[S]
    shared = shared_layout(inputs["w_in"], inputs["w_out"], inputs["w1"], inputs["w3"], inputs["w2"])
    in_maps = [core_inputs(b, S, consts, shared, **inputs) for b in range(B)]
    res = run_bass_kernel_spmd(nc, in_maps, core_ids=list(range(B)))
    return np.stack([np.asarray(r["out"]) for r in res.results], axis=0).astype(np.float32)
```

```python
import contextlib
import numpy as np
import concourse.bass as bass
import concourse.mybir as mybir
from concourse.bass_utils import run_bass_kernel_spmd

F32 = mybir.dt.float32
F32R = mybir.dt.float32r
I32 = mybir.dt.int32
BF16 = mybir.dt.bfloat16
AF = mybir.ActivationFunctionType
ALU = mybir.AluOpType
AX = mybir.AxisListType

D = 1024
KD = 8
NE = 64
DE = 512
CAP = 256
EPS = 1e-6
ENGINES = ("tensor", "vector", "scalar", "gpsimd", "sync")


class Op:
    __slots__ = ("eng", "fn", "is_dma", "needs_inc", "inc_index", "waits", "order_deps",
                 "dma_sem", "dma_val", "dma_prev", "idx", "cost", "xfer", "succ", "ndeps",
                 "finish", "ready")

    def __init__(self, eng, fn, is_dma):
        self.eng = eng
        self.fn = fn
        self.is_dma = is_dma
        self.needs_inc = False
        self.inc_index = None
        self.waits = []
        self.order_deps = []
        self.dma_sem = None
        self.dma_val = None
        self.dma_prev = 0
        self.succ = []
        self.ndeps = 0
        self.finish = 0.0
        self.ready = 0.0
        self.cost = 0.1
        self.xfer = 0.0


class _Mock:
    def __init__(self):
        self.calls = []

    def __getattr__(self, name):
        def f(*a, **k):
            self.calls.append((name, a, k))
            return self
        return f


def _fsize(ap):
    try:
        return int(ap.free_size())
    except Exception:
        return 256


def _estimate(op):
    m = _Mock()
    try:
        op.fn(m)
        name, a, k = m.calls[0]
    except Exception:
        return
    if op.is_dma:
        try:
            o = k.get("out", a[0] if a else None)
            i = k.get("in_", None)
            nb = min(o.nbytes(), i.nbytes()) if name == "indirect_dma_start" else max(o.nbytes(), i.nbytes())
        except Exception:
            nb = 65536
        op.xfer = nb / 300e3
        op.cost = 1.2 if op.eng == "gpsimd" else 0.08
        return
    if op.eng == "tensor":
        if name == "matmul":
            n = _fsize(a[2])
            mult = 4.0 if a[1].dtype == F32 else 1.0
            op.cost = max(n * mult, 64.0) / 2000.0
        else:
            op.cost = 0.13
    elif op.eng == "vector":
        o = k.get("out", a[0] if a else None)
        n = _fsize(o)
        if name == "tensor_reduce" or name == "max":
            n = _fsize(a[1])
        if name == "tensor_tensor_scan":
            n *= 2
        op.cost = (max(n, 64) + 60) / 960.0
    elif op.eng == "scalar":
        o = k.get("out", a[0] if a else None)
        op.cost = (max(_fsize(o), 64) + 250) / 1400.0
    else:
        op.cost = 0.2


class Sched:
    def __init__(self, nc, n_dma_sems=48):
        self.nc = nc
        self.last_writer = {}
        self.readers = {}
        self.n_dma_sems = n_dma_sems
        self.all_ops = []
        self.barrier_op = None
        self.last_of = {e: None for e in ENGINES}

    def add(self, eng, fn, reads=(), writes=(), dma=False):
        op = Op(eng, fn, dma)
        op.idx = len(self.all_ops)
        deps = []
        if self.barrier_op is not None:
            deps.append(self.barrier_op)
        for k in reads:
            w = self.last_writer.get(k)
            if w is not None:
                deps.append(w)
        for k in writes:
            w = self.last_writer.get(k)
            if w is not None:
                deps.append(w)
            deps.extend(self.readers.get(k, ()))
        seen = set()
        for d in deps:
            if id(d) in seen or d is op:
                continue
            seen.add(id(d))
            if (not d.is_dma) and d.eng == eng and eng == "tensor":
                op.order_deps.append(d)
            else:
                op.waits.append(d)
        for k in writes:
            self.last_writer[k] = op
            self.readers[k] = []
        for k in reads:
            if k not in writes:
                self.readers.setdefault(k, []).append(op)
        self.all_ops.append(op)
        self.last_of[eng] = op
        return op

    def barrier(self, fn):
        op = self.add("vector", fn, writes=["__barrier__"])
        have = set(id(w) for w in op.waits)
        for d in self.all_ops[:-1]:
            if id(d) not in have:
                op.waits.append(d)
        self.barrier_op = op
        self.last_writer = {}
        self.readers = {}
        return op

    def schedule(self):
        import heapq
        ops = self.all_ops
        for op in ops:
            _estimate(op)
            op.ndeps = 0
            op.succ = []
        for op in ops:
            for d in op.waits:
                d.succ.append(op)
                op.ndeps += 1
            for d in op.order_deps:
                d.succ.append(op)
                op.ndeps += 1
        future = {e: [] for e in ENGINES}
        avail = {e: [] for e in ENGINES}
        free_at = {e: 0.0 for e in ENGINES}
        dma_free = [0.0]
        order = {e: [] for e in ENGINES}
        for op in ops:
            if op.ndeps == 0:
                heapq.heappush(future[op.eng], (0.0, op.idx, op))
        remaining = len(ops)
        while remaining:
            best = None
            for e in ENGINES:
                fu, av = future[e], avail[e]
                while fu and fu[0][0] <= free_at[e]:
                    r, i, o = heapq.heappop(fu)
                    heapq.heappush(av, (i, o))
                if av:
                    cand = (free_at[e], av[0][0], e, True)
                elif fu:
                    cand = (fu[0][0], fu[0][1], e, False)
                else:
                    continue
                if best is None or cand < best:
                    best = cand
            assert best is not None, "scheduler stuck (dependency cycle?)"
            t, _, e, from_av = best
            if from_av:
                _, op = heapq.heappop(avail[e])
            else:
                _, _, op = heapq.heappop(future[e])
            start = max(t, free_at[e])
            free_at[e] = start + op.cost
            if op.is_dma:
                s0 = max(start + op.cost, dma_free[0])
                dma_free[0] = s0 + op.xfer
                op.finish = s0 + op.xfer + 2.0
            else:
                op.finish = start + op.cost
            order[e].append(op)
            remaining -= 1
            for sc in op.succ:
                lat = 0.0 if (sc.eng == op.eng and not op.is_dma) else 0.35
                r = op.finish + lat
                if r > sc.ready:
                    sc.ready = r
                sc.ndeps -= 1
                if sc.ndeps == 0:
                    heapq.heappush(future[sc.eng], (sc.ready, sc.idx, sc))
        self.est_time = max(op.finish for op in ops)
        return order

    def emit(self, final_wait_ops=()):
        nc = self.nc
        order = self.schedule()
        pos = {}
        for e in ENGINES:
            for i, op in enumerate(order[e]):
                pos[id(op)] = i
        for op in self.all_ops:
            if len(op.waits) > 256:
                keep = {}
                dm = []
                for d in op.waits:
                    if d.is_dma:
                        dm.append(d)
                    elif d.eng not in keep or pos[id(d)] > pos[id(keep[d.eng])]:
                        keep[d.eng] = d
                op.waits = dm + list(keep.values())
        for op in self.all_ops:
            for d in op.waits:
                if not d.is_dma:
                    d.needs_inc = True
        half = self.n_dma_sems // 2
        for e in ENGINES:
            c = 0
            rr = 0
            counts = [0] * self.n_dma_sems
            for op in order[e]:
                if op.is_dma:
                    base = half if e == "gpsimd" else 0
                    s = base + rr
                    rr = (rr + 1) % half
                    op.dma_sem = s
                    op.dma_prev = counts[s]
                    counts[s] += 16
                    op.dma_val = counts[s]
                elif op.needs_inc:
                    c += 1
                    op.inc_index = c
        with contextlib.ExitStack() as st:
            esem = {e: st.enter_context(nc.semaphore("es_" + e)) for e in ENGINES}
            dsem = [st.enter_context(nc.semaphore("ds_%d" % i)) for i in range(self.n_dma_sems)]
            block = st.enter_context(nc.Block())
            sched = self

            def run_engine(e, engobj):
                waited_e = {x: 0 for x in ENGINES}
                waited_d = [0] * sched.n_dma_sems
                for op in order[e]:
                    need_e = {}
                    need_d = {}
                    for d in op.waits:
                        if d.is_dma:
                            if d.dma_val > need_d.get(d.dma_sem, 0):
                                need_d[d.dma_sem] = d.dma_val
                        else:
                            if d.inc_index > need_e.get(d.eng, 0):
                                need_e[d.eng] = d.inc_index
                    if op.is_dma and op.dma_prev > need_d.get(op.dma_sem, 0):
                        need_d[op.dma_sem] = op.dma_prev
                    for pe, v in need_e.items():
                        if v > waited_e[pe]:
                            engobj.wait_ge(esem[pe], v)
                            waited_e[pe] = v
                    for s, v in need_d.items():
                        if v > waited_d[s]:
                            engobj.wait_ge(dsem[s], v)
                            waited_d[s] = v
                    ins = op.fn(engobj)
                    if op.is_dma:
                        ins.then_inc(dsem[op.dma_sem], 16)
                    elif op.needs_inc:
                        ins.then_inc(esem[e], 1)
                if e == "sync":
                    for d in final_wait_ops:
                        if d.dma_val > waited_d[d.dma_sem]:
                            engobj.wait_ge(dsem[d.dma_sem], d.dma_val)
                            waited_d[d.dma_sem] = d.dma_val

            @block.tensor
            def _(t):
                run_engine("tensor", t)

            @block.vector
            def _(v):
                run_engine("vector", v)

            @block.scalar
            def _(s):
                run_engine("scalar", s)

            @block.gpsimd
            def _(g):
                run_engine("gpsimd", g)

            @block.sync
            def _(sy):
                run_engine("sync", sy)


def make_consts(S):
    H, C, Dh = 4, 128, 128
    log_g = np.log(1.0 - 2.0 ** (-5.0 - np.arange(H, dtype=np.float64)))
    idx = np.arange(C, dtype=np.float64)
    diff = idx[:, None] - idx[None, :]
    decay = np.where(diff >= 0, np.exp(np.maximum(diff, 0.0)[None] * log_g[:, None, None]), 0.0)
    decayT = np.transpose(decay, (2, 0, 1))
    q_decay = np.exp((idx + 1.0)[None, :] * log_g[:, None])
    k_decay = np.exp((C - 1.0 - idx)[None, :] * log_g[:, None])
    chunk_decay = np.exp(C * log_g)
    QD = np.broadcast_to(q_decay[None, :, :], (128, H, C))
    KDc = np.transpose(k_decay, (1, 0))
    CDc = np.broadcast_to(chunk_decay[None, :], (128, H))
    half = Dh // 2
    inv = 10000.0 ** (-np.arange(half, dtype=np.float32).astype(np.float64) / half)
    pos = np.arange(S, dtype=np.float64)
    ang = (pos[:, None].astype(np.float32) * inv[None, :].astype(np.float32)).astype(np.float64)
    cos = np.cos(ang)
    sin = np.sin(ang)
    cosF = np.concatenate([cos, cos], axis=1)
    sinF = np.concatenate([-sin, sin], axis=1)
    sc = Dh ** -0.5
    rope = np.stack([cosF, sinF, cosF * sc, sinF * sc], axis=1)
    U = np.triu(np.ones((128, 128)), k=1)
    eC = np.broadcast_to((np.arange(NE) * CAP)[None, :], (128, NE))
    f = lambda a: np.ascontiguousarray(a, dtype=np.float32)
    return dict(c_ident=f(np.eye(128)), c_decayT=f(decayT), c_QD=f(QD), c_KD=f(KDc), c_CD=f(CDc),
                c_rope=f(rope), c_U=f(U), c_ones=f(np.ones((128, 128))), c_eC=f(eC))


def bc128(v):
    v = np.asarray(v, dtype=np.float32).reshape(1, -1)
    return np.ascontiguousarray(np.broadcast_to(v, (128, v.shape[1])))


def chan_major(v):
    return np.ascontiguousarray(np.asarray(v, np.float32).reshape(4, 128).T)


def block_diag(w):
    out = np.zeros((128, 4, 128), np.float32)
    for c in range(4):
        for hh in range(2):
            out[hh * 64:(hh + 1) * 64, c, hh * 64:(hh + 1) * 64] = w[2 * c + hh]
    return out


def build_nc(S, debug=None, stop_after=None):
    NT = S // 128
    NST = S // 512
    nc = bass.Bass("TRN2", target_bir_lowering=False)

    def din(name, shape, dt=F32):
        return nc.dram_tensor(name, list(shape), dt, kind="ExternalInput").ap()

    x_d = din("x", [S, D])
    p_d = din("p", [S, 256])
    w_in_d = din("w_in", [6, 128, KD * 512])
    w_out_d = din("w_out", [2, 128, KD * 512])
    w1_d = din("w1", [NE, 128, KD * DE])
    w3_d = din("w3", [NE, 128, KD * DE])
    w2_d = din("w2", [NE, 128, 4 * D])
    wg_d = din("w_ple_gate", [D, D])
    wp_d = din("w_ple_proj", [256, D])
    wrt_d = din("w_rt", [D, 72])
    brt_d = din("b_rt", [128, 72])
    gmix_d = din("g_mix", [128, D])
    gffn_d = din("g_ffn", [128, D])
    gple_d = din("g_ple", [128, D])
    gfin_d = din("g_final", [128, D])
    bple_d = din("b_ple", [128, D])
    cw_d = din("conv_w", [128, 4, 4])
    cb_d = din("conv_b", [128, 4])
    wa_d = din("lru_wa", [128, 4, 128])
    wx_d = din("lru_wx", [128, 4, 128])
    ba_d = din("lru_ba", [128, 4])
    bx_d = din("lru_bx", [128, 4])
    lam_d = din("lru_lam", [128, 4])
    ident_d = din("c_ident", [128, 128])
    decT_d = din("c_decayT", [128, 4, 128])
    QD_d = din("c_QD", [128, 4, 128])
    KD_d = din("c_KD", [128, 4])
    CD_d = din("c_CD", [128, 4])
    rope_d = din("c_rope", [S, 4, 128])
    U_d = din("c_U", [128, 128])
    ones_d = din("c_ones", [128, 128])
    eC_d = din("c_eC", [128, NE])
    out_d = nc.dram_tensor("out", [S, D], F32, kind="ExternalOutput").ap()
    X1_d = nc.dram_tensor("X1s", [S, D], F32, kind="Internal").ap()
    XS_d = nc.dram_tensor("XSs", [NE * CAP, D], BF16, kind="Internal").ap()
    Y_d = nc.dram_tensor("Ys", [NE * CAP, D], F32, kind="Internal").ap()
    dbg_d = None
    if debug is not None:
        dbg_d = nc.dram_tensor("dbg", [S, D], F32, kind="ExternalOutput").ap()

    with contextlib.ExitStack() as st:
        RO_BASE, RO_SIZE = 0, 16384
        ARENA = 52800 - RO_SIZE
        arena = st.enter_context(nc.sbuf_tensor("arena", [128, ARENA], F32))
        arena_ro = st.enter_context(nc.sbuf_tensor("arena_ro", [128, RO_SIZE], F32))
        PL_BASE = 400
        off = [0]
        roff = [RO_BASE]

        def A(n, *shape, ro=False, bf=False):
            if ro:
                o = roff[0]
                roff[0] += n
                assert roff[0] <= RO_BASE + RO_SIZE, ("ro overflow", roff[0])
            else:
                o = off[0]
                off[0] += n
                assert off[0] <= ARENA, ("arena overflow", o, off[0])
            ap = (arena_ro if ro else arena)[:, o:o + n]
            if bf:
                ap = ap.bitcast(BF16)
            if len(shape) == 2:
                ap = ap.rearrange("p (a b) -> p a b", a=shape[0])
            elif len(shape) == 3:
                ap = ap.rearrange("p (a b c) -> p a b c", a=shape[0], b=shape[1])
            return ap

        def psum(name, n):
            return st.enter_context(nc.psum_tensor(name, [128, n], F32))

        psT_t = psum("psT", 1024)
        psO_t = psum("psO", 1024)
        psP_t = psum("psP", 1024)
        psA_t = psum("psA", 512)
        psB_t = psum("psB", 512)
        psT = psT_t[:, :].rearrange("p (a b) -> p a b", a=8)
        psO = psO_t[:, :]
        psP = psP_t[:, :]
        psA = psA_t[:, :]
        psB = psB_t[:, :]

        S_ = Sched(nc)
        rec = [None]

        def add(eng, fn, reads=(), writes=(), dma=False):
            if rec[0] is not None:
                rec[0].append((eng, fn, tuple(reads), tuple(writes), dma))
                return None
            return S_.add(eng, fn, reads=reads, writes=writes, dma=dma)

        def record(f, *args):
            rec[0] = []
            f(*args)
            out = rec[0]
            rec[0] = None
            return out

        def emit_merged(*chains):
            chains = [c for c in chains if c]
            idx = [0] * len(chains)
            while True:
                live = [i for i in range(len(chains)) if idx[i] < len(chains[i])]
                if not live:
                    break
                i = min(live, key=lambda i: idx[i] / len(chains[i]))
                eng, fn, rd, wr, dma = chains[i][idx[i]]
                idx[i] += 1
                S_.add(eng, fn, reads=rd, writes=wr, dma=dma)

        ident = A(128)
        dest1 = A(NT)
        dest2 = A(NT)
        wsel1 = A(NT)
        wsel2 = A(NT)
        dest1i = dest1.bitcast(I32)
        dest2i = dest2.bitcast(I32)
        bscr = A(1)
        identb = A(64, bf=True)
        persist_end = off[0]
        assert persist_end <= PL_BASE
        off[0] = PL_BASE

        add("sync", lambda e: e.dma_start(out=ident, in_=ident_d), writes=["ident"], dma=True)
        add("vector", lambda e: e.tensor_copy(identb, ident), reads=["ident"], writes=["identb"])

        U_t = A(128)
        ones_t = A(128)
        decT = A(512, 4, 128)
        QDt = A(512, 4, 128)
        KDt = A(4)
        CDt = A(4)
        eCt = A(NE)
        gmix = A(D)
        gffn = A(D)
        cw = A(16, 4, 4)
        cb = A(4)
        WA = A(512, 4, 128)
        WX = A(512, 4, 128)
        ba = A(4)
        bx = A(4)
        lam = A(4)
        cneg = A(4)
        c2 = A(4)
        wrt = A(KD * 72, KD, 72)
        brt = A(72)
        hstate = A(4)
        Rcum = A(NE)
        state = A(512, 4, 128)
        tmp4 = [A(4) for _ in range(6)]

        for (dst, src, key) in [(U_t, U_d, "U"), (ones_t, ones_d, "ones"), (decT, decT_d, "decT"),
                                (QDt, QD_d, "QD"), (KDt, KD_d, "KD"), (CDt, CD_d, "CD"), (eCt, eC_d, "eC"),
                                (gmix, gmix_d, "gmix"), (gffn, gffn_d, "gffn"), (cw, cw_d, "cw"), (cb, cb_d, "cb"),
                                (WA, wa_d, "WA"), (WX, wx_d, "WX"), (ba, ba_d, "ba"), (bx, bx_d, "bx"),
                                (lam, lam_d, "lam"), (brt, brt_d, "brt")]:
            add("sync", lambda e, dst=dst, src=src: e.dma_start(out=dst, in_=src), writes=[key], dma=True)
        add("sync", lambda e: e.dma_start(out=wrt, in_=wrt_d.rearrange("(c p) n -> p c n", p=128)), writes=["wrt"], dma=True)
        add("vector", lambda e: e.memset(hstate, 0.0), writes=["hstate"])
        add("vector", lambda e: e.memset(Rcum, 0.0), writes=["Rcum"])
        add("vector", lambda e: e.memset(state, 0.0), writes=["state"])

        ya, za, sa, s2, acc, t0 = tmp4
        add("vector", lambda e: e.tensor_scalar(ya, lam, -1.0, None, ALU.mult), reads=["lam"], writes=["ya"])
        add("vector", lambda e: e.tensor_tensor(za, ya, lam, ALU.min), reads=["ya", "lam"], writes=["za"])
        add("scalar", lambda e: e.activation(out=za, in_=za, func=AF.Exp), reads=["za"], writes=["za"])
        add("vector", lambda e: e.tensor_scalar(sa, za, 2.0, None, ALU.add), reads=["za"], writes=["sa"])
        add("vector", lambda e: e.reciprocal(sa, sa), reads=["sa"], writes=["sa"])
        add("vector", lambda e: e.tensor_tensor(sa, sa, za, ALU.mult), reads=["sa", "za"], writes=["sa"])
        add("vector", lambda e: e.tensor_tensor(s2, sa, sa, ALU.mult), reads=["sa"], writes=["s2"])
        add("vector", lambda e: e.memset(acc, 1.0 / 15.0), writes=["acc"])
        for k in (13, 11, 9, 7, 5, 3, 1):
            add("vector", lambda e: e.tensor_tensor(acc, acc, s2, ALU.mult), reads=["acc", "s2"], writes=["acc"])
            add("vector", lambda e, k=k: e.tensor_scalar(acc, acc, 1.0 / k, None, ALU.add), reads=["acc"], writes=["acc"])
        add("vector", lambda e: e.tensor_tensor(acc, acc, sa, ALU.mult), reads=["acc", "sa"], writes=["acc"])
        add("vector", lambda e: e.tensor_scalar(t0, ya, 0.0, None, ALU.max), reads=["ya"], writes=["t0"])
        add("vector", lambda e: e.scalar_tensor_tensor(t0, acc, 2.0, t0, ALU.mult, ALU.add), reads=["acc", "t0"], writes=["t0"])
        add("vector", lambda e: e.tensor_scalar(cneg, t0, -8.0, None, ALU.mult), reads=["t0"], writes=["cneg"])
        add("vector", lambda e: e.tensor_scalar(c2, t0, -16.0, None, ALU.mult), reads=["t0"], writes=["c2"])

        setup_end = off[0]
        wbuf = [A(KD * 512, KD, 512, ro=True) for _ in range(2)]
        xt = [A(D) for _ in range(4)]
        hn = A(D)
        hT = A(KD * 512, KD, 512, ro=True)
        XL = A(4 * 515, 4, 515)
        GL = A(4 * 512, 4, 512)
        ylruT = A(4 * 512, 4, 512, ro=True)
        lt = [A(512) for _ in range(6)]
        QKVG = [A(4 * 512, 4, 512) for _ in range(4)]
        yretT = A(4 * 512, 4, 512, ro=True)
        rt_ = [A(512) for _ in range(7)]
        ropet = A(512, 4, 128)
        hfT = A(KD * 128, KD, 128)
        hfb = A(512, bf=True)
        rs = [A(80) for _ in range(8)]
        sm = [A(1) for _ in range(16)]
        rse = [A(4) for _ in range(4)]
        phaseA_end = off[0]

        add("vector", lambda e: e.memset(XL[:, :, 0:3], 0.0), writes=["XL"])
        zt = A(1024, bf=True)

        def rms_scale(xin, xkey, gbc, gkey, outap, outkey, tag, si=0):
            ss, rstd = sm[si], sm[si + 1]
            ssk, rk = "ss%d" % si, "rstd%d" % si
            add("scalar", lambda e: e.activation(out=outap, in_=xin, func=AF.Square, accum_out=ss),
                reads=[xkey], writes=[outkey, ssk])
            add("vector", lambda e: e.tensor_scalar(ss, ss, 1.0 / D, EPS, ALU.mult, ALU.add), reads=[ssk], writes=[ssk])
            add("scalar", lambda e: e.activation(out=ss, in_=ss, func=AF.Sqrt), reads=[ssk], writes=[ssk])
            add("vector", lambda e: e.reciprocal(rstd, ss), reads=[ssk], writes=[rk])
            add("vector", lambda e: e.scalar_tensor_tensor(outap, xin, rstd, gbc, ALU.mult, ALU.mult),
                reads=[xkey, rk, gkey], writes=[outkey])

        def transpose8(src, srckey, dst3, dstkey, n=8, rounded=True, eng="scalar"):
            pk = ["psT", "psT2"] if n > 4 else ["psT"]
            for c in range(n):
                add("tensor", lambda e, c=c: e.transpose(psT[:, c, :], src[:, c * 128:(c + 1) * 128], ident),
                    reads=[srckey, "ident"], writes=pk)
            o = dst3.bitcast(F32R) if rounded else dst3
            if eng == "scalar":
                add("scalar", lambda e: e.activation(out=o, in_=psT[:, 0:n, :], func=AF.Copy), reads=pk, writes=[dstkey])
            else:
                add("vector", lambda e: e.tensor_copy(o, psT[:, 0:n, :]), reads=pk, writes=[dstkey])

        wcount = [0]

        def load_group(src_ap, nk=KD):
            k = wcount[0] % 2
            wcount[0] += 1
            dst = wbuf[k].bitcast(F32R).rearrange("p c n -> p (c n)")
            add("gpsimd", lambda e: e.dma_start(out=dst, in_=src_ap, max_dma_last_dim=8192),
                writes=["wbuf%d" % k], dma=True)
            return wbuf[k].bitcast(F32R), "wbuf%d" % k

        pp = [0]

        def next_ps():
            pp[0] ^= 1
            return (psA, "psA") if pp[0] else (psB, "psB")

        for stile in range(NST):
            for j in range(4):
                t = stile * 4 + j
                add("sync", lambda e, j=j, t=t: e.dma_start(out=xt[j], in_=x_d[t * 128:(t + 1) * 128, :]),
                    writes=["xt%d" % j], dma=True)
                rms_scale(xt[j], "xt%d" % j, gmix, "gmix", hn, "hn", "a")
                transpose8(hn, "hn", hT[:, :, j * 128:(j + 1) * 128], "hT")
            XS_v = XS_d.rearrange("(p r) d -> p (r d)", p=128)
            nz = (NE * CAP // 128) * D // 2048

            def emit_zero(part):
                if part == 0:
                    add("vector", lambda e: e.memset(zt, 0.0), writes=["zt"])
                for kz in range(part * nz // 4, (part + 1) * nz // 4):
                    add("sync", lambda e, kz=kz: e.dma_start(out=XS_v[:, kz * 2048:(kz + 1) * 2048], in_=zt),
                        reads=["zt", "ylruT"], writes=["XSz%d" % kz], dma=True)
                if part == 3:
                    add("vector", lambda e: e.memset(bscr, 0.0), reads=["XSz%d" % kz for kz in range(nz)], writes=["XSd"])
            hTr = hT.bitcast(F32R)
            for g, (dst, dkey, o0) in enumerate([(XL, "XL", 3), (GL, "GL", 0)]):
                wb, wkey = load_group(w_in_d[g])
                for c in range(4):
                    ps, pkey = next_ps()
                    for kc in range(KD):
                        add("tensor", lambda e, ps=ps, wb=wb, kc=kc, c=c: e.matmul(
                            ps, wb[:, kc, c * 128:(c + 1) * 128], hTr[:, kc, :], start=(kc == 0), stop=(kc == KD - 1)),
                            reads=[wkey, "hT"], writes=[pkey])
                    add("scalar", lambda e, ps=ps, dst=dst, c=c, o0=o0: e.activation(out=dst[:, c, o0:o0 + 512], in_=ps, func=AF.Copy),
                        reads=[pkey], writes=[dkey])
            def stage_c(c):
                xc, r_, i_, a_, u_, g_ = lt
                add("vector", lambda e, c=c: e.tensor_scalar(xc, XL[:, c, 3:515], cw[:, c, 3:4], cb[:, c:c + 1], ALU.mult, ALU.add),
                    reads=["XL", "cw", "cb"], writes=["xc"])
                for k in (2, 1, 0):
                    add("vector", lambda e, c=c, k=k: e.scalar_tensor_tensor(xc, XL[:, c, k:k + 512], cw[:, c, k:k + 1], xc, ALU.mult, ALU.add),
                        reads=["XL", "cw", "xc"], writes=["xc"])
                add("tensor", lambda e, c=c: e.matmul(psP[:, 0:512], WA[:, c, :], xc, start=True, stop=True), reads=["WA", "xc"], writes=["psP0"])
                add("tensor", lambda e, c=c: e.matmul(psP[:, 512:1024], WX[:, c, :], xc, start=True, stop=True), reads=["WX", "xc"], writes=["psP1"])
                add("scalar", lambda e, c=c: e.activation(out=r_, in_=psP[:, 0:512], func=AF.Sigmoid, bias=ba[:, c:c + 1]), reads=["psP0", "ba"], writes=["r_"])
                add("scalar", lambda e, c=c: e.activation(out=i_, in_=psP[:, 512:1024], func=AF.Sigmoid, bias=bx[:, c:c + 1]), reads=["psP1", "bx"], writes=["i_"])
                add("scalar", lambda e, c=c: e.activation(out=a_, in_=r_, func=AF.Exp, scale=cneg[:, c:c + 1]), reads=["r_", "cneg"], writes=["a_"])
                add("scalar", lambda e, c=c: e.activation(out=u_, in_=r_, func=AF.Exp, scale=c2[:, c:c + 1]), reads=["r_", "c2"], writes=["u_"])
                add("vector", lambda e: e.tensor_scalar(u_, u_, -1.0, 1.0, ALU.mult, ALU.add), reads=["u_"], writes=["u_"])
                add("scalar", lambda e: e.activation(out=u_, in_=u_, func=AF.Sqrt), reads=["u_"], writes=["u_"])
                add("vector", lambda e: e.tensor_tensor(i_, i_, xc, ALU.mult), reads=["i_", "xc"], writes=["i_"])
                add("vector", lambda e: e.tensor_tensor(u_, u_, i_, ALU.mult), reads=["u_", "i_"], writes=["u_"])
                add("vector", lambda e, c=c: e.tensor_tensor_scan(r_, a_, u_, hstate[:, c:c + 1], ALU.mult, ALU.add),
                    reads=["a_", "u_", "hstate"], writes=["r_"])
                add("vector", lambda e, c=c: e.tensor_copy(hstate[:, c:c + 1], r_[:, 511:512]), reads=["r_"], writes=["hstate"])
                add("vector", lambda e, c=c: e.tensor_tensor(g_, GL[:, c, :], GL[:, c, :], ALU.mult), reads=["GL"], writes=["g_"])
                add("vector", lambda e: e.tensor_scalar(g_, g_, 0.044715, 1.0, ALU.mult, ALU.add), reads=["g_"], writes=["g_"])
                add("vector", lambda e, c=c: e.tensor_tensor(g_, g_, GL[:, c, :], ALU.mult), reads=["g_", "GL"], writes=["g_"])
                add("scalar", lambda e: e.activation(out=g_, in_=g_, func=AF.Sigmoid, scale=1.5957691216057308), reads=["g_"], writes=["g_"])
                add("vector", lambda e, c=c: e.tensor_tensor(g_, g_, GL[:, c, :], ALU.mult), reads=["g_", "GL"], writes=["g_"])
                add("vector", lambda e, c=c: e.tensor_tensor(ylruT[:, c, :].bitcast(F32R), r_, g_, ALU.mult), reads=["r_", "g_"], writes=["ylruT"])

            def stage_d(gi):
                wb, wkey = load_group(w_in_d[2 + gi])
                for j in range(4):
                    ps, pkey = next_ps()
                    for kc in range(KD):
                        add("tensor", lambda e, ps=ps, wb=wb, kc=kc, j=j: e.matmul(
                            ps, hTr[:, kc, j * 128:(j + 1) * 128], wb[:, kc, :], start=(kc == 0), stop=(kc == KD - 1)),
                            reads=[wkey, "hT"], writes=[pkey])
                    add("scalar", lambda e, ps=ps, gi=gi, j=j: e.activation(out=QKVG[gi][:, j, :], in_=ps, func=AF.Copy),
                        reads=[pkey], writes=["qkvg%d_%d" % (gi, j)])

            def stage_e(j):
                t = stile * 4 + j
                Qj = QKVG[0][:, j, :].rearrange("p (h d) -> p h d", h=4)
                Kj = QKVG[1][:, j, :].rearrange("p (h d) -> p h d", h=4)
                Vj = QKVG[2][:, j, :].rearrange("p (h d) -> p h d", h=4)
                Gj = QKVG[3][:, j, :]
                qr, kr, ta, qT, qdT, kT, kd = [r.rearrange("p (h d) -> p h d", h=4) for r in rt_]
                osb, osq, sT = qr, kr, ta
                add("sync", lambda e, t=t: e.dma_start(out=ropet, in_=rope_d[t * 128:(t + 1) * 128, :, :]), writes=["rope"], dma=True)

                def rotary(src, skey, dst, dkey, ci):
                    cosb = ropet[:, ci, :].unsqueeze(1).to_broadcast([128, 4, 128])
                    add("vector", lambda e: e.tensor_tensor(dst, src, cosb, ALU.mult), reads=[skey, "rope"], writes=[dkey])
                    s_lo = ropet[:, ci + 1, 0:64].unsqueeze(1).to_broadcast([128, 4, 64])
                    s_hi = ropet[:, ci + 1, 64:128].unsqueeze(1).to_broadcast([128, 4, 64])
                    add("vector", lambda e: e.tensor_tensor(ta[:, :, 0:64], src[:, :, 64:128], s_lo, ALU.mult), reads=[skey, "rope"], writes=["R2"])
                    add("vector", lambda e: e.tensor_tensor(ta[:, :, 64:128], src[:, :, 0:64], s_hi, ALU.mult), reads=[skey, "rope"], writes=["R2"])
                    add("vector", lambda e: e.tensor_tensor(dst, dst, ta, ALU.add), reads=[dkey, "R2"], writes=[dkey])

                rotary(Qj, "qkvg0_%d" % j, qr, "R0", 0)
                rotary(Kj, "qkvg1_%d" % j, kr, "R1", 2)
                for h in range(4):
                    add("tensor", lambda e, h=h: e.transpose(psT[:, h, :], qr[:, h, :], ident), reads=["R0", "ident"], writes=["psT"])
                add("scalar", lambda e: e.activation(out=qT, in_=psT[:, 0:4, :], func=AF.Copy), reads=["psT"], writes=["R3"])
                add("vector", lambda e: e.tensor_tensor(qdT, qT, QDt, ALU.mult), reads=["R3", "QD"], writes=["R4"])
                for h in range(4):
                    add("tensor", lambda e, h=h: e.transpose(psT[:, h, :], kr[:, h, :], ident), reads=["R1", "ident"], writes=["psT"])
                add("scalar", lambda e: e.activation(out=kT, in_=psT[:, 0:4, :], func=AF.Copy), reads=["psT"], writes=["R5"])
                add("vector", lambda e: e.tensor_tensor(kd, kr, KDt.unsqueeze(2).to_broadcast([128, 4, 128]), ALU.mult), reads=["R1", "KD"], writes=["R6"])
                psA3 = psA.rearrange("p (h d) -> p h d", h=4)
                psB3 = psB.rearrange("p (h d) -> p h d", h=4)
                psO3 = psO[:, 0:512].rearrange("p (h d) -> p h d", h=4)
                for h in range(4):
                    add("tensor", lambda e, h=h: e.matmul(psA3[:, h, :], kT[:, h, :], qT[:, h, :], start=True, stop=True),
                        reads=["R5", "R3"], writes=["psA"])
                add("vector", lambda e: e.tensor_tensor(sT, psA3, decT, ALU.mult), reads=["psA", "decT"], writes=["R2"])
                for h in range(4):
                    add("tensor", lambda e, h=h, Vj=Vj: e.matmul(psB3[:, h, :], sT[:, h, :], Vj[:, h, :], start=True, stop=False),
                        reads=["R2", "qkvg2_%d" % j], writes=["psB"])
                    add("tensor", lambda e, h=h: e.matmul(psB3[:, h, :], qdT[:, h, :], state[:, h, :], start=False, stop=True),
                        reads=["R4", "state"], writes=["psB"])
                for h in range(4):
                    add("tensor", lambda e, h=h, Vj=Vj: e.matmul(psO3[:, h, :], kd[:, h, :], Vj[:, h, :], start=True, stop=True),
                        reads=["R6", "qkvg2_%d" % j], writes=["psO"])
                add("vector", lambda e: e.tensor_tensor(state, state, CDt.unsqueeze(2).to_broadcast([128, 4, 128]), ALU.mult),
                    reads=["state", "CD"], writes=["state"])
                add("vector", lambda e: e.tensor_tensor(state, state, psO3, ALU.add), reads=["state", "psO"], writes=["state"])
                s1, s2_, mu, var = rse
                add("scalar", lambda e: e.activation(out=osb, in_=psB3, func=AF.Copy), reads=["psB"], writes=["R0"])
                add("scalar", lambda e: e.activation(out=osq, in_=psB3, func=AF.Square), reads=["psB"], writes=["R1"])
                add("vector", lambda e: e.tensor_reduce(s1, osb, AX.X, ALU.add), reads=["R0"], writes=["s1"])
                add("vector", lambda e: e.tensor_reduce(s2_, osq, AX.X, ALU.add), reads=["R1"], writes=["s2_"])
                add("vector", lambda e: e.tensor_scalar(mu, s1, 1.0 / 128, None, ALU.mult), reads=["s1"], writes=["mu"])
                add("vector", lambda e: e.tensor_tensor(var, mu, mu, ALU.mult), reads=["mu"], writes=["var"])
                add("vector", lambda e: e.scalar_tensor_tensor(var, s2_, 1.0 / 128, var, ALU.mult, ALU.subtract), reads=["s2_", "var"], writes=["var"])
                add("vector", lambda e: e.tensor_scalar(var, var, EPS, None, ALU.add), reads=["var"], writes=["var"])
                add("scalar", lambda e: e.activation(out=var, in_=var, func=AF.Sqrt), reads=["var"], writes=["var"])
                add("vector", lambda e: e.reciprocal(var, var), reads=["var"], writes=["var"])
                add("vector", lambda e: e.tensor_tensor(osb, osb, mu.unsqueeze(2).to_broadcast([128, 4, 128]), ALU.subtract), reads=["R0", "mu"], writes=["R0"])
                add("vector", lambda e: e.tensor_tensor(osb, osb, var.unsqueeze(2).to_broadcast([128, 4, 128]), ALU.mult), reads=["R0", "var"], writes=["R0"])
                osq2 = rt_[1]
                osb2 = rt_[0]
                add("scalar", lambda e, Gj=Gj: e.activation(out=osq2, in_=Gj, func=AF.Silu), reads=["qkvg3_%d" % j], writes=["R1"])
                add("vector", lambda e: e.tensor_tensor(osb2, osb2, osq2, ALU.mult), reads=["R0", "R1"], writes=["R0"])
                for h in range(4):
                    add("tensor", lambda e, h=h: e.transpose(psT[:, h, :], osb[:, h, :], ident), reads=["R0", "ident"], writes=["psT"])
                add("scalar", lambda e, j=j: e.activation(out=yretT[:, j, :].rearrange("p (h d) -> p h d", h=4).bitcast(F32R),
                                                        in_=psT[:, 0:4, :], func=AF.Copy), reads=["psT"], writes=["yretT%d" % j])

            ylr = ylruT.bitcast(F32R)
            yrr = yretT.bitcast(F32R)

            def stage_f(j, wbs):
                for half in range(2):
                    wb, wkey = wbs[half]
                    for kc in range(KD):
                        if kc < 4:
                            lhs = ylr[:, kc, j * 128:(j + 1) * 128]
                            rk = "ylruT"
                        else:
                            lhs = yrr[:, j, (kc - 4) * 128:(kc - 3) * 128]
                            rk = "yretT%d" % j
                        add("tensor", lambda e, lhs=lhs, wb=wb, kc=kc, half=half: e.matmul(
                            psP[:, half * 512:(half + 1) * 512], lhs, wb[:, kc, :], start=(kc == 0), stop=(kc == KD - 1)),
                            reads=[wkey, rk], writes=["psP%d" % half])
                add("vector", lambda e: e.tensor_tensor(xt[j], xt[j], psP, ALU.add), reads=["psP0", "psP1", "xt%d" % j], writes=["xt%d" % j])

            def stage_g(j):
                t = stile * 4 + j
                x1 = xt[j]
                xk = "xt%d" % j
                add("sync", lambda e, t=t, x1=x1: e.dma_start(out=X1_d[t * 128:(t + 1) * 128, :], in_=x1), reads=[xk], writes=["X1d"], dma=True)
                hf = hn
                rms_scale(x1, xk, gffn, "gffn", hf, "hn", "g")
                if debug == "hf":
                    add("sync", lambda e, t=t: e.dma_start(out=dbg_d[t * 128:(t + 1) * 128, :], in_=hf), reads=["hn"], dma=True)
                if debug == "x1":
                    add("sync", lambda e, t=t, x1=x1: e.dma_start(out=dbg_d[t * 128:(t + 1) * 128, :], in_=x1), reads=[xk], dma=True)
                add("scalar", lambda e: e.activation(out=hfb, in_=hf, func=AF.Copy), reads=["hn"], writes=["hfb"])
                for rnd in range(2):
                    for c in range(4):
                        add("tensor", lambda e, c=c, rnd=rnd: e.transpose(psT[:, 4 + c, :], hf[:, (rnd * 4 + c) * 128:(rnd * 4 + c + 1) * 128], ident),
                            reads=["hn", "ident"], writes=["psT2"])
                    add("scalar", lambda e, rnd=rnd: e.activation(out=hfT[:, rnd * 4:(rnd + 1) * 4, :], in_=psT[:, 4:8, :], func=AF.Copy),
                        reads=["psT2"], writes=["hfT"])
                psR = psO[:, 512:584]
                psC = psO[:, 640:704]
                for kc in range(KD):
                    add("tensor", lambda e, kc=kc: e.matmul(psR, hfT[:, kc, :], wrt[:, kc, :], start=(kc == 0), stop=(kc == KD - 1)),
                        reads=["hfT", "wrt"], writes=["psO1"])
                lg = rs[0][:, 0:72]
                goh, ein, mx8, oh1, oh2 = rs[1][:, 0:8], rs[2][:, 0:8], rs[3][:, 0:8], rs[4][:, 0:8], rs[5][:, 0:8]
                tmp64, A1, A2 = rs[6][:, 0:64], rs[7][:, 0:64], rs[1][:, 8:72]
                gmax, nmax, gsum, gw, dd = sm[2], sm[3], sm[4], sm[5], sm[6]
                gl_, el_ = lg[:, 0:8], lg[:, 8:72]
                add("vector", lambda e: e.tensor_tensor(lg, psR, brt, ALU.add), reads=["psO1", "brt"], writes=["lg"])
                add("vector", lambda e: e.tensor_reduce(gmax, gl_, AX.X, ALU.max), reads=["lg"], writes=["gmax"])
                add("vector", lambda e: e.tensor_scalar(goh, gl_, gmax, None, ALU.is_equal), reads=["lg", "gmax"], writes=["goh"])
                add("vector", lambda e: e.tensor_scalar(nmax, gmax, -1.0, None, ALU.mult), reads=["gmax"], writes=["nmax"])
                add("scalar", lambda e: e.activation(out=ein, in_=gl_, func=AF.Exp, bias=nmax, accum_out=gsum), reads=["lg", "nmax"], writes=["ein", "gsum"])
                add("vector", lambda e: e.reciprocal(gw, gsum), reads=["gsum"], writes=["gw"])
                add("vector", lambda e: e.tensor_tensor(tmp64.rearrange("p (g j) -> p g j", g=8), el_.rearrange("p (g j) -> p g j", g=8),
                                                        goh.unsqueeze(2).to_broadcast([128, 8, 8]), ALU.mult), reads=["lg", "goh"], writes=["tmp64"])
                add("vector", lambda e: e.tensor_reduce(ein, tmp64.rearrange("p (g j) -> p j g", g=8), AX.X, ALU.add), reads=["tmp64"], writes=["ein"])
                add("vector", lambda e: e.max(mx8, ein), reads=["ein"], writes=["mx8"])
                add("vector", lambda e: e.tensor_scalar(oh1, ein, mx8[:, 0:1], None, ALU.is_equal), reads=["ein", "mx8"], writes=["oh1"])
                add("vector", lambda e: e.tensor_scalar(oh2, ein, mx8[:, 1:2], None, ALU.is_equal), reads=["ein", "mx8"], writes=["oh2"])
                add("vector", lambda e: e.tensor_tensor(dd, mx8[:, 0:1], mx8[:, 1:2], ALU.subtract), reads=["mx8"], writes=["dd"])
                add("scalar", lambda e: e.activation(out=dd, in_=dd, func=AF.Sigmoid), reads=["dd"], writes=["dd"])
                add("vector", lambda e, t=t: e.tensor_tensor(wsel1[:, t:t + 1], dd, gw, ALU.mult), reads=["dd", "gw"], writes=["wsel1"])
                add("vector", lambda e, t=t: e.tensor_tensor(wsel2[:, t:t + 1], gw, wsel1[:, t:t + 1], ALU.subtract), reads=["gw", "wsel1"], writes=["wsel2"])
                gb = goh.unsqueeze(2).to_broadcast([128, 8, 8])
                add("vector", lambda e: e.tensor_tensor(A1.rearrange("p (g j) -> p g j", g=8), gb, oh1.unsqueeze(1).to_broadcast([128, 8, 8]), ALU.mult),
                    reads=["goh", "oh1"], writes=["A1"])
                add("vector", lambda e: e.tensor_tensor(A2.rearrange("p (g j) -> p g j", g=8), gb, oh2.unsqueeze(1).to_broadcast([128, 8, 8]), ALU.mult),
                    reads=["goh", "oh2"], writes=["A2"])
                add("vector", lambda e: e.tensor_tensor(tmp64, A1, A2, ALU.add), reads=["A1", "A2"], writes=["tmp64"])
                add("tensor", lambda e: e.matmul(psC, U_t, tmp64, start=True, stop=False), reads=["U", "tmp64"], writes=["psO1"])
                add("tensor", lambda e: e.matmul(psC, ones_t, Rcum, start=False, stop=True), reads=["ones", "Rcum"], writes=["psO1"])
                add("vector", lambda e: e.tensor_tensor(Rcum, Rcum, tmp64, ALU.add), reads=["Rcum", "tmp64"], writes=["Rcum"])
                pe_ = rs[6][:, 0:64]
                add("vector", lambda e: e.scalar_tensor_tensor(pe_, psC, float(CAP - 1), eCt, ALU.min, ALU.add), reads=["psO1", "eC", "tmp64"], writes=["tmp64"])
                add("vector", lambda e: e.tensor_tensor(A1, A1, pe_, ALU.mult), reads=["A1", "tmp64"], writes=["A1"])
                add("vector", lambda e: e.tensor_tensor(A2, A2, pe_, ALU.mult), reads=["A2", "tmp64"], writes=["A2"])
                d1f, d2f = sm[7], sm[8]
                add("vector", lambda e: e.tensor_reduce(d1f, A1, AX.X, ALU.add), reads=["A1"], writes=["d1f"])
                add("vector", lambda e: e.tensor_reduce(d2f, A2, AX.X, ALU.add), reads=["A2"], writes=["d2f"])
                add("vector", lambda e, t=t: e.tensor_scalar(dest1i[:, t:t + 1], d1f, float(NE * CAP - 1), None, ALU.min), reads=["d1f"], writes=["dest1"])
                add("vector", lambda e, t=t: e.tensor_scalar(dest2i[:, t:t + 1], d2f, float(NE * CAP - 1), None, ALU.min), reads=["d2f"], writes=["dest2"])
                if debug in ("x1", "hf"):
                    return
                add("gpsimd", lambda e, t=t: e.indirect_dma_start(out=XS_d, out_offset=bass.IndirectOffsetOnAxis(ap=dest1i[:, t:t + 1], axis=0),
                                                                  in_=hfb, in_offset=None), reads=["hfb", "dest1"], writes=["XSd"], dma=True)
                add("gpsimd", lambda e, t=t: e.indirect_dma_start(out=XS_d, out_offset=bass.IndirectOffsetOnAxis(ap=dest2i[:, t:t + 1], axis=0),
                                                                  in_=hfb, in_offset=None), reads=["hfb", "dest2"], writes=["XSd"], dma=True)


            stage_d(0)
            for gi in range(3):
                emit_merged(record(stage_c, gi), record(stage_d, gi + 1))
                if stile == 0:
                    emit_zero(gi)
            wbs = [load_group(w_out_d[half]) for half in range(2)]

            def stage_fg(j):
                stage_f(j, wbs)
                stage_g(j)

            emit_merged(record(stage_c, 3), record(stage_e, 0))
            if stile == 0:
                emit_zero(3)
            add("vector", lambda e: e.tensor_copy(XL[:, :, 0:3], XL[:, :, 512:515]), reads=["XL"], writes=["XL"])
            for j in range(4):
                emit_merged(record(stage_fg, j), record(stage_e, j + 1) if j + 1 < 4 else [])

        if debug in ("x1", "hf"):
            S_.emit(final_wait_ops=[op for op in S_.all_ops if op.is_dma])
            return nc
        S_.barrier(lambda e: e.memset(bscr, 0.0))
        off[0] = PL_BASE
        W1b = [A(KD * 256, KD, 512, bf=True) for _ in range(2)]
        W3b = [A(KD * 256, KD, 512, bf=True) for _ in range(2)]
        W2b = [A(4 * 512, 4, 1024, bf=True) for _ in range(2)]
        xs = [A(D, 2, D, bf=True) for _ in range(2)]
        xsT = A(KD * 128, KD, 256, bf=True)
        gT = A(4 * 128, 4, 256, bf=True)
        silt = A(256)
        yb = [A(D) for _ in range(2)]
        psTb = [psT_t[:, 0:512].bitcast(BF16).rearrange("p (a b) -> p a b", a=8),
                psT_t[:, 512:1024].bitcast(BF16).rearrange("p (a b) -> p a b", a=8)]
        psTk = ["psT", "psT2"]
        hbanks = [(psA, "psA"), (psB, "psB"), (psO[:, 0:512], "psO0"), (psO[:, 512:1024], "psO1")]
        ybanks = [(psP[:, 0:512], "psP0"), (psP[:, 512:1024], "psP1")]
        silt2 = [silt, A(256)]

        def b_loads(ex):
            k = ex % 2
            fl = lambda ap: ap.rearrange("p c n -> p (c n)")
            add("gpsimd", lambda e: e.dma_start(out=fl(W1b[k]), in_=w1_d[ex], max_dma_last_dim=8192), writes=["W1b%d" % k], dma=True)
            add("gpsimd", lambda e: e.dma_start(out=fl(W3b[k]), in_=w3_d[ex], max_dma_last_dim=8192), writes=["W3b%d" % k], dma=True)
            add("gpsimd", lambda e: e.dma_start(out=fl(W2b[k]), in_=w2_d[ex], max_dma_last_dim=8192), writes=["W2b%d" % k], dma=True)
            add("sync", lambda e: e.dma_start(out=xs[k], in_=XS_d[ex * CAP:(ex + 1) * CAP, :].rearrange("(b p) d -> p b d", p=128)),
                reads=["XSd"], writes=["xs%d" % k], dma=True)

        ycnt = [0]

        def b_compute(ex):
            k = ex % 2
            for blk in range(2):
                for c in range(KD):
                    add("tensor", lambda e, c=c, blk=blk: e.transpose(psTb[blk][:, c, :], xs[k][:, blk, c * 128:(c + 1) * 128], identb),
                        reads=["xs%d" % k, "identb"], writes=[psTk[blk]])
                if blk == 0:
                    add("scalar", lambda e: e.activation(out=xsT[:, :, 0:128], in_=psTb[0], func=AF.Copy), reads=[psTk[0]], writes=["xsT"])
                else:
                    add("vector", lambda e: e.tensor_copy(xsT[:, :, 128:256], psTb[1]), reads=[psTk[1]], writes=["xsT"])
            for f in range(4):
                (h1, h1k), (h3, h3k) = hbanks[2 * (f % 2)], hbanks[2 * (f % 2) + 1]
                sl = silt2[f % 2]
                slk = "silt%d" % (f % 2)
                for kc in range(KD):
                    add("tensor", lambda e, f=f, kc=kc, h1=h1: e.matmul(h1[:, 0:256], W1b[k][:, kc, f * 128:(f + 1) * 128], xsT[:, kc, :],
                                                                     start=(kc == 0), stop=(kc == KD - 1)), reads=["W1b%d" % k, "xsT"], writes=[h1k])
                for kc in range(KD):
                    add("tensor", lambda e, f=f, kc=kc, h3=h3: e.matmul(h3[:, 0:256], W3b[k][:, kc, f * 128:(f + 1) * 128], xsT[:, kc, :],
                                                                     start=(kc == 0), stop=(kc == KD - 1)), reads=["W3b%d" % k, "xsT"], writes=[h3k])
                add("scalar", lambda e, h1=h1, sl=sl: e.activation(out=sl, in_=h1[:, 0:256], func=AF.Silu), reads=[h1k], writes=[slk])
                add("vector", lambda e, f=f, h3=h3, sl=sl: e.tensor_tensor(gT[:, f, :], sl, h3[:, 0:256], ALU.mult), reads=[slk, h3k], writes=["gT%d" % f])
            for blk in range(2):
                yk = ycnt[0] % 2
                ycnt[0] += 1
                for half in range(2):
                    yp, ypk = ybanks[half]
                    for f in range(4):
                        add("tensor", lambda e, yp=yp, half=half, f=f, blk=blk: e.matmul(
                            yp, gT[:, f, blk * 128:(blk + 1) * 128], W2b[k][:, f, half * 512:(half + 1) * 512],
                            start=(f == 0), stop=(f == 3)), reads=["gT%d" % f, "W2b%d" % k], writes=[ypk])
                    if half == 0:
                        add("scalar", lambda e, yk=yk, yp=yp: e.activation(out=yb[yk][:, 0:512], in_=yp, func=AF.Copy), reads=[ypk], writes=["yb%d" % yk])
                    else:
                        add("vector", lambda e, yk=yk, yp=yp: e.tensor_copy(yb[yk][:, 512:1024], yp), reads=[ypk], writes=["yb%d" % yk])
                r0 = ex * CAP + blk * 128
                add("sync", lambda e, yk=yk, r0=r0: e.dma_start(out=Y_d[r0:r0 + 128, :], in_=yb[yk]), reads=["yb%d" % yk], writes=["Yd"], dma=True)

        roff[0] = RO_BASE
        WG = A(KD * D, KD, D, ro=True)
        WP = A(2 * D, 2, D, ro=True)
        gple = A(D)
        gfin = A(D)
        bple = A(D)
        x1c = [A(D) for _ in range(2)]
        yac = [A(D) for _ in range(2)]
        ybc = [A(D) for _ in range(2)]
        pt = [A(256) for _ in range(2)]
        hp = A(D)
        hpT2 = [A(KD * 128, KD, 128, ro=True) for _ in range(2)]
        pT2 = [A(256, 2, 128, ro=True) for _ in range(2)]
        gate = A(D)
        outt = [A(D) for _ in range(2)]
        sm = [A(1) for _ in range(4)]
        add("gpsimd", lambda e: e.dma_start(out=WG.bitcast(F32R), in_=wg_d.rearrange("(c p) n -> p c n", p=128)), writes=["WG"], dma=True)
        add("gpsimd", lambda e: e.dma_start(out=WP.bitcast(F32R), in_=wp_d.rearrange("(c p) n -> p c n", p=128)), writes=["WP"], dma=True)
        for (dst, src, key) in [(gple, gple_d, "gple"), (gfin, gfin_d, "gfin"), (bple, bple_d, "bple")]:
            add("sync", lambda e, dst=dst, src=src: e.dma_start(out=dst, in_=src), writes=[key], dma=True)
        WGr = WG.bitcast(F32R)
        WPr = WP.bitcast(F32R)
        b_loads(0)
        for ex in range(NE):
            if ex + 1 < NE:
                b_loads(ex + 1)
            b_compute(ex)

        def c_loads(t):
            k = t % 2
            add("sync", lambda e: e.dma_start(out=x1c[k], in_=X1_d[t * 128:(t + 1) * 128, :]), reads=["X1d"], writes=["x1c%d" % k], dma=True)
            add("sync", lambda e: e.dma_start(out=pt[k], in_=p_d[t * 128:(t + 1) * 128, :]), writes=["pt%d" % k], dma=True)
            add("gpsimd", lambda e: e.indirect_dma_start(out=yac[k], out_offset=None, in_=Y_d,
                                                         in_offset=bass.IndirectOffsetOnAxis(ap=dest1i[:, t:t + 1], axis=0)),
                reads=["Yd", "dest1"], writes=["yac%d" % k], dma=True)
            add("gpsimd", lambda e: e.indirect_dma_start(out=ybc[k], out_offset=None, in_=Y_d,
                                                         in_offset=bass.IndirectOffsetOnAxis(ap=dest2i[:, t:t + 1], axis=0)),
                reads=["Yd", "dest2"], writes=["ybc%d" % k], dma=True)

        def c_stage1(t):
            k = t % 2
            x2 = x1c[k]
            xk = "x1c%d" % k
            hpT, pT_ = hpT2[k], pT2[k]
            add("vector", lambda e: e.scalar_tensor_tensor(x2, yac[k], wsel1[:, t:t + 1], x2, ALU.mult, ALU.add),
                reads=["yac%d" % k, "wsel1", xk], writes=[xk])
            add("vector", lambda e: e.scalar_tensor_tensor(x2, ybc[k], wsel2[:, t:t + 1], x2, ALU.mult, ALU.add),
                reads=["ybc%d" % k, "wsel2", xk], writes=[xk])
            rms_scale(x2, xk, gple, "gple", hp, "hp", "c", si=0)
            transpose8(hp, "hp", hpT, "hpT%d" % k)
            for c in range(2):
                add("tensor", lambda e, c=c: e.transpose(psA[:, c * 128:(c + 1) * 128], pt[k][:, c * 128:(c + 1) * 128], ident),
                    reads=["pt%d" % k, "ident"], writes=["psA"])
            add("vector", lambda e: e.tensor_copy(pT_.bitcast(F32R), psA[:, 0:256].rearrange("p (a b) -> p a b", a=2)), reads=["psA"], writes=["pT_%d" % k])

        def c_stage2(t):
            k = t % 2
            x2 = x1c[k]
            xk = "x1c%d" % k
            hpTr = hpT2[k].bitcast(F32R)
            pTr = pT2[k].bitcast(F32R)
            for half in range(2):
                for kc in range(KD):
                    add("tensor", lambda e, half=half, kc=kc: e.matmul(psO[:, half * 512:(half + 1) * 512], hpTr[:, kc, :], WGr[:, kc, half * 512:(half + 1) * 512],
                                                                      start=(kc == 0), stop=(kc == KD - 1)), reads=["hpT%d" % k, "WG"], writes=["psO0", "psO1"])
            for half in range(2):
                for kc in range(2):
                    add("tensor", lambda e, half=half, kc=kc: e.matmul(psP[:, half * 512:(half + 1) * 512], pTr[:, kc, :], WPr[:, kc, half * 512:(half + 1) * 512],
                                                                      start=(kc == 0), stop=(kc == 1)), reads=["pT_%d" % k, "WP"], writes=["psP0", "psP1"])
            add("vector", lambda e: e.tensor_tensor(gate, psO, bple, ALU.add), reads=["psO0", "psO1", "bple"], writes=["gate"])
            add("scalar", lambda e: e.activation(out=gate, in_=gate, func=AF.Sigmoid), reads=["gate"], writes=["gate"])
            add("vector", lambda e: e.tensor_tensor(gate, gate, psP, ALU.mult), reads=["gate", "psP0", "psP1"], writes=["gate"])
            add("vector", lambda e: e.tensor_tensor(x2, x2, gate, ALU.add), reads=["gate", xk], writes=[xk])
            rms_scale(x2, xk, gfin, "gfin", outt[k], "outt%d" % k, "f", si=2)
            add("sync", lambda e: e.dma_start(out=out_d[t * 128:(t + 1) * 128, :], in_=outt[k]), reads=["outt%d" % k], dma=True)

        c_loads(0)
        if NT > 1:
            c_loads(1)
        c_stage1(0)
        for t in range(NT):
            ch2 = record(c_stage2, t)
            ch1 = record(c_stage1, t + 1) if t + 1 < NT else []
            emit_merged(ch2, ch1)
            if t + 2 < NT:
                c_loads(t + 2)

        S_.emit(final_wait_ops=[op for op in S_.all_ops if op.is_dma])
    return nc


def core_inputs(b, S, consts, shared, x, p, g_mix, w_in, conv_w, conv_b, lru_wa, lru_ba, lru_wx, lru_bx, lru_lambda, w_out,
                g_ffn, w_router_group, b_router_group, w_router_expert, b_router_expert, w1, w3, w2,
                g_ple, w_ple_gate, b_ple_gate, w_ple_proj, g_final):
    f = lambda a: np.ascontiguousarray(a, dtype=np.float32)
    m = dict(
        x=f(x[b]), p=f(p[0, b]), w_in=shared["w_in"], w_out=shared["w_out"], w1=shared["w1"], w3=shared["w3"], w2=shared["w2"],
        w_ple_gate=f(w_ple_gate[0]), w_ple_proj=f(w_ple_proj[0]),
        w_rt=f(np.concatenate([w_router_group[0], w_router_expert[0]], axis=1)),
        b_rt=bc128(np.concatenate([b_router_group[0], b_router_expert[0]], axis=0)),
        g_mix=bc128(g_mix[0]), g_ffn=bc128(g_ffn[0]), g_ple=bc128(g_ple[0]), g_final=bc128(g_final), b_ple=bc128(b_ple_gate[0]),
        conv_w=f(np.transpose(np.asarray(conv_w[0]).reshape(4, 4, 128), (2, 1, 0))),
        conv_b=chan_major(conv_b[0]),
        lru_wa=block_diag(np.asarray(lru_wa[0])), lru_wx=block_diag(np.asarray(lru_wx[0])),
        lru_ba=chan_major(np.asarray(lru_ba[0]).reshape(-1)), lru_bx=chan_major(np.asarray(lru_bx[0]).reshape(-1)),
        lru_lam=chan_major(lru_lambda[0]),
    )
    m.update(consts)
    return m


def shared_layout(w_in, w_out, w1, w3, w2):
    def pmajor(w):
        lead = w.shape[:-2]
        C = w.shape[-2] // 128
        n = w.shape[-1]
        v = np.asarray(w, np.float32).reshape(*lead, C, 128, n)
        v = np.moveaxis(v, -3, -2)
        return np.ascontiguousarray(v).reshape(*lead, 128, C * n)
    w_in_g = np.stack([np.asarray(w_in[0])[:, g * 512:(g + 1) * 512] for g in range(6)], axis=0)
    w_out_g = np.stack([np.asarray(w_out[0])[:, h * 512:(h + 1) * 512] for h in range(2)], axis=0)
    return dict(w_in=pmajor(w_in_g), w_out=pmajor(w_out_g), w1=pmajor(w1[0]), w3=pmajor(w3[0]), w2=pmajor(w2[0]))


_NC_CACHE = {}


def kernel(**inputs):
    inputs = {k: np.asarray(v) for k, v in inputs.items()}
    x = inputs["x"]
    B, S, _ = x.shape
    consts = make_consts(S)
    if S not in _NC_CACHE:
        _NC_CACHE[S] = build_nc(S)
    nc = _NC_CACHE[S]
    shared = shared_layout(inputs["w_in"], inputs["w_out"], inputs["w1"], inputs["w3"], inputs["w2"])
    in_maps = [core_inputs(b, S, consts, shared, **inputs) for b in range(B)]
    res = run_bass_kernel_spmd(nc, in_maps, core_ids=list(range(B)))
    return np.stack([np.asarray(r["out"]) for r in res.results], axis=0).astype(np.float32)
```

```python
import contextlib
import numpy as np
import concourse.bass as bass
import concourse.mybir as mybir
from concourse.bass_utils import run_bass_kernel_spmd

F32 = mybir.dt.float32
F32R = mybir.dt.float32r
I32 = mybir.dt.int32
BF16 = mybir.dt.bfloat16
AF = mybir.ActivationFunctionType
ALU = mybir.AluOpType
AX = mybir.AxisListType

D = 1024
KD = 8
NE = 64
DE = 512
CAP = 256
EPS = 1e-6
ENGINES = ("tensor", "vector", "scalar", "gpsimd", "sync")


class Op:
    __slots__ = ("eng", "fn", "is_dma", "needs_inc", "inc_index", "waits", "order_deps",
                 "dma_sem", "dma_val", "dma_prev", "idx", "cost", "xfer", "succ", "ndeps",
                 "finish", "ready")

    def __init__(self, eng, fn, is_dma):
        self.eng = eng
        self.fn = fn
        self.is_dma = is_dma
        self.needs_inc = False
        self.inc_index = None
        self.waits = []
        self.order_deps = []
        self.dma_sem = None
        self.dma_val = None
        self.dma_prev = 0
        self.succ = []
        self.ndeps = 0
        self.finish = 0.0
        self.ready = 0.0
        self.cost = 0.1
        self.xfer = 0.0


class _Mock:
    def __init__(self):
        self.calls = []

    def __getattr__(self, name):
        def f(*a, **k):
            self.calls.append((name, a, k))
            return self
        return f


def _fsize(ap):
    try:
        return int(ap.free_size())
    except Exception:
        return 256


def _estimate(op):
    m = _Mock()
    try:
        op.fn(m)
        name, a, k = m.calls[0]
    except Exception:
        return
    if op.is_dma:
        try:
            o = k.get("out", a[0] if a else None)
            i = k.get("in_", None)
            nb = min(o.nbytes(), i.nbytes()) if name == "indirect_dma_start" else max(o.nbytes(), i.nbytes())
        except Exception:
            nb = 65536
        op.xfer = nb / 300e3
        op.cost = 1.2 if op.eng == "gpsimd" else 0.08
        return
    if op.eng == "tensor":
        if name == "matmul":
            n = _fsize(a[2])
            mult = 4.0 if a[1].dtype == F32 else 1.0
            op.cost = max(n * mult, 64.0) / 2000.0
        else:
            op.cost = 0.13
    elif op.eng == "vector":
        o = k.get("out", a[0] if a else None)
        n = _fsize(o)
        if name == "tensor_reduce" or name == "max":
            n = _fsize(a[1])
        if name == "tensor_tensor_scan":
            n *= 2
        op.cost = (max(n, 64) + 60) / 960.0
    elif op.eng == "scalar":
        o = k.get("out", a[0] if a else None)
        op.cost = (max(_fsize(o), 64) + 250) / 1400.0
    else:
        op.cost = 0.2


class Sched:
    def __init__(self, nc, n_dma_sems=48):
        self.nc = nc
        self.last_writer = {}
        self.readers = {}
        self.n_dma_sems = n_dma_sems
        self.all_ops = []
        self.barrier_op = None
        self.last_of = {e: None for e in ENGINES}

    def add(self, eng, fn, reads=(), writes=(), dma=False):
        op = Op(eng, fn, dma)
        op.idx = len(self.all_ops)
        deps = []
        if self.barrier_op is not None:
            deps.append(self.barrier_op)
        for k in reads:
            w = self.last_writer.get(k)
            if w is not None:
                deps.append(w)
        for k in writes:
            w = self.last_writer.get(k)
            if w is not None:
                deps.append(w)
            deps.extend(self.readers.get(k, ()))
        seen = set()
        for d in deps:
            if id(d) in seen or d is op:
                continue
            seen.add(id(d))
            if (not d.is_dma) and d.eng == eng and eng == "tensor":
                op.order_deps.append(d)
            else:
                op.waits.append(d)
        for k in writes:
            self.last_writer[k] = op
            self.readers[k] = []
        for k in reads:
            if k not in writes:
                self.readers.setdefault(k, []).append(op)
        self.all_ops.append(op)
        self.last_of[eng] = op
        return op

    def barrier(self, fn):
        op = self.add("vector", fn, writes=["__barrier__"])
        have = set(id(w) for w in op.waits)
        for d in self.all_ops[:-1]:
            if id(d) not in have:
                op.waits.append(d)
        self.barrier_op = op
        self.last_writer = {}
        self.readers = {}
        return op

    def schedule(self):
        import heapq
        ops = self.all_ops
        for op in ops:
            _estimate(op)
            op.ndeps = 0
            op.succ = []
        for op in ops:
            for d in op.waits:
                d.succ.append(op)
                op.ndeps += 1
            for d in op.order_deps:
                d.succ.append(op)
                op.ndeps += 1
        bl = {}
        for op in reversed(ops):
            m = 0.0
            for sc in op.succ:
                v = bl[id(sc)] + (0.0 if (sc.eng == op.eng and not op.is_dma) else 0.35)
                if v > m:
                    m = v
            bl[id(op)] = m + op.cost + ((op.xfer + 2.0) if op.is_dma else 0.0)
        future = {e: [] for e in ENGINES}
        avail = {e: [] for e in ENGINES}
        free_at = {e: 0.0 for e in ENGINES}
        dma_free = [0.0]
        order = {e: [] for e in ENGINES}
        for op in ops:
            if op.ndeps == 0:
                heapq.heappush(future[op.eng], (0.0, op.idx, op))
        remaining = len(ops)
        while remaining:
            best = None
            for e in ENGINES:
                fu, av = future[e], avail[e]
                while fu and fu[0][0] <= free_at[e]:
                    r, i, o = heapq.heappop(fu)
                    heapq.heappush(av, (-bl[id(o)], i, o))
                if av:
                    cand = (free_at[e], av[0][1], e, True)
                elif fu:
                    cand = (fu[0][0], fu[0][1], e, False)
                else:
                    continue
                if best is None or cand < best:
                    best = cand
            assert best is not None, "scheduler stuck (dependency cycle?)"
            t, _, e, from_av = best
            if from_av:
                _, _, op = heapq.heappop(avail[e])
            else:
                _, _, op = heapq.heappop(future[e])
            start = max(t, free_at[e])
            free_at[e] = start + op.cost
            if op.is_dma:
                s0 = max(start + op.cost, dma_free[0])
                dma_free[0] = s0 + op.xfer
                op.finish = s0 + op.xfer + 2.0
            else:
                op.finish = start + op.cost
            order[e].append(op)
            remaining -= 1
            for sc in op.succ:
                lat = 0.0 if (sc.eng == op.eng and not op.is_dma) else 0.35
                r = op.finish + lat
                if r > sc.ready:
                    sc.ready = r
                sc.ndeps -= 1
                if sc.ndeps == 0:
                    heapq.heappush(future[sc.eng], (sc.ready, sc.idx, sc))
        self.est_time = max(op.finish for op in ops)
        return order

    def emit(self, final_wait_ops=()):
        nc = self.nc
        order = self.schedule()
        pos = {}
        for e in ENGINES:
            for i, op in enumerate(order[e]):
                pos[id(op)] = i
        for op in self.all_ops:
            if len(op.waits) > 256:
                keep = {}
                dm = []
                for d in op.waits:
                    if d.is_dma:
                        dm.append(d)
                    elif d.eng not in keep or pos[id(d)] > pos[id(keep[d.eng])]:
                        keep[d.eng] = d
                op.waits = dm + list(keep.values())
        for op in self.all_ops:
            for d in op.waits:
                if not d.is_dma:
                    d.needs_inc = True
        half = self.n_dma_sems // 2
        for e in ENGINES:
            c = 0
            rr = 0
            counts = [0] * self.n_dma_sems
            for op in order[e]:
                if op.is_dma:
                    base = half if e == "gpsimd" else 0
                    s = base + rr
                    rr = (rr + 1) % half
                    op.dma_sem = s
                    op.dma_prev = counts[s]
                    counts[s] += 16
                    op.dma_val = counts[s]
                elif op.needs_inc:
                    c += 1
                    op.inc_index = c
        with contextlib.ExitStack() as st:
            esem = {e: st.enter_context(nc.semaphore("es_" + e)) for e in ENGINES}
            dsem = [st.enter_context(nc.semaphore("ds_%d" % i)) for i in range(self.n_dma_sems)]
            block = st.enter_context(nc.Block())
            sched = self

            def run_engine(e, engobj):
                waited_e = {x: 0 for x in ENGINES}
                waited_d = [0] * sched.n_dma_sems
                for op in order[e]:
                    need_e = {}
                    need_d = {}
                    for d in op.waits:
                        if d.is_dma:
                            if d.dma_val > need_d.get(d.dma_sem, 0):
                                need_d[d.dma_sem] = d.dma_val
                        else:
                            if d.inc_index > need_e.get(d.eng, 0):
                                need_e[d.eng] = d.inc_index
                    if op.is_dma and op.dma_prev > need_d.get(op.dma_sem, 0):
                        need_d[op.dma_sem] = op.dma_prev
                    for pe, v in need_e.items():
                        if v > waited_e[pe]:
                            engobj.wait_ge(esem[pe], v)
                            waited_e[pe] = v
                    for s, v in need_d.items():
                        if v > waited_d[s]:
                            engobj.wait_ge(dsem[s], v)
                            waited_d[s] = v
                    ins = op.fn(engobj)
                    if op.is_dma:
                        ins.then_inc(dsem[op.dma_sem], 16)
                    elif op.needs_inc:
                        ins.then_inc(esem[e], 1)
                if e == "sync":
                    for d in final_wait_ops:
                        if d.dma_val > waited_d[d.dma_sem]:
                            engobj.wait_ge(dsem[d.dma_sem], d.dma_val)
                            waited_d[d.dma_sem] = d.dma_val

            @block.tensor
            def _(t):
                run_engine("tensor", t)

            @block.vector
            def _(v):
                run_engine("vector", v)

            @block.scalar
            def _(s):
                run_engine("scalar", s)

            @block.gpsimd
            def _(g):
                run_engine("gpsimd", g)

            @block.sync
            def _(sy):
                run_engine("sync", sy)


def make_consts(S):
    H, C, Dh = 4, 128, 128
    log_g = np.log(1.0 - 2.0 ** (-5.0 - np.arange(H, dtype=np.float64)))
    idx = np.arange(C, dtype=np.float64)
    diff = idx[:, None] - idx[None, :]
    decay = np.where(diff >= 0, np.exp(np.maximum(diff, 0.0)[None] * log_g[:, None, None]), 0.0)
    decayT = np.transpose(decay, (2, 0, 1))
    q_decay = np.exp((idx + 1.0)[None, :] * log_g[:, None])
    k_decay = np.exp((C - 1.0 - idx)[None, :] * log_g[:, None])
    chunk_decay = np.exp(C * log_g)
    QD = np.broadcast_to(q_decay[None, :, :], (128, H, C))
    KDc = np.transpose(k_decay, (1, 0))
    CDc = np.broadcast_to(chunk_decay[None, :], (128, H))
    half = Dh // 2
    inv = 10000.0 ** (-np.arange(half, dtype=np.float32).astype(np.float64) / half)
    pos = np.arange(S, dtype=np.float64)
    ang = (pos[:, None].astype(np.float32) * inv[None, :].astype(np.float32)).astype(np.float64)
    cos = np.cos(ang)
    sin = np.sin(ang)
    cosF = np.concatenate([cos, cos], axis=1)
    sinF = np.concatenate([-sin, sin], axis=1)
    sc = Dh ** -0.5
    rope = np.stack([cosF, sinF, cosF * sc, sinF * sc], axis=1)
    U = np.triu(np.ones((128, 128)), k=1)
    eC = np.broadcast_to((np.arange(NE) * CAP)[None, :], (128, NE))
    f = lambda a: np.ascontiguousarray(a, dtype=np.float32)
    return dict(c_ident=f(np.eye(128)), c_decayT=f(decayT), c_QD=f(QD), c_KD=f(KDc), c_CD=f(CDc),
                c_rope=f(rope), c_U=f(U), c_ones=f(np.ones((128, 128))), c_eC=f(eC))


def bc128(v):
    v = np.asarray(v, dtype=np.float32).reshape(1, -1)
    return np.ascontiguousarray(np.broadcast_to(v, (128, v.shape[1])))


def chan_major(v):
    return np.ascontiguousarray(np.asarray(v, np.float32).reshape(4, 128).T)


def block_diag(w):
    out = np.zeros((128, 4, 128), np.float32)
    for c in range(4):
        for hh in range(2):
            out[hh * 64:(hh + 1) * 64, c, hh * 64:(hh + 1) * 64] = w[2 * c + hh]
    return out


def build_nc(S, debug=None, stop_after=None):
    NT = S // 128
    NST = S // 512
    nc = bass.Bass("TRN2", target_bir_lowering=False)

    def din(name, shape, dt=F32):
        return nc.dram_tensor(name, list(shape), dt, kind="ExternalInput").ap()

    x_d = din("x", [S, D])
    p_d = din("p", [S, 256])
    w_in_d = din("w_in", [6, 128, KD * 512])
    w_out_d = din("w_out", [2, 128, KD * 512])
    w1_d = din("w1", [NE, 128, KD * DE])
    w3_d = din("w3", [NE, 128, KD * DE])
    w2_d = din("w2", [NE, 128, 4 * D])
    wg_d = din("w_ple_gate", [D, D])
    wp_d = din("w_ple_proj", [256, D])
    wrt_d = din("w_rt", [D, 72])
    brt_d = din("b_rt", [128, 72])
    gmix_d = din("g_mix", [128, D])
    gffn_d = din("g_ffn", [128, D])
    gple_d = din("g_ple", [128, D])
    gfin_d = din("g_final", [128, D])
    bple_d = din("b_ple", [128, D])
    cw_d = din("conv_w", [128, 4, 4])
    cb_d = din("conv_b", [128, 4])
    wa_d = din("lru_wa", [128, 4, 128])
    wx_d = din("lru_wx", [128, 4, 128])
    ba_d = din("lru_ba", [128, 4])
    bx_d = din("lru_bx", [128, 4])
    lam_d = din("lru_lam", [128, 4])
    ident_d = din("c_ident", [128, 128])
    decT_d = din("c_decayT", [128, 4, 128])
    QD_d = din("c_QD", [128, 4, 128])
    KD_d = din("c_KD", [128, 4])
    CD_d = din("c_CD", [128, 4])
    rope_d = din("c_rope", [S, 4, 128])
    U_d = din("c_U", [128, 128])
    ones_d = din("c_ones", [128, 128])
    eC_d = din("c_eC", [128, NE])
    out_d = nc.dram_tensor("out", [S, D], F32, kind="ExternalOutput").ap()
    X1_d = nc.dram_tensor("X1s", [S, D], F32, kind="Internal").ap()
    XS_d = nc.dram_tensor("XSs", [NE * CAP, D], BF16, kind="Internal").ap()
    Y_d = nc.dram_tensor("Ys", [NE * CAP, D], F32, kind="Internal").ap()
    dbg_d = None
    if debug is not None:
        dbg_d = nc.dram_tensor("dbg", [S, D], F32, kind="ExternalOutput").ap()

    with contextlib.ExitStack() as st:
        RO_BASE, RO_SIZE = 0, 16384
        ARENA = 52800 - RO_SIZE
        arena = st.enter_context(nc.sbuf_tensor("arena", [128, ARENA], F32))
        arena_ro = st.enter_context(nc.sbuf_tensor("arena_ro", [128, RO_SIZE], F32))
        PL_BASE = 400
        off = [0]
        roff = [RO_BASE]

        def A(n, *shape, ro=False, bf=False):
            if ro:
                o = roff[0]
                roff[0] += n
                assert roff[0] <= RO_BASE + RO_SIZE, ("ro overflow", roff[0])
            else:
                o = off[0]
                off[0] += n
                assert off[0] <= ARENA, ("arena overflow", o, off[0])
            ap = (arena_ro if ro else arena)[:, o:o + n]
            if bf:
                ap = ap.bitcast(BF16)
            if len(shape) == 2:
                ap = ap.rearrange("p (a b) -> p a b", a=shape[0])
            elif len(shape) == 3:
                ap = ap.rearrange("p (a b c) -> p a b c", a=shape[0], b=shape[1])
            return ap

        def psum(name, n):
            return st.enter_context(nc.psum_tensor(name, [128, n], F32))

        psT_t = psum("psT", 1024)
        psO_t = psum("psO", 1024)
        psP_t = psum("psP", 1024)
        psA_t = psum("psA", 512)
        psB_t = psum("psB", 512)
        psT = psT_t[:, :].rearrange("p (a b) -> p a b", a=8)
        psO = psO_t[:, :]
        psP = psP_t[:, :]
        psA = psA_t[:, :]
        psB = psB_t[:, :]

        S_ = Sched(nc)
        rec = [None]

        def add(eng, fn, reads=(), writes=(), dma=False):
            if rec[0] is not None:
                rec[0].append((eng, fn, tuple(reads), tuple(writes), dma))
                return None
            return S_.add(eng, fn, reads=reads, writes=writes, dma=dma)

        def record(f, *args):
            rec[0] = []
            f(*args)
            out = rec[0]
            rec[0] = None
            return out

        def emit_merged(*chains):
            chains = [c for c in chains if c]
            idx = [0] * len(chains)
            while True:
                live = [i for i in range(len(chains)) if idx[i] < len(chains[i])]
                if not live:
                    break
                i = min(live, key=lambda i: idx[i] / len(chains[i]))
                eng, fn, rd, wr, dma = chains[i][idx[i]]
                idx[i] += 1
                S_.add(eng, fn, reads=rd, writes=wr, dma=dma)

        ident = A(128)
        dest1 = A(NT)
        dest2 = A(NT)
        wsel1 = A(NT)
        wsel2 = A(NT)
        dest1i = dest1.bitcast(I32)
        dest2i = dest2.bitcast(I32)
        bscr = A(1)
        identb = A(64, bf=True)
        persist_end = off[0]
        assert persist_end <= PL_BASE
        off[0] = PL_BASE

        add("sync", lambda e: e.dma_start(out=ident, in_=ident_d), writes=["ident"], dma=True)
        add("vector", lambda e: e.tensor_copy(identb, ident), reads=["ident"], writes=["identb"])

        U_t = A(128)
        ones_t = A(128)
        decT = A(512, 4, 128)
        QDt = A(512, 4, 128)
        KDt = A(4)
        CDt = A(4)
        eCt = A(NE)
        gmix = A(D)
        gffn = A(D)
        cw = A(16, 4, 4)
        cb = A(4)
        WA = A(512, 4, 128)
        WX = A(512, 4, 128)
        ba = A(4)
        bx = A(4)
        lam = A(4)
        cneg = A(4)
        c2 = A(4)
        wrt = A(KD * 72, KD, 72)
        brt = A(72)
        hstate = A(4)
        Rcum = A(NE)
        state = A(512, 4, 128)
        tmp4 = [A(4) for _ in range(6)]

        for (dst, src, key) in [(U_t, U_d, "U"), (ones_t, ones_d, "ones"), (decT, decT_d, "decT"),
                                (QDt, QD_d, "QD"), (KDt, KD_d, "KD"), (CDt, CD_d, "CD"), (eCt, eC_d, "eC"),
                                (gmix, gmix_d, "gmix"), (gffn, gffn_d, "gffn"), (cw, cw_d, "cw"), (cb, cb_d, "cb"),
                                (WA, wa_d, "WA"), (WX, wx_d, "WX"), (ba, ba_d, "ba"), (bx, bx_d, "bx"),
                                (lam, lam_d, "lam"), (brt, brt_d, "brt")]:
            add("sync", lambda e, dst=dst, src=src: e.dma_start(out=dst, in_=src), writes=[key], dma=True)
        add("sync", lambda e: e.dma_start(out=wrt, in_=wrt_d.rearrange("(c p) n -> p c n", p=128)), writes=["wrt"], dma=True)
        add("vector", lambda e: e.memset(hstate, 0.0), writes=["hstate"])
        add("vector", lambda e: e.memset(Rcum, 0.0), writes=["Rcum"])
        add("vector", lambda e: e.memset(state, 0.0), writes=["state"])

        ya, za, sa, s2, acc, t0 = tmp4
        add("vector", lambda e: e.tensor_scalar(ya, lam, -1.0, None, ALU.mult), reads=["lam"], writes=["ya"])
        add("vector", lambda e: e.tensor_tensor(za, ya, lam, ALU.min), reads=["ya", "lam"], writes=["za"])
        add("scalar", lambda e: e.activation(out=za, in_=za, func=AF.Exp), reads=["za"], writes=["za"])
        add("vector", lambda e: e.tensor_scalar(sa, za, 2.0, None, ALU.add), reads=["za"], writes=["sa"])
        add("vector", lambda e: e.reciprocal(sa, sa), reads=["sa"], writes=["sa"])
        add("vector", lambda e: e.tensor_tensor(sa, sa, za, ALU.mult), reads=["sa", "za"], writes=["sa"])
        add("vector", lambda e: e.tensor_tensor(s2, sa, sa, ALU.mult), reads=["sa"], writes=["s2"])
        add("vector", lambda e: e.memset(acc, 1.0 / 15.0), writes=["acc"])
        for k in (13, 11, 9, 7, 5, 3, 1):
            add("vector", lambda e: e.tensor_tensor(acc, acc, s2, ALU.mult), reads=["acc", "s2"], writes=["acc"])
            add("vector", lambda e, k=k: e.tensor_scalar(acc, acc, 1.0 / k, None, ALU.add), reads=["acc"], writes=["acc"])
        add("vector", lambda e: e.tensor_tensor(acc, acc, sa, ALU.mult), reads=["acc", "sa"], writes=["acc"])
        add("vector", lambda e: e.tensor_scalar(t0, ya, 0.0, None, ALU.max), reads=["ya"], writes=["t0"])
        add("vector", lambda e: e.scalar_tensor_tensor(t0, acc, 2.0, t0, ALU.mult, ALU.add), reads=["acc", "t0"], writes=["t0"])
        add("vector", lambda e: e.tensor_scalar(cneg, t0, -8.0, None, ALU.mult), reads=["t0"], writes=["cneg"])
        add("vector", lambda e: e.tensor_scalar(c2, t0, -16.0, None, ALU.mult), reads=["t0"], writes=["c2"])

        setup_end = off[0]
        wbuf = [A(KD * 512, KD, 512, ro=True) for _ in range(2)]
        xt = [A(D) for _ in range(4)]
        hn = A(D)
        hT = A(KD * 512, KD, 512, ro=True)
        XL = A(4 * 515, 4, 515)
        GL = A(4 * 512, 4, 512)
        ylruT = A(4 * 512, 4, 512, ro=True)
        lt = [A(512) for _ in range(6)]
        QKVG = [A(4 * 512, 4, 512) for _ in range(4)]
        yretT = A(4 * 512, 4, 512, ro=True)
        rt_ = [A(512) for _ in range(7)]
        ropet = A(512, 4, 128)
        hfT = A(KD * 128, KD, 128)
        hfb = A(512, bf=True)
        rs = [A(80) for _ in range(8)]
        sm = [A(1) for _ in range(16)]
        rse = [A(4) for _ in range(4)]
        phaseA_end = off[0]

        add("vector", lambda e: e.memset(XL[:, :, 0:3], 0.0), writes=["XL"])
        zt = A(1024, bf=True)

        def rms_scale(xin, xkey, gbc, gkey, outap, outkey, tag, si=0):
            ss, rstd = sm[si], sm[si + 1]
            ssk, rk = "ss%d" % si, "rstd%d" % si
            add("scalar", lambda e: e.activation(out=outap, in_=xin, func=AF.Square, accum_out=ss),
                reads=[xkey], writes=[outkey, ssk])
            add("vector", lambda e: e.tensor_scalar(ss, ss, 1.0 / D, EPS, ALU.mult, ALU.add), reads=[ssk], writes=[ssk])
            add("scalar", lambda e: e.activation(out=ss, in_=ss, func=AF.Sqrt), reads=[ssk], writes=[ssk])
            add("vector", lambda e: e.reciprocal(rstd, ss), reads=[ssk], writes=[rk])
            add("vector", lambda e: e.scalar_tensor_tensor(outap, xin, rstd, gbc, ALU.mult, ALU.mult),
                reads=[xkey, rk, gkey], writes=[outkey])

        def transpose8(src, srckey, dst3, dstkey, n=8, rounded=True, eng="scalar"):
            pk = ["psT", "psT2"] if n > 4 else ["psT"]
            for c in range(n):
                add("tensor", lambda e, c=c: e.transpose(psT[:, c, :], src[:, c * 128:(c + 1) * 128], ident),
                    reads=[srckey, "ident"], writes=pk)
            o = dst3.bitcast(F32R) if rounded else dst3
            if eng == "scalar":
                add("scalar", lambda e: e.activation(out=o, in_=psT[:, 0:n, :], func=AF.Copy), reads=pk, writes=[dstkey])
            else:
                add("vector", lambda e: e.tensor_copy(o, psT[:, 0:n, :]), reads=pk, writes=[dstkey])

        wcount = [0]

        def load_group(src_ap, nk=KD):
            k = wcount[0] % 2
            wcount[0] += 1
            dst = wbuf[k].bitcast(F32R).rearrange("p c n -> p (c n)")
            add("gpsimd", lambda e: e.dma_start(out=dst, in_=src_ap, max_dma_last_dim=8192),
                writes=["wbuf%d" % k], dma=True)
            return wbuf[k].bitcast(F32R), "wbuf%d" % k

        pp = [0]

        def next_ps():
            pp[0] ^= 1
            return (psA, "psA") if pp[0] else (psB, "psB")

        for stile in range(NST):
            for j in range(4):
                t = stile * 4 + j
                add("sync", lambda e, j=j, t=t: e.dma_start(out=xt[j], in_=x_d[t * 128:(t + 1) * 128, :]),
                    writes=["xt%d" % j], dma=True)
                rms_scale(xt[j], "xt%d" % j, gmix, "gmix", hn, "hn", "a")
                transpose8(hn, "hn", hT[:, :, j * 128:(j + 1) * 128], "hT")
            XS_v = XS_d.rearrange("(p r) d -> p (r d)", p=128)
            nz = (NE * CAP // 128) * D // 2048

            def emit_zero(part):
                if part == 0:
                    add("vector", lambda e: e.memset(zt, 0.0), writes=["zt"])
                for kz in range(part * nz // 4, (part + 1) * nz // 4):
                    add("sync", lambda e, kz=kz: e.dma_start(out=XS_v[:, kz * 2048:(kz + 1) * 2048], in_=zt),
                        reads=["zt", "ylruT"], writes=["XSz%d" % kz], dma=True)
                if part == 3:
                    add("vector", lambda e: e.memset(bscr, 0.0), reads=["XSz%d" % kz for kz in range(nz)], writes=["XSd"])
            hTr = hT.bitcast(F32R)
            for g, (dst, dkey, o0) in enumerate([(XL, "XL", 3), (GL, "GL", 0)]):
                wb, wkey = load_group(w_in_d[g])
                for c in range(4):
                    ps, pkey = next_ps()
                    for kc in range(KD):
                        add("tensor", lambda e, ps=ps, wb=wb, kc=kc, c=c: e.matmul(
                            ps, wb[:, kc, c * 128:(c + 1) * 128], hTr[:, kc, :], start=(kc == 0), stop=(kc == KD - 1)),
                            reads=[wkey, "hT"], writes=[pkey])
                    add("scalar", lambda e, ps=ps, dst=dst, c=c, o0=o0: e.activation(out=dst[:, c, o0:o0 + 512], in_=ps, func=AF.Copy),
                        reads=[pkey], writes=[dkey])
            def stage_c(c):
                xc, r_, i_, a_, u_, g_ = lt
                add("vector", lambda e, c=c: e.tensor_scalar(xc, XL[:, c, 3:515], cw[:, c, 3:4], cb[:, c:c + 1], ALU.mult, ALU.add),
                    reads=["XL", "cw", "cb"], writes=["xc"])
                for k in (2, 1, 0):
                    add("vector", lambda e, c=c, k=k: e.scalar_tensor_tensor(xc, XL[:, c, k:k + 512], cw[:, c, k:k + 1], xc, ALU.mult, ALU.add),
                        reads=["XL", "cw", "xc"], writes=["xc"])
                add("tensor", lambda e, c=c: e.matmul(psP[:, 0:512], WA[:, c, :], xc, start=True, stop=True), reads=["WA", "xc"], writes=["psP0"])
                add("tensor", lambda e, c=c: e.matmul(psP[:, 512:1024], WX[:, c, :], xc, start=True, stop=True), reads=["WX", "xc"], writes=["psP1"])
                add("scalar", lambda e, c=c: e.activation(out=r_, in_=psP[:, 0:512], func=AF.Sigmoid, bias=ba[:, c:c + 1]), reads=["psP0", "ba"], writes=["r_"])
                add("scalar", lambda e, c=c: e.activation(out=i_, in_=psP[:, 512:1024], func=AF.Sigmoid, bias=bx[:, c:c + 1]), reads=["psP1", "bx"], writes=["i_"])
                add("scalar", lambda e, c=c: e.activation(out=a_, in_=r_, func=AF.Exp, scale=cneg[:, c:c + 1]), reads=["r_", "cneg"], writes=["a_"])
                add("scalar", lambda e, c=c: e.activation(out=u_, in_=r_, func=AF.Exp, scale=c2[:, c:c + 1]), reads=["r_", "c2"], writes=["u_"])
                add("vector", lambda e: e.tensor_scalar(u_, u_, -1.0, 1.0, ALU.mult, ALU.add), reads=["u_"], writes=["u_"])
                add("scalar", lambda e: e.activation(out=u_, in_=u_, func=AF.Sqrt), reads=["u_"], writes=["u_"])
                add("vector", lambda e: e.tensor_tensor(i_, i_, xc, ALU.mult), reads=["i_", "xc"], writes=["i_"])
                add("vector", lambda e: e.tensor_tensor(u_, u_, i_, ALU.mult), reads=["u_", "i_"], writes=["u_"])
                add("vector", lambda e, c=c: e.tensor_tensor_scan(r_, a_, u_, hstate[:, c:c + 1], ALU.mult, ALU.add),
                    reads=["a_", "u_", "hstate"], writes=["r_"])
                add("vector", lambda e, c=c: e.tensor_copy(hstate[:, c:c + 1], r_[:, 511:512]), reads=["r_"], writes=["hstate"])
                add("vector", lambda e, c=c: e.tensor_tensor(g_, GL[:, c, :], GL[:, c, :], ALU.mult), reads=["GL"], writes=["g_"])
                add("vector", lambda e: e.tensor_scalar(g_, g_, 0.044715, 1.0, ALU.mult, ALU.add), reads=["g_"], writes=["g_"])
                add("vector", lambda e, c=c: e.tensor_tensor(g_, g_, GL[:, c, :], ALU.mult), reads=["g_", "GL"], writes=["g_"])
                add("scalar", lambda e: e.activation(out=g_, in_=g_, func=AF.Sigmoid, scale=1.5957691216057308), reads=["g_"], writes=["g_"])
                add("vector", lambda e, c=c: e.tensor_tensor(g_, g_, GL[:, c, :], ALU.mult), reads=["g_", "GL"], writes=["g_"])
                add("vector", lambda e, c=c: e.tensor_tensor(ylruT[:, c, :].bitcast(F32R), r_, g_, ALU.mult), reads=["r_", "g_"], writes=["ylruT"])

            def stage_d(gi):
                wb, wkey = load_group(w_in_d[2 + gi])
                for j in range(4):
                    ps, pkey = next_ps()
                    for kc in range(KD):
                        add("tensor", lambda e, ps=ps, wb=wb, kc=kc, j=j: e.matmul(
                            ps, hTr[:, kc, j * 128:(j + 1) * 128], wb[:, kc, :], start=(kc == 0), stop=(kc == KD - 1)),
                            reads=[wkey, "hT"], writes=[pkey])
                    add("scalar", lambda e, ps=ps, gi=gi, j=j: e.activation(out=QKVG[gi][:, j, :], in_=ps, func=AF.Copy),
                        reads=[pkey], writes=["qkvg%d_%d" % (gi, j)])

            def stage_e(j):
                t = stile * 4 + j
                Qj = QKVG[0][:, j, :].rearrange("p (h d) -> p h d", h=4)
                Kj = QKVG[1][:, j, :].rearrange("p (h d) -> p h d", h=4)
                Vj = QKVG[2][:, j, :].rearrange("p (h d) -> p h d", h=4)
                Gj = QKVG[3][:, j, :]
                qr, kr, ta, qT, qdT, kT, kd = [r.rearrange("p (h d) -> p h d", h=4) for r in rt_]
                osb, osq, sT = qr, kr, ta
                add("sync", lambda e, t=t: e.dma_start(out=ropet, in_=rope_d[t * 128:(t + 1) * 128, :, :]), writes=["rope"], dma=True)

                def rotary(src, skey, dst, dkey, ci):
                    cosb = ropet[:, ci, :].unsqueeze(1).to_broadcast([128, 4, 128])
                    add("vector", lambda e: e.tensor_tensor(dst, src, cosb, ALU.mult), reads=[skey, "rope"], writes=[dkey])
                    s_lo = ropet[:, ci + 1, 0:64].unsqueeze(1).to_broadcast([128, 4, 64])
                    s_hi = ropet[:, ci + 1, 64:128].unsqueeze(1).to_broadcast([128, 4, 64])
                    add("vector", lambda e: e.tensor_tensor(ta[:, :, 0:64], src[:, :, 64:128], s_lo, ALU.mult), reads=[skey, "rope"], writes=["R2"])
                    add("vector", lambda e: e.tensor_tensor(ta[:, :, 64:128], src[:, :, 0:64], s_hi, ALU.mult), reads=[skey, "rope"], writes=["R2"])
                    add("vector", lambda e: e.tensor_tensor(dst, dst, ta, ALU.add), reads=[dkey, "R2"], writes=[dkey])

                rotary(Qj, "qkvg0_%d" % j, qr, "R0", 0)
                rotary(Kj, "qkvg1_%d" % j, kr, "R1", 2)
                for h in range(4):
                    add("tensor", lambda e, h=h: e.transpose(psT[:, h, :], qr[:, h, :], ident), reads=["R0", "ident"], writes=["psT"])
                add("scalar", lambda e: e.activation(out=qT, in_=psT[:, 0:4, :], func=AF.Copy), reads=["psT"], writes=["R3"])
                add("vector", lambda e: e.tensor_tensor(qdT, qT, QDt, ALU.mult), reads=["R3", "QD"], writes=["R4"])
                for h in range(4):
                    add("tensor", lambda e, h=h: e.transpose(psT[:, h, :], kr[:, h, :], ident), reads=["R1", "ident"], writes=["psT"])
                add("scalar", lambda e: e.activation(out=kT, in_=psT[:, 0:4, :], func=AF.Copy), reads=["psT"], writes=["R5"])
                add("vector", lambda e: e.tensor_tensor(kd, kr, KDt.unsqueeze(2).to_broadcast([128, 4, 128]), ALU.mult), reads=["R1", "KD"], writes=["R6"])
                psA3 = psA.rearrange("p (h d) -> p h d", h=4)
                psB3 = psB.rearrange("p (h d) -> p h d", h=4)
                psO3 = psO[:, 0:512].rearrange("p (h d) -> p h d", h=4)
                for h in range(4):
                    add("tensor", lambda e, h=h: e.matmul(psA3[:, h, :], kT[:, h, :], qT[:, h, :], start=True, stop=True),
                        reads=["R5", "R3"], writes=["psA"])
                add("vector", lambda e: e.tensor_tensor(sT, psA3, decT, ALU.mult), reads=["psA", "decT"], writes=["R2"])
                for h in range(4):
                    add("tensor", lambda e, h=h, Vj=Vj: e.matmul(psB3[:, h, :], sT[:, h, :], Vj[:, h, :], start=True, stop=False),
                        reads=["R2", "qkvg2_%d" % j], writes=["psB"])
                    add("tensor", lambda e, h=h: e.matmul(psB3[:, h, :], qdT[:, h, :], state[:, h, :], start=False, stop=True),
                        reads=["R4", "state"], writes=["psB"])
                for h in range(4):
                    add("tensor", lambda e, h=h, Vj=Vj: e.matmul(psO3[:, h, :], kd[:, h, :], Vj[:, h, :], start=True, stop=True),
                        reads=["R6", "qkvg2_%d" % j], writes=["psO"])
                add("vector", lambda e: e.tensor_tensor(state, state, CDt.unsqueeze(2).to_broadcast([128, 4, 128]), ALU.mult),
                    reads=["state", "CD"], writes=["state"])
                add("vector", lambda e: e.tensor_tensor(state, state, psO3, ALU.add), reads=["state", "psO"], writes=["state"])
                s1, s2_, mu, var = rse
                add("scalar", lambda e: e.activation(out=osb, in_=psB3, func=AF.Copy), reads=["psB"], writes=["R0"])
                add("scalar", lambda e: e.activation(out=osq, in_=psB3, func=AF.Square), reads=["psB"], writes=["R1"])
                add("vector", lambda e: e.tensor_reduce(s1, osb, AX.X, ALU.add), reads=["R0"], writes=["s1"])
                add("vector", lambda e: e.tensor_reduce(s2_, osq, AX.X, ALU.add), reads=["R1"], writes=["s2_"])
                add("vector", lambda e: e.tensor_scalar(mu, s1, 1.0 / 128, None, ALU.mult), reads=["s1"], writes=["mu"])
                add("vector", lambda e: e.tensor_tensor(var, mu, mu, ALU.mult), reads=["mu"], writes=["var"])
                add("vector", lambda e: e.scalar_tensor_tensor(var, s2_, 1.0 / 128, var, ALU.mult, ALU.subtract), reads=["s2_", "var"], writes=["var"])
                add("vector", lambda e: e.tensor_scalar(var, var, EPS, None, ALU.add), reads=["var"], writes=["var"])
                add("scalar", lambda e: e.activation(out=var, in_=var, func=AF.Sqrt), reads=["var"], writes=["var"])
                add("vector", lambda e: e.reciprocal(var, var), reads=["var"], writes=["var"])
                add("vector", lambda e: e.tensor_tensor(osb, osb, mu.unsqueeze(2).to_broadcast([128, 4, 128]), ALU.subtract), reads=["R0", "mu"], writes=["R0"])
                add("vector", lambda e: e.tensor_tensor(osb, osb, var.unsqueeze(2).to_broadcast([128, 4, 128]), ALU.mult), reads=["R0", "var"], writes=["R0"])
                osq2 = rt_[1]
                osb2 = rt_[0]
                add("scalar", lambda e, Gj=Gj: e.activation(out=osq2, in_=Gj, func=AF.Silu), reads=["qkvg3_%d" % j], writes=["R1"])
                add("vector", lambda e: e.tensor_tensor(osb2, osb2, osq2, ALU.mult), reads=["R0", "R1"], writes=["R0"])
                for h in range(4):
                    add("tensor", lambda e, h=h: e.transpose(psT[:, h, :], osb[:, h, :], ident), reads=["R0", "ident"], writes=["psT"])
                add("scalar", lambda e, j=j: e.activation(out=yretT[:, j, :].rearrange("p (h d) -> p h d", h=4).bitcast(F32R),
                                                        in_=psT[:, 0:4, :], func=AF.Copy), reads=["psT"], writes=["yretT%d" % j])

            ylr = ylruT.bitcast(F32R)
            yrr = yretT.bitcast(F32R)

            def stage_f(j, wbs):
                for half in range(2):
                    wb, wkey = wbs[half]
                    for kc in range(KD):
                        if kc < 4:
                            lhs = ylr[:, kc, j * 128:(j + 1) * 128]
                            rk = "ylruT"
                        else:
                            lhs = yrr[:, j, (kc - 4) * 128:(kc - 3) * 128]
                            rk = "yretT%d" % j
                        add("tensor", lambda e, lhs=lhs, wb=wb, kc=kc, half=half: e.matmul(
                            psP[:, half * 512:(half + 1) * 512], lhs, wb[:, kc, :], start=(kc == 0), stop=(kc == KD - 1)),
                            reads=[wkey, rk], writes=["psP%d" % half])
                add("vector", lambda e: e.tensor_tensor(xt[j], xt[j], psP, ALU.add), reads=["psP0", "psP1", "xt%d" % j], writes=["xt%d" % j])

            def stage_g(j):
                t = stile * 4 + j
                x1 = xt[j]
                xk = "xt%d" % j
                add("sync", lambda e, t=t, x1=x1: e.dma_start(out=X1_d[t * 128:(t + 1) * 128, :], in_=x1), reads=[xk], writes=["X1d"], dma=True)
                hf = hn
                rms_scale(x1, xk, gffn, "gffn", hf, "hn", "g")
                if debug == "hf":
                    add("sync", lambda e, t=t: e.dma_start(out=dbg_d[t * 128:(t + 1) * 128, :], in_=hf), reads=["hn"], dma=True)
                if debug == "x1":
                    add("sync", lambda e, t=t, x1=x1: e.dma_start(out=dbg_d[t * 128:(t + 1) * 128, :], in_=x1), reads=[xk], dma=True)
                add("scalar", lambda e: e.activation(out=hfb, in_=hf, func=AF.Copy), reads=["hn"], writes=["hfb"])
                for rnd in range(2):
                    for c in range(4):
                        add("tensor", lambda e, c=c, rnd=rnd: e.transpose(psT[:, 4 + c, :], hf[:, (rnd * 4 + c) * 128:(rnd * 4 + c + 1) * 128], ident),
                            reads=["hn", "ident"], writes=["psT2"])
                    add("scalar", lambda e, rnd=rnd: e.activation(out=hfT[:, rnd * 4:(rnd + 1) * 4, :], in_=psT[:, 4:8, :], func=AF.Copy),
                        reads=["psT2"], writes=["hfT"])
                psR = psO[:, 512:584]
                psC = psO[:, 640:704]
                for kc in range(KD):
                    add("tensor", lambda e, kc=kc: e.matmul(psR, hfT[:, kc, :], wrt[:, kc, :], start=(kc == 0), stop=(kc == KD - 1)),
                        reads=["hfT", "wrt"], writes=["psO1"])
                lg = rs[0][:, 0:72]
                goh, ein, mx8, oh1, oh2 = rs[1][:, 0:8], rs[2][:, 0:8], rs[3][:, 0:8], rs[4][:, 0:8], rs[5][:, 0:8]
                tmp64, A1, A2 = rs[6][:, 0:64], rs[7][:, 0:64], rs[1][:, 8:72]
                gmax, nmax, gsum, gw, dd = sm[2], sm[3], sm[4], sm[5], sm[6]
                gl_, el_ = lg[:, 0:8], lg[:, 8:72]
                add("vector", lambda e: e.tensor_tensor(lg, psR, brt, ALU.add), reads=["psO1", "brt"], writes=["lg"])
                add("vector", lambda e: e.tensor_reduce(gmax, gl_, AX.X, ALU.max), reads=["lg"], writes=["gmax"])
                add("vector", lambda e: e.tensor_scalar(goh, gl_, gmax, None, ALU.is_equal), reads=["lg", "gmax"], writes=["goh"])
                add("vector", lambda e: e.tensor_scalar(nmax, gmax, -1.0, None, ALU.mult), reads=["gmax"], writes=["nmax"])
                add("scalar", lambda e: e.activation(out=ein, in_=gl_, func=AF.Exp, bias=nmax, accum_out=gsum), reads=["lg", "nmax"], writes=["ein", "gsum"])
                add("vector", lambda e: e.reciprocal(gw, gsum), reads=["gsum"], writes=["gw"])
                add("vector", lambda e: e.tensor_tensor(tmp64.rearrange("p (g j) -> p g j", g=8), el_.rearrange("p (g j) -> p g j", g=8),
                                                        goh.unsqueeze(2).to_broadcast([128, 8, 8]), ALU.mult), reads=["lg", "goh"], writes=["tmp64"])
                add("vector", lambda e: e.tensor_reduce(ein, tmp64.rearrange("p (g j) -> p j g", g=8), AX.X, ALU.add), reads=["tmp64"], writes=["ein"])
                add("vector", lambda e: e.max(mx8, ein), reads=["ein"], writes=["mx8"])
                add("vector", lambda e: e.tensor_scalar(oh1, ein, mx8[:, 0:1], None, ALU.is_equal), reads=["ein", "mx8"], writes=["oh1"])
                add("vector", lambda e: e.tensor_scalar(oh2, ein, mx8[:, 1:2], None, ALU.is_equal), reads=["ein", "mx8"], writes=["oh2"])
                add("vector", lambda e: e.tensor_tensor(dd, mx8[:, 0:1], mx8[:, 1:2], ALU.subtract), reads=["mx8"], writes=["dd"])
                add("scalar", lambda e: e.activation(out=dd, in_=dd, func=AF.Sigmoid), reads=["dd"], writes=["dd"])
                add("vector", lambda e, t=t: e.tensor_tensor(wsel1[:, t:t + 1], dd, gw, ALU.mult), reads=["dd", "gw"], writes=["wsel1"])
                add("vector", lambda e, t=t: e.tensor_tensor(wsel2[:, t:t + 1], gw, wsel1[:, t:t + 1], ALU.subtract), reads=["gw", "wsel1"], writes=["wsel2"])
                gb = goh.unsqueeze(2).to_broadcast([128, 8, 8])
                add("vector", lambda e: e.tensor_tensor(A1.rearrange("p (g j) -> p g j", g=8), gb, oh1.unsqueeze(1).to_broadcast([128, 8, 8]), ALU.mult),
                    reads=["goh", "oh1"], writes=["A1"])
                add("vector", lambda e: e.tensor_tensor(A2.rearrange("p (g j) -> p g j", g=8), gb, oh2.unsqueeze(1).to_broadcast([128, 8, 8]), ALU.mult),
                    reads=["goh", "oh2"], writes=["A2"])
                add("vector", lambda e: e.tensor_tensor(tmp64, A1, A2, ALU.add), reads=["A1", "A2"], writes=["tmp64"])
                add("tensor", lambda e: e.matmul(psC, U_t, tmp64, start=True, stop=False), reads=["U", "tmp64"], writes=["psO1"])
                add("tensor", lambda e: e.matmul(psC, ones_t, Rcum, start=False, stop=True), reads=["ones", "Rcum"], writes=["psO1"])
                add("vector", lambda e: e.tensor_tensor(Rcum, Rcum, tmp64, ALU.add), reads=["Rcum", "tmp64"], writes=["Rcum"])
                pe_ = rs[6][:, 0:64]
                add("vector", lambda e: e.scalar_tensor_tensor(pe_, psC, float(CAP - 1), eCt, ALU.min, ALU.add), reads=["psO1", "eC", "tmp64"], writes=["tmp64"])
                add("vector", lambda e: e.tensor_tensor(A1, A1, pe_, ALU.mult), reads=["A1", "tmp64"], writes=["A1"])
                add("vector", lambda e: e.tensor_tensor(A2, A2, pe_, ALU.mult), reads=["A2", "tmp64"], writes=["A2"])
                d1f, d2f = sm[7], sm[8]
                add("vector", lambda e: e.tensor_reduce(d1f, A1, AX.X, ALU.add), reads=["A1"], writes=["d1f"])
                add("vector", lambda e: e.tensor_reduce(d2f, A2, AX.X, ALU.add), reads=["A2"], writes=["d2f"])
                add("vector", lambda e, t=t: e.tensor_scalar(dest1i[:, t:t + 1], d1f, float(NE * CAP - 1), None, ALU.min), reads=["d1f"], writes=["dest1"])
                add("vector", lambda e, t=t: e.tensor_scalar(dest2i[:, t:t + 1], d2f, float(NE * CAP - 1), None, ALU.min), reads=["d2f"], writes=["dest2"])
                if debug in ("x1", "hf"):
                    return
                add("gpsimd", lambda e, t=t: e.indirect_dma_start(out=XS_d, out_offset=bass.IndirectOffsetOnAxis(ap=dest1i[:, t:t + 1], axis=0),
                                                                  in_=hfb, in_offset=None), reads=["hfb", "dest1"], writes=["XSd"], dma=True)
                add("gpsimd", lambda e, t=t: e.indirect_dma_start(out=XS_d, out_offset=bass.IndirectOffsetOnAxis(ap=dest2i[:, t:t + 1], axis=0),
                                                                  in_=hfb, in_offset=None), reads=["hfb", "dest2"], writes=["XSd"], dma=True)


            stage_d(0)
            for gi in range(3):
                emit_merged(record(stage_c, gi), record(stage_d, gi + 1))
                if stile == 0:
                    emit_zero(gi)
            wbs = [load_group(w_out_d[half]) for half in range(2)]

            def stage_fg(j):
                stage_f(j, wbs)
                stage_g(j)

            emit_merged(record(stage_c, 3), record(stage_e, 0))
            if stile == 0:
                emit_zero(3)
            add("vector", lambda e: e.tensor_copy(XL[:, :, 0:3], XL[:, :, 512:515]), reads=["XL"], writes=["XL"])
            for j in range(4):
                emit_merged(record(stage_fg, j), record(stage_e, j + 1) if j + 1 < 4 else [])

        if debug in ("x1", "hf"):
            S_.emit(final_wait_ops=[op for op in S_.all_ops if op.is_dma])
            return nc
        S_.barrier(lambda e: e.memset(bscr, 0.0))
        off[0] = PL_BASE
        W1b = [A(KD * 256, KD, 512, bf=True) for _ in range(2)]
        W3b = [A(KD * 256, KD, 512, bf=True) for _ in range(2)]
        W2b = [A(4 * 512, 4, 1024, bf=True) for _ in range(2)]
        xs = [A(D, 2, D, bf=True) for _ in range(2)]
        xsT = A(KD * 128, KD, 256, bf=True)
        gT = A(4 * 128, 4, 256, bf=True)
        silt = A(256)
        yb = [A(D) for _ in range(2)]
        psTb = [psT_t[:, 0:512].bitcast(BF16).rearrange("p (a b) -> p a b", a=8),
                psT_t[:, 512:1024].bitcast(BF16).rearrange("p (a b) -> p a b", a=8)]
        psTk = ["psT", "psT2"]
        hbanks = [(psA, "psA"), (psB, "psB"), (psO[:, 0:512], "psO0"), (psO[:, 512:1024], "psO1")]
        ybanks = [(psP[:, 0:512], "psP0"), (psP[:, 512:1024], "psP1")]
        silt2 = [silt, A(256)]

        def b_loads(ex):
            k = ex % 2
            fl = lambda ap: ap.rearrange("p c n -> p (c n)")
            add("gpsimd", lambda e: e.dma_start(out=fl(W1b[k]), in_=w1_d[ex], max_dma_last_dim=8192), writes=["W1b%d" % k], dma=True)
            add("gpsimd", lambda e: e.dma_start(out=fl(W3b[k]), in_=w3_d[ex], max_dma_last_dim=8192), writes=["W3b%d" % k], dma=True)
            add("gpsimd", lambda e: e.dma_start(out=fl(W2b[k]), in_=w2_d[ex], max_dma_last_dim=8192), writes=["W2b%d" % k], dma=True)
            add("sync", lambda e: e.dma_start(out=xs[k], in_=XS_d[ex * CAP:(ex + 1) * CAP, :].rearrange("(b p) d -> p b d", p=128)),
                reads=["XSd"], writes=["xs%d" % k], dma=True)

        ycnt = [0]

        def b_compute(ex):
            k = ex % 2
            for blk in range(2):
                for c in range(KD):
                    add("tensor", lambda e, c=c, blk=blk: e.transpose(psTb[blk][:, c, :], xs[k][:, blk, c * 128:(c + 1) * 128], identb),
                        reads=["xs%d" % k, "identb"], writes=[psTk[blk]])
                if blk == 0:
                    add("scalar", lambda e: e.activation(out=xsT[:, :, 0:128], in_=psTb[0], func=AF.Copy), reads=[psTk[0]], writes=["xsT"])
                else:
                    add("vector", lambda e: e.tensor_copy(xsT[:, :, 128:256], psTb[1]), reads=[psTk[1]], writes=["xsT"])
            for f in range(4):
                (h1, h1k), (h3, h3k) = hbanks[2 * (f % 2)], hbanks[2 * (f % 2) + 1]
                sl = silt2[f % 2]
                slk = "silt%d" % (f % 2)
                for kc in range(KD):
                    add("tensor", lambda e, f=f, kc=kc, h1=h1: e.matmul(h1[:, 0:256], W1b[k][:, kc, f * 128:(f + 1) * 128], xsT[:, kc, :],
                                                                     start=(kc == 0), stop=(kc == KD - 1)), reads=["W1b%d" % k, "xsT"], writes=[h1k])
                for kc in range(KD):
                    add("tensor", lambda e, f=f, kc=kc, h3=h3: e.matmul(h3[:, 0:256], W3b[k][:, kc, f * 128:(f + 1) * 128], xsT[:, kc, :],
                                                                     start=(kc == 0), stop=(kc == KD - 1)), reads=["W3b%d" % k, "xsT"], writes=[h3k])
                add("scalar", lambda e, h1=h1, sl=sl: e.activation(out=sl, in_=h1[:, 0:256], func=AF.Silu), reads=[h1k], writes=[slk])
                add("vector", lambda e, f=f, h3=h3, sl=sl: e.tensor_tensor(gT[:, f, :], sl, h3[:, 0:256], ALU.mult), reads=[slk, h3k], writes=["gT%d" % f])
            for blk in range(2):
                yk = ycnt[0] % 2
                ycnt[0] += 1
                for half in range(2):
                    yp, ypk = ybanks[half]
                    for f in range(4):
                        add("tensor", lambda e, yp=yp, half=half, f=f, blk=blk: e.matmul(
                            yp, gT[:, f, blk * 128:(blk + 1) * 128], W2b[k][:, f, half * 512:(half + 1) * 512],
                            start=(f == 0), stop=(f == 3)), reads=["gT%d" % f, "W2b%d" % k], writes=[ypk])
                    if half == 0:
                        add("scalar", lambda e, yk=yk, yp=yp: e.activation(out=yb[yk][:, 0:512], in_=yp, func=AF.Copy), reads=[ypk], writes=["yb%d" % yk])
                    else:
                        add("vector", lambda e, yk=yk, yp=yp: e.tensor_copy(yb[yk][:, 512:1024], yp), reads=[ypk], writes=["yb%d" % yk])
                r0 = ex * CAP + blk * 128
                add("sync", lambda e, yk=yk, r0=r0: e.dma_start(out=Y_d[r0:r0 + 128, :], in_=yb[yk]), reads=["yb%d" % yk], writes=["Yd"], dma=True)

        roff[0] = RO_BASE
        WG = A(KD * D, KD, D, ro=True)
        WP = A(2 * D, 2, D, ro=True)
        gple = A(D)
        gfin = A(D)
        bple = A(D)
        x1c = [A(D) for _ in range(2)]
        yac = [A(D) for _ in range(2)]
        ybc = [A(D) for _ in range(2)]
        pt = [A(256) for _ in range(2)]
        hp = A(D)
        hpT2 = [A(KD * 128, KD, 128, ro=True) for _ in range(2)]
        pT2 = [A(256, 2, 128, ro=True) for _ in range(2)]
        gate = A(D)
        outt = [A(D) for _ in range(2)]
        sm = [A(1) for _ in range(4)]
        add("gpsimd", lambda e: e.dma_start(out=WG.bitcast(F32R), in_=wg_d.rearrange("(c p) n -> p c n", p=128)), writes=["WG"], dma=True)
        add("gpsimd", lambda e: e.dma_start(out=WP.bitcast(F32R), in_=wp_d.rearrange("(c p) n -> p c n", p=128)), writes=["WP"], dma=True)
        for (dst, src, key) in [(gple, gple_d, "gple"), (gfin, gfin_d, "gfin"), (bple, bple_d, "bple")]:
            add("sync", lambda e, dst=dst, src=src: e.dma_start(out=dst, in_=src), writes=[key], dma=True)
        WGr = WG.bitcast(F32R)
        WPr = WP.bitcast(F32R)
        b_loads(0)
        for ex in range(NE):
            if ex + 1 < NE:
                b_loads(ex + 1)
            b_compute(ex)

        def c_loads(t):
            k = t % 2
            add("sync", lambda e: e.dma_start(out=x1c[k], in_=X1_d[t * 128:(t + 1) * 128, :]), reads=["X1d"], writes=["x1c%d" % k], dma=True)
            add("sync", lambda e: e.dma_start(out=pt[k], in_=p_d[t * 128:(t + 1) * 128, :]), writes=["pt%d" % k], dma=True)
            add("gpsimd", lambda e: e.indirect_dma_start(out=yac[k], out_offset=None, in_=Y_d,
                                                         in_offset=bass.IndirectOffsetOnAxis(ap=dest1i[:, t:t + 1], axis=0)),
                reads=["Yd", "dest1"], writes=["yac%d" % k], dma=True)
            add("gpsimd", lambda e: e.indirect_dma_start(out=ybc[k], out_offset=None, in_=Y_d,
                                                         in_offset=bass.IndirectOffsetOnAxis(ap=dest2i[:, t:t + 1], axis=0)),
                reads=["Yd", "dest2"], writes=["ybc%d" % k], dma=True)

        def c_stage1(t):
            k = t % 2
            x2 = x1c[k]
            xk = "x1c%d" % k
            hpT, pT_ = hpT2[k], pT2[k]
            add("vector", lambda e: e.scalar_tensor_tensor(x2, yac[k], wsel1[:, t:t + 1], x2, ALU.mult, ALU.add),
                reads=["yac%d" % k, "wsel1", xk], writes=[xk])
            add("vector", lambda e: e.scalar_tensor_tensor(x2, ybc[k], wsel2[:, t:t + 1], x2, ALU.mult, ALU.add),
                reads=["ybc%d" % k, "wsel2", xk], writes=[xk])
            rms_scale(x2, xk, gple, "gple", hp, "hp", "c", si=0)
            transpose8(hp, "hp", hpT, "hpT%d" % k)
            for c in range(2):
                add("tensor", lambda e, c=c: e.transpose(psA[:, c * 128:(c + 1) * 128], pt[k][:, c * 128:(c + 1) * 128], ident),
                    reads=["pt%d" % k, "ident"], writes=["psA"])
            add("vector", lambda e: e.tensor_copy(pT_.bitcast(F32R), psA[:, 0:256].rearrange("p (a b) -> p a b", a=2)), reads=["psA"], writes=["pT_%d" % k])

        def c_stage2(t):
            k = t % 2
            x2 = x1c[k]
            xk = "x1c%d" % k
            hpTr = hpT2[k].bitcast(F32R)
            pTr = pT2[k].bitcast(F32R)
            for half in range(2):
                for kc in range(KD):
                    add("tensor", lambda e, half=half, kc=kc: e.matmul(psO[:, half * 512:(half + 1) * 512], hpTr[:, kc, :], WGr[:, kc, half * 512:(half + 1) * 512],
                                                                      start=(kc == 0), stop=(kc == KD - 1)), reads=["hpT%d" % k, "WG"], writes=["psO0", "psO1"])
            for half in range(2):
                for kc in range(2):
                    add("tensor", lambda e, half=half, kc=kc: e.matmul(psP[:, half * 512:(half + 1) * 512], pTr[:, kc, :], WPr[:, kc, half * 512:(half + 1) * 512],
                                                                      start=(kc == 0), stop=(kc == 1)), reads=["pT_%d" % k, "WP"], writes=["psP0", "psP1"])
            add("vector", lambda e: e.tensor_tensor(gate, psO, bple, ALU.add), reads=["psO0", "psO1", "bple"], writes=["gate"])
            add("scalar", lambda e: e.activation(out=gate, in_=gate, func=AF.Sigmoid), reads=["gate"], writes=["gate"])
            add("vector", lambda e: e.tensor_tensor(gate, gate, psP, ALU.mult), reads=["gate", "psP0", "psP1"], writes=["gate"])
            add("vector", lambda e: e.tensor_tensor(x2, x2, gate, ALU.add), reads=["gate", xk], writes=[xk])
            rms_scale(x2, xk, gfin, "gfin", outt[k], "outt%d" % k, "f", si=2)
            add("sync", lambda e: e.dma_start(out=out_d[t * 128:(t + 1) * 128, :], in_=outt[k]), reads=["outt%d" % k], dma=True)

        c_loads(0)
        if NT > 1:
            c_loads(1)
        c_stage1(0)
        for t in range(NT):
            ch2 = record(c_stage2, t)
            ch1 = record(c_stage1, t + 1) if t + 1 < NT else []
            emit_merged(ch2, ch1)
            if t + 2 < NT:
                c_loads(t + 2)

        S_.emit(final_wait_ops=[op for op in S_.all_ops if op.is_dma])
    return nc


def core_inputs(b, S, consts, shared, x, p, g_mix, w_in, conv_w, conv_b, lru_wa, lru_ba, lru_wx, lru_bx, lru_lambda, w_out,
                g_ffn, w_router_group, b_router_group, w_router_expert, b_router_expert, w1, w3, w2,
                g_ple, w_ple_gate, b_ple_gate, w_ple_proj, g_final):
    f = lambda a: np.ascontiguousarray(a, dtype=np.float32)
    m = dict(
        x=f(x[b]), p=f(p[0, b]), w_in=shared["w_in"], w_out=shared["w_out"], w1=shared["w1"], w3=shared["w3"], w2=shared["w2"],
        w_ple_gate=f(w_ple_gate[0]), w_ple_proj=f(w_ple_proj[0]),
        w_rt=f(np.concatenate([w_router_group[0], w_router_expert[0]], axis=1)),
        b_rt=bc128(np.concatenate([b_router_group[0], b_router_expert[0]], axis=0)),
        g_mix=bc128(g_mix[0]), g_ffn=bc128(g_ffn[0]), g_ple=bc128(g_ple[0]), g_final=bc128(g_final), b_ple=bc128(b_ple_gate[0]),
        conv_w=f(np.transpose(np.asarray(conv_w[0]).reshape(4, 4, 128), (2, 1, 0))),
        conv_b=chan_major(conv_b[0]),
        lru_wa=block_diag(np.asarray(lru_wa[0])), lru_wx=block_diag(np.asarray(lru_wx[0])),
        lru_ba=chan_major(np.asarray(lru_ba[0]).reshape(-1)), lru_bx=chan_major(np.asarray(lru_bx[0]).reshape(-1)),
        lru_lam=chan_major(lru_lambda[0]),
    )
    m.update(consts)
    return m


def shared_layout(w_in, w_out, w1, w3, w2):
    def pmajor(w):
        lead = w.shape[:-2]
        C = w.shape[-2] // 128
        n = w.shape[-1]
        v = np.asarray(w, np.float32).reshape(*lead, C, 128, n)
        v = np.moveaxis(v, -3, -2)
        return np.ascontiguousarray(v).reshape(*lead, 128, C * n)
    w_in_g = np.stack([np.asarray(w_in[0])[:, g * 512:(g + 1) * 512] for g in range(6)], axis=0)
    w_out_g = np.stack([np.asarray(w_out[0])[:, h * 512:(h + 1) * 512] for h in range(2)], axis=0)
    return dict(w_in=pmajor(w_in_g), w_out=pmajor(w_out_g), w1=pmajor(w1[0]), w3=pmajor(w3[0]), w2=pmajor(w2[0]))


_NC_CACHE = {}


def kernel(**inputs):
    inputs = {k: np.asarray(v) for k, v in inputs.items()}
    x = inputs["x"]
    B, S, _ = x.shape
    consts = make_consts(S)
    if S not in _NC_CACHE:
        _NC_CACHE[S] = build_nc(S)
    nc = _NC_CACHE[S]
    shared = shared_layout(inputs["w_in"], inputs["w_out"], inputs["w1"], inputs["w3"], inputs["w2"])
    in_maps = [core_inputs(b, S, consts, shared, **inputs) for b in range(B)]
    res = run_bass_kernel_spmd(nc, in_maps, core_ids=list(range(B)))
    return np.stack([np.asarray(r["out"]) for r in res.results], axis=0).astype(np.float32)
```
